# Optimizing a Trainium2 kernel written in Bass

```python
import math
import jax, jax.numpy as jnp
from jax import lax
import numpy as np

D_MODEL = 1024
BATCH = 2
SEQ = 8192
DEPTH = 1

D_MIX = D_MODEL
HG_HEADS = 4
HG_DK = 128
HG_DV = 128
HG_WIDTH = HG_HEADS * HG_DV
HG_CHUNK = 64
SG_GROUPS = 4
SG_CH = 128
SG_WIDTH = SG_GROUPS * SG_CH
SG_CHUNK = 128
IN_COLS = 3 * HG_HEADS * HG_DK + HG_WIDTH + 2 * SG_WIDTH
SPLIT_POINTS = (HG_HEADS * HG_DK, 2 * HG_HEADS * HG_DK, 2 * HG_HEADS * HG_DK + HG_WIDTH,
                2 * HG_HEADS * HG_DK + 2 * HG_WIDTH)
MOE_GROUPS = 4
MOE_EXPERTS_PER_GROUP = 8
MOE_EXPERTS = MOE_GROUPS * MOE_EXPERTS_PER_GROUP
MOE_TOP_K = 2
MOE_HIDDEN = 512
MOE_BLOCK = 128
ALPHA = (2.0 * DEPTH) ** 0.25
BETA = (8.0 * DEPTH) ** -0.25
LN_EPS = 1e-5
RMS_EPS = 1e-6

kernel_name = "hymba_hgrn2_gmlp_hiermoe_deepnorm_adaln"


def _layernorm(x, g, b):
    xf = x.astype(jnp.float32)
    mu = jnp.mean(xf, axis=-1, keepdims=True)
    var = jnp.mean(jnp.square(xf - mu), axis=-1, keepdims=True)
    y = (xf - mu) * lax.rsqrt(var + LN_EPS) * g.astype(jnp.float32) + b.astype(jnp.float32)
    return y.astype(x.dtype)


def _modulate(x, shift, scale):
    return x * (1.0 + scale[:, None, :]) + shift[:, None, :]


def _to_chunks(t, c):
    b, s, h, d = t.shape
    return t.reshape(b, s // c, c, h, d).transpose(1, 0, 3, 2, 4)


def _hgrn2_mixer(q, fz, iv, og, lb, gn_w):
    b, s, _ = q.shape
    dt = q.dtype
    q = jax.nn.silu(q.astype(jnp.float32)).reshape(b, s, HG_HEADS, HG_DK)
    lbh = lb.reshape(HG_HEADS, HG_DK)
    f = lbh + (1.0 - lbh) * jax.nn.sigmoid(fz.astype(jnp.float32).reshape(b, s, HG_HEADS, HG_DK))
    logf = jnp.log(f)
    k = 1.0 - f
    v = iv.astype(jnp.float32).reshape(b, s, HG_HEADS, HG_DV)
    qc, kc, vc, lfc = (_to_chunks(t, HG_CHUNK) for t in (q, k, v, logf))
    causal = jnp.tril(jnp.ones((HG_CHUNK, HG_CHUNK), dtype=bool))

    def step(state, inp):
        qq, kk, vv, lf = inp
        cum = jnp.cumsum(lf, axis=2)
        diff = cum[:, :, :, None, :] - cum[:, :, None, :, :]
        decay = jnp.exp(jnp.where(causal[None, None, :, :, None], diff, -jnp.inf))
        scores = jnp.einsum('bhtd,bhsd,bhtsd->bhts', qq, kk, decay)
        o_intra = jnp.einsum('bhts,bhsv->bhtv', scores, vv)
        o_inter = jnp.einsum('bhtd,bhdv->bhtv', qq * jnp.exp(cum), state)
        last = cum[:, :, -1:, :]
        new_state = jnp.exp(last[:, :, 0, :])[..., None] * state + \
            jnp.einsum('bhsd,bhsv->bhdv', kk * jnp.exp(last - cum), vv)
        return new_state, o_intra + o_inter

    s0 = jnp.zeros((b, HG_HEADS, HG_DK, HG_DV), jnp.float32)
    _, oc = lax.scan(step, s0, (qc, kc, vc, lfc))
    o = oc.transpose(1, 0, 3, 2, 4).reshape(b, s, HG_HEADS, HG_DV)
    o = o * lax.rsqrt(jnp.mean(jnp.square(o), axis=-1, keepdims=True) + RMS_EPS) * gn_w.astype(jnp.float32)
    o = o * jax.nn.silu(og.astype(jnp.float32).reshape(b, s, HG_HEADS, HG_DV))
    return o.reshape(b, s, HG_WIDTH).astype(dt)


def _spatial_gating_mixer(z, ln_g, ln_b, w_s, b_s):
    b, s, _ = z.shape
    z = jax.nn.gelu(z, approximate=False)
    u, v = z[..., :SG_WIDTH], z[..., SG_WIDTH:]
    v = _layernorm(v, ln_g, ln_b)
    v = v.reshape(b, s // SG_CHUNK, SG_CHUNK, SG_GROUPS, SG_CH)
    w_causal = w_s * jnp.tril(jnp.ones((SG_CHUNK, SG_CHUNK), dtype=w_s.dtype))
    mixed = jnp.einsum('gts,bnsgc->bntgc', w_causal, v) + b_s.T[None, None, :, :, None]
    return u * mixed.reshape(b, s, SG_WIDTH)


def _hier_moe(h, rg_w, rg_b, re_w, re_b, w_up, w_down):
    b, s, d = h.shape
    t = b * s
    xf = h.reshape(t, d)
    x32 = xf.astype(jnp.float32)
    g_logits = x32 @ rg_w.astype(jnp.float32) + rg_b.astype(jnp.float32)
    g_prob = jax.nn.softmax(g_logits, axis=-1)
    p_group, g_idx = lax.top_k(g_prob, 1)
    e_logits_all = jnp.einsum('td,gde->tge', x32, re_w.astype(jnp.float32)) + re_b.astype(jnp.float32)
    e_logits = jnp.take_along_axis(e_logits_all, g_idx[:, :, None], axis=1)[:, 0]
    e_prob = jax.nn.softmax(e_logits, axis=-1)
    top_p, top_j = lax.top_k(e_prob, MOE_TOP_K)
    weights = p_group * top_p / jnp.sum(top_p, axis=-1, keepdims=True)
    expert = g_idx * MOE_EXPERTS_PER_GROUP + top_j

    n_assign = t * MOE_TOP_K
    e_flat = expert.reshape(n_assign)
    tok_flat = jnp.repeat(jnp.arange(t, dtype=jnp.int32), MOE_TOP_K)
    w_flat = weights.reshape(n_assign)
    order = jnp.argsort(e_flat)
    e_sorted, tok_sorted, w_sorted = e_flat[order], tok_flat[order], w_flat[order]
    counts = jax.ops.segment_sum(jnp.ones((n_assign,), jnp.int32), e_flat, num_segments=MOE_EXPERTS)
    start = jnp.cumsum(counts) - counts
    padded = (counts + MOE_BLOCK - 1) // MOE_BLOCK * MOE_BLOCK
    pend = jnp.cumsum(padded)
    pstart = pend - padded
    dest = pstart[e_sorted] + (jnp.arange(n_assign, dtype=jnp.int32) - start[e_sorted])
    n_blocks = -(-n_assign // MOE_BLOCK) + MOE_EXPERTS
    n_rows = n_blocks * MOE_BLOCK
    pad_tok = jnp.zeros((n_rows,), jnp.int32).at[dest].set(tok_sorted)
    pad_w = jnp.zeros((n_rows,), jnp.float32).at[dest].set(w_sorted)
    block_e = jnp.minimum(jnp.searchsorted(pend, jnp.arange(n_blocks, dtype=jnp.int32) * MOE_BLOCK,
                                           side='right'), MOE_EXPERTS - 1).astype(jnp.int32)
    xb = xf[pad_tok].reshape(n_blocks, MOE_BLOCK, d)

    def expert_block(args):
        xblk, e = args
        gu = xblk @ w_up[e]
        return (jax.nn.silu(gu[:, :MOE_HIDDEN]) * gu[:, MOE_HIDDEN:]) @ w_down[e]

    yb = lax.map(expert_block, (xb, block_e)).reshape(n_rows, d)
    y = (yb.astype(jnp.float32) * pad_w[:, None]).astype(h.dtype)
    out = jnp.zeros((t, d), h.dtype).at[pad_tok].add(y)
    return out.reshape(b, s, d)


def setup_inputs(seed: int = 0) -> dict:
    key = jax.random.key(seed)
    ks = jax.random.split(key, 24)
    f32 = jnp.float32
    L, D = DEPTH, D_MODEL

    def nrm(k, shape, scale):
        return jax.random.normal(k, shape, f32) * scale

    return {
        "x": nrm(ks[0], (BATCH, SEQ, D), 1.0),
        "c": nrm(ks[1], (BATCH, D), 1.0),
        "w_ada": nrm(ks[2], (L, D, 6 * D), 0.1 * D ** -0.5),
        "b_ada": nrm(ks[3], (L, 6 * D), 0.01),
        "w_in": nrm(ks[4], (L, D, IN_COLS), D ** -0.5),
        "lb_logits": nrm(ks[5], (L + 1, HG_HEADS * HG_DK), 1.0),
        "hg_norm_w": 1.0 + nrm(ks[6], (L, HG_DV), 0.02),
        "sg_ln_g": 1.0 + nrm(ks[7], (L, SG_WIDTH), 0.02),
        "sg_ln_b": nrm(ks[8], (L, SG_WIDTH), 0.02),
        "sg_w": nrm(ks[9], (L, SG_GROUPS, SG_CHUNK, SG_CHUNK), SG_CHUNK ** -0.5),
        "sg_b": 1.0 + nrm(ks[10], (L, SG_GROUPS, SG_CHUNK), 0.02),
        "w_out": nrm(ks[11], (L, D_MIX, D), BETA * D_MIX ** -0.5),
        "ln1_g": 1.0 + nrm(ks[12], (L, D), 0.02),
        "ln1_b": nrm(ks[13], (L, D), 0.02),
        "router_group_w": nrm(ks[14], (L, D, MOE_GROUPS), D ** -0.5),
        "router_group_b": nrm(ks[15], (L, MOE_GROUPS), 0.01),
        "router_expert_w": nrm(ks[16], (L, MOE_GROUPS, D, MOE_EXPERTS_PER_GROUP), D ** -0.5),
        "router_expert_b": nrm(ks[17], (L, MOE_GROUPS, MOE_EXPERTS_PER_GROUP), 0.01),
        "w_up": nrm(ks[18], (L, MOE_EXPERTS, D, 2 * MOE_HIDDEN), D ** -0.5),
        "w_down": nrm(ks[19], (L, MOE_EXPERTS, MOE_HIDDEN, D), BETA * MOE_HIDDEN ** -0.5),
        "ln2_g": 1.0 + nrm(ks[20], (L, D), 0.02),
        "ln2_b": nrm(ks[21], (L, D), 0.02),
    }


def reference(x, c, w_ada, b_ada, w_in, lb_logits, hg_norm_w, sg_ln_g, sg_ln_b, sg_w, sg_b,
              w_out, ln1_g, ln1_b, router_group_w, router_group_b, router_expert_w,
              router_expert_b, w_up, w_down, ln2_g, ln2_b):
    lower_bounds = jnp.cumsum(jax.nn.softmax(lb_logits.astype(jnp.float32), axis=0), axis=0)
    c_act = jax.nn.silu(c)
    for l in range(DEPTH):
        ada = c_act @ w_ada[l] + b_ada[l]
        sh1, sc1, g1, sh2, sc2, g2 = jnp.split(ada, 6, axis=-1)
        h = _modulate(x, sh1, sc1)
        proj = h @ w_in[l]
        q, fz, iv, og, z = jnp.split(proj, SPLIT_POINTS, axis=-1)
        y_a = _hgrn2_mixer(q, fz, iv, og, lower_bounds[l], hg_norm_w[l])
        y_b = _spatial_gating_mixer(z, sg_ln_g[l], sg_ln_b[l], sg_w[l], sg_b[l])
        y = jnp.concatenate([y_a, y_b], axis=-1) @ w_out[l]
        x = _layernorm(ALPHA * x + (1.0 + g1[:, None, :]) * y, ln1_g[l], ln1_b[l])
        h2 = _modulate(x, sh2, sc2)
        m = _hier_moe(h2, router_group_w[l], router_group_b[l], router_expert_w[l],
                      router_expert_b[l], w_up[l], w_down[l])
        x = _layernorm(ALPHA * x + (1.0 + g2[:, None, :]) * m, ln2_g[l], ln2_b[l])
    return x
```

```python
import numpy as np
import concourse.bass as bass
import concourse.mybir as mybir
from concourse.bass_utils import run_bass_kernel_spmd

F32 = mybir.dt.float32
BF16 = mybir.dt.bfloat16
I32 = mybir.dt.int32
U32 = mybir.dt.uint32
AF = mybir.ActivationFunctionType
ALU = mybir.AluOpType
AX = mybir.AxisListType

P = 128
D = 1024
NT = 16
NPREV = 48
NE = 32
CAP = 384
NSB = CAP // 128
HID = 512
ALPHA = 2.0 ** 0.25
LN_EPS = 1e-5
RMS_EPS = 1e-6
SAME_ENG_SYNC = True
_DBG = False


class Buf:
    __slots__ = ("name", "writer", "readers")

    def __init__(self, name=""):
        self.name = name
        self.writer = None
        self.readers = []


class Tl:
    __slots__ = ("ap", "b")

    def __init__(self, ap, name=""):
        self.ap = ap
        self.b = Buf(name)


def _b(x):
    return x.b if isinstance(x, Tl) else x


class Sched:
    def __init__(self, nc, n_dma_sems=24, n_sw_sems=64):
        self.nc = nc
        self.n_sw_sems = n_sw_sems
        self.sw_sems = []
        self.sw_next = 0
        self.engs = ["pe", "act", "dve", "pool", "sp"]
        self.prog = {e: [] for e in self.engs}
        self.count = {e: 0 for e in self.engs}
        self.sem = {}
        self.known = {e: {} for e in self.engs}
        self.n_dma_sems = n_dma_sems
        self.dma_sems = []
        self.dma_sem_val = []
        self.dma_rr = 0
        self._cms = []

    def open(self):
        for e in ["pe", "act", "dve", "pool"]:
            cm = self.nc.semaphore("s_" + e)
            self.sem[e] = cm.__enter__()
            self._cms.append(cm)
        for i in range(self.n_dma_sems):
            cm = self.nc.semaphore("s_dma%d" % i)
            self.dma_sems.append(cm.__enter__())
            self.dma_sem_val.append(0)
            self._cms.append(cm)
        for i in range(self.n_sw_sems):
            cm = self.nc.semaphore("s_sw%d" % i)
            self.sw_sems.append(cm.__enter__())
            self._cms.append(cm)

    def close(self):
        for cm in reversed(self._cms):
            cm.__exit__(None, None, None)

    def _semh(self, key):
        if isinstance(key, tuple):
            return self.sw_sems[key[1]] if key[0] == "sw" else self.dma_sems[key[1]]
        return self.sem[key]

    def _collect(self, eng, reads, writes):
        need = {}

        def add(ev, raw):
            if ev is None:
                return
            k, v = ev
            if k == eng:
                if eng == "pe" or not SAME_ENG_SYNC or not raw:
                    return
            if self.known[eng].get(k, 0) >= v:
                return
            if need.get(k, 0) < v:
                need[k] = v
        for b in reads:
            add(_b(b).writer, True)
        for b in writes:
            bb = _b(b)
            add(bb.writer, False)
            for r in bb.readers:
                add(r, False)
        return need

    def _emit_waits(self, eng, need):
        waits = []
        for k, v in need.items():
            self.known[eng][k] = v
            waits.append((self._semh(k), v))
        return waits

    def op(self, eng, fn, reads=(), writes=()):
        need = self._collect(eng, reads, writes)
        waits = self._emit_waits(eng, need)
        self.count[eng] += 1
        idx = self.count[eng]
        sem = self.sem[eng]

        def thunk(h, waits=waits, fn=fn, sem=sem):
            for s, v in waits:
                h.wait_ge(s, v)
            fn(h).then_inc(sem, 1)
        self.prog[eng].append(thunk)
        ev = (eng, idx)
        for b in reads:
            _b(b).readers.append(ev)
        for b in writes:
            bb = _b(b)
            bb.writer = ev
            bb.readers = []
        return ev

    def dma(self, q, fn, reads=(), writes=()):
        if q == "pool":
            return self._dma_sw(fn, reads, writes)
        need = self._collect(q, reads, writes)
        i = self.dma_rr
        self.dma_rr = (self.dma_rr + 1) % self.n_dma_sems
        key = ("dma", i)
        prev = self.dma_sem_val[i]
        if prev > 0 and self.known[q].get(key, 0) < prev:
            need[key] = max(need.get(key, 0), prev)
        waits = self._emit_waits(q, need)
        val = prev + 16
        self.dma_sem_val[i] = val
        sem = self.dma_sems[i]

        def thunk(h, waits=waits, fn=fn, sem=sem):
            for s, v in waits:
                h.wait_ge(s, v)
            fn(h).then_inc(sem, 16)
        self.prog[q].append(thunk)
        ev = (key, val)
        for b in reads:
            _b(b).readers.append(ev)
        for b in writes:
            bb = _b(b)
            bb.writer = ev
            bb.readers = []
        return ev

    def _dma_sw(self, fn, reads, writes):
        q = "pool"
        need = self._collect(q, reads, writes)
        waits = self._emit_waits(q, need)
        i = self.sw_next
        self.sw_next += 1
        assert i < self.n_sw_sems, "out of software-DMA semaphores"
        sem = self.sw_sems[i]

        def thunk(h, waits=waits, fn=fn, sem=sem):
            for s, v in waits:
                h.wait_ge(s, v)
            fn(h).then_inc(sem, 16)
        self.prog[q].append(thunk)
        ev = (("sw", i), 16)
        for b in reads:
            _b(b).readers.append(ev)
        for b in writes:
            bb = _b(b)
            bb.writer = ev
            bb.readers = []
        return ev

    def barrier(self, engs=None):
        for e in (engs or self.engs):
            need = {}
            for o in ["pe", "act", "dve", "pool"]:
                if o != e and self.count[o] > self.known[e].get(o, 0):
                    need[o] = self.count[o]
            for i in range(self.n_dma_sems):
                key = ("dma", i)
                v = self.dma_sem_val[i]
                if v > self.known[e].get(key, 0):
                    need[key] = v
            for i in range(self.sw_next):
                key = ("sw", i)
                if self.known[e].get(key, 0) < 16:
                    need[key] = 16
            waits = self._emit_waits(e, need)

            def thunk(h, waits=waits):
                for s, v in waits:
                    h.wait_ge(s, v)
            self.prog[e].append(thunk)

    def run(self):
        nc = self.nc
        with nc.Block() as block:
            @block.tensor
            def _(h):
                for t in self.prog["pe"]:
                    t(h)

            @block.scalar
            def _(h):
                for t in self.prog["act"]:
                    t(h)

            @block.vector
            def _(h):
                for t in self.prog["dve"]:
                    t(h)

            @block.gpsimd
            def _(h):
                for t in self.prog["pool"]:
                    t(h)

            @block.sync
            def _(h):
                for t in self.prog["sp"]:
                    t(h)


class Arena:
    def __init__(self, ap_f32, nbytes):
        self.ap = ap_f32
        self.nbytes = nbytes
        self.off = 0
        self.peak = 0

    def alloc(self, n, dt, name=""):
        sz = {F32: 4, BF16: 2, I32: 4, U32: 4}[dt] * n
        sz = (sz + 31) // 32 * 32
        assert self.off + sz <= self.nbytes, ("arena overflow", name, self.off, sz, self.nbytes)
        v = self.ap[:, self.off // 4:(self.off + sz) // 4]
        if dt != F32:
            v = v.bitcast(dt)
        self.off += sz
        self.peak = max(self.peak, self.off)
        return Tl(v[:, 0:n], name)

    def mark(self):
        return self.off

    def release(self, m):
        self.off = m


def build_nc(dbg=False, stop=None):
    nc = bass.Bass("TRN2", target_bir_lowering=False)

    def din(name, shape, dt=F32):
        return nc.dram_tensor(name, shape, dt, kind="ExternalInput").ap()

    x_own = din("x_own", [NT * P, D])
    x_prev = din("x_prev", [NPREV * P, D])
    valid_in = din("valid", [P, NPREV])
    c_row = din("c_row", [1, D])
    w_ada = din("w_ada", [D, 6 * D])
    b_ada = din("b_ada", [1, 6 * D])
    w_in = din("w_in", [D, 3072])
    lbT = din("lbT", [P, 8])
    gnw_in = din("gnw", [P, 1])
    sg_ln_g = din("sg_ln_g", [1, 512])
    sg_ln_b = din("sg_ln_b", [1, 512])
    sg_wT = din("sg_wT", [P, 4, P])
    sg_b = din("sg_b", [1, 512])
    w_out = din("w_out", [D, D])
    ln1_g = din("ln1_g", [1, D])
    ln1_b = din("ln1_b", [1, D])
    rw = din("rw", [D, 36])
    rb = din("rb", [1, 36])
    w_up = din("w_up", [NE, D, 2 * HID])
    w_down = din("w_down", [NE, HID, D])
    ln2_g = din("ln2_g", [1, D])
    ln2_b = din("ln2_b", [1, D])
    out = nc.dram_tensor("out", [NT * P, D], F32, kind="ExternalOutput").ap()

    sk = "ExternalOutput" if dbg else "Internal"
    xpad = nc.dram_tensor("xpad", [NE * CAP, D], BF16, kind="Internal").ap()
    ypad = nc.dram_tensor("ypad", [NE * CAP, D], BF16, kind="Internal").ap()
    if dbg:
        dbg_ypad = nc.dram_tensor("dbg_ypad", [NE * CAP, D], BF16, kind="ExternalOutput").ap()
        dbg_xpad = nc.dram_tensor("dbg_xpad", [NE * CAP, D], BF16, kind="ExternalOutput").ap()
        dbg_wu = nc.dram_tensor("dbg_wu", [P, 8 * 1024], BF16, kind="ExternalOutput").ap()
    x1scr = nc.dram_tensor("x1scr", [NT * P, D], F32, kind=sk).ap()
    if dbg:
        dbg_S = nc.dram_tensor("dbg_S", [P, 512], F32, kind="ExternalOutput").ap()
        dbg_misc = nc.dram_tensor("dbg_misc", [P, 4 * NT + NE], F32, kind="ExternalOutput").ap()
        dbg_par = nc.dram_tensor("dbg_par", [P, 5 * D], F32, kind="ExternalOutput").ap()

    S = Sched(nc)
    S.open()
    ARENA_BYTES = 211968
    arena_cm = nc.sbuf_tensor("arena", [P, ARENA_BYTES // 4], F32)
    arena_t = arena_cm.__enter__()
    A = Arena(arena_t[:], ARENA_BYTES)
    bank_cms = [nc.psum_tensor("bank%d" % i, [P, 512], F32) for i in range(8)]
    bk = [Tl(cm.__enter__()[:], "bank%d" % i) for i, cm in enumerate(bank_cms)]

    def act(out_, in_, func, reads, writes, bias=None, scale=None):
        kw = {}
        if bias is not None:
            kw["bias"] = bias
        if scale is not None:
            kw["scale"] = scale
        S.op("act", lambda h: h.activation(out=out_, in_=in_, func=func, **kw), reads, writes)

    def tt(eng, out_, in0, in1, op, reads, writes):
        S.op(eng, lambda h: h.tensor_tensor(out=out_, in0=in0, in1=in1, op=op), reads, writes)

    def ts(eng, out_, in0, s1, s2, op0, op1, reads, writes):
        if op1 is None:
            S.op(eng, lambda h: h.tensor_scalar(out=out_, in0=in0, scalar1=s1, scalar2=None, op0=op0), reads, writes)
        else:
            S.op(eng, lambda h: h.tensor_scalar(out=out_, in0=in0, scalar1=s1, scalar2=s2, op0=op0, op1=op1), reads, writes)

    def stt(out_, in0, scalar, in1, op0, op1, reads, writes):
        S.op("dve", lambda h: h.scalar_tensor_tensor(out=out_, in0=in0, scalar=scalar, in1=in1, op0=op0, op1=op1),
             reads, writes)

    def cp(eng, out_, in_, reads, writes):
        if eng == "act":
            S.op("act", lambda h: h.copy(out=out_, in_=in_), reads, writes)
        else:
            S.op(eng, lambda h: h.tensor_copy(out=out_, in_=in_), reads, writes)

    def mm(out_, lhsT, rhs, start, stop, reads, writes):
        S.op("pe", lambda h: h.matmul(out_, lhsT=lhsT, rhs=rhs, start=start, stop=stop), reads, writes)

    def tr(out_, in_, ident, reads, writes):
        S.op("pe", lambda h: h.transpose(out=out_, in_=in_, identity=ident), reads, writes)

    def dma(q, out_, in_, reads, writes):
        S.dma(q, lambda h: h.dma_start(out=out_, in_=in_), reads, writes)

    def red(out_, in_, op, reads, writes):
        S.op("dve", lambda h: h.tensor_reduce(out=out_, in_=in_, axis=AX.X, op=op), reads, writes)

    def v4(ap):
        return ap.rearrange("p (h t) -> p h t", h=4)

    def v8(ap):
        return ap.rearrange("p (k t) -> p k t", k=8)

    identf = A.alloc(P, F32, "identf")
    identb = A.alloc(P, BF16, "identb")
    mask_f = A.alloc(P, F32, "mask_f")
    ustrict = A.alloc(P, F32, "ustrict")
    ones_f = A.alloc(P, F32, "ones_f")
    ones_b = A.alloc(P, BF16, "ones_b")
    io_pj = A.alloc(P, F32, "io_pj")
    ebase = A.alloc(NE, F32, "ebase")
    eidx = A.alloc(NE, F32, "eidx")
    consts = A.alloc(8, F32, "consts")
    slot0 = A.alloc(NT, I32, "slot0")
    slot1 = A.alloc(NT, I32, "slot1")
    wgt0 = A.alloc(NT, F32, "wgt0")
    wgt1 = A.alloc(NT, F32, "wgt1")
    cntbase = A.alloc(NE, F32, "cntbase")
    ln2g_b = A.alloc(D, F32, "ln2g_b")
    ln2b_b = A.alloc(D, F32, "ln2b_b")
    g2p_b = A.alloc(D, F32, "g2p_b")

    S.op("pool", lambda h: h.iota(io_pj.ap, pattern=[[1, P]], base=0, channel_multiplier=-1,
                                  allow_small_or_imprecise_dtypes=True), [], [io_pj])
    S.op("dve", lambda h: h.tensor_single_scalar(out=identf.ap, in_=io_pj.ap, scalar=0.0, op=ALU.is_equal), [io_pj], [identf])
    S.op("dve", lambda h: h.tensor_single_scalar(out=mask_f.ap, in_=io_pj.ap, scalar=0.0, op=ALU.is_ge), [io_pj], [mask_f])
    S.op("dve", lambda h: h.tensor_single_scalar(out=ustrict.ap, in_=io_pj.ap, scalar=0.0, op=ALU.is_gt), [io_pj], [ustrict])
    cp("dve", identb.ap, identf.ap, [identf], [identb])
    S.op("pool", lambda h: h.memset(ones_f.ap, 1.0), [], [ones_f])
    S.op("pool", lambda h: h.memset(ones_b.ap, 1.0), [], [ones_b])
    S.op("pool", lambda h: h.iota(ebase.ap, pattern=[[CAP, NE]], base=0, channel_multiplier=0,
                                  allow_small_or_imprecise_dtypes=True), [], [ebase])
    S.op("pool", lambda h: h.iota(eidx.ap, pattern=[[1, NE]], base=0, channel_multiplier=0,
                                  allow_small_or_imprecise_dtypes=True), [], [eidx])
    S.op("pool", lambda h: h.memset(cntbase.ap, 0.0), [], [cntbase])
    for j, val in enumerate([1.0, 4 * LN_EPS, LN_EPS, RMS_EPS, 0.0]):
        S.op("pool", (lambda v, jj: (lambda h: h.memset(consts.ap[:, jj:jj + 1], v)))(val, j), [], [consts])
    C_ONE = consts.ap[:, 0:1]
    C_4EPS = consts.ap[:, 1:2]
    C_EPS = consts.ap[:, 2:3]
    C_REPS = consts.ap[:, 3:4]
    C_ZERO = consts.ap[:, 4:5]
    for b_ in bk:
        S.op("dve", (lambda bb: (lambda h: h.memset(bb.ap, 0.0)))(b_), [], [b_])
    dma("sp", ln2g_b.ap, ln2_g.broadcast_to([P, D]), [], [ln2g_b])
    dma("sp", ln2b_b.ap, ln2_b.broadcast_to([P, D]), [], [ln2b_b])

    m_persist = A.mark()
    w_in_b = A.alloc(8 * 3072, BF16, "w_in_b")
    w_out_b = A.alloc(8 * D, BF16, "w_out_b")
    shr1_b = A.alloc(D, F32, "shr1_b")
    g1p_b = A.alloc(D, F32, "g1p_b")
    sc2p_b = A.alloc(D, F32, "sc2p_b")
    sh2_b = A.alloc(D, F32, "sh2_b")
    ln1g_b = A.alloc(D, F32, "ln1g_b")
    ln1b_b = A.alloc(D, F32, "ln1b_b")
    lng_b = A.alloc(512, F32, "lng_b")
    lnb_b = A.alloc(512, F32, "lnb_b")
    WcT = A.alloc(512, BF16, "WcT")
    sgb_b = A.alloc(512, F32, "sgb_b")
    RW = A.alloc(8 * 36, F32, "RW")
    rb_b = A.alloc(36, F32, "rb_b")
    oml = A.alloc(4, F32, "oml")
    noml = A.alloc(4, F32, "noml")
    gnw = A.alloc(1, F32, "gnw")
    Sst = A.alloc(512, F32, "Sst")
    Sbf = A.alloc(512, BF16, "Sbf")
    sc1p = A.alloc(8, F32, "sc1p")
    validt = A.alloc(NPREV, F32, "validt")
    w_in_v = w_in_b.ap.rearrange("p (k n) -> p k n", k=8)
    w_out_v = w_out_b.ap.rearrange("p (k n) -> p k n", k=8)
    RW_v = RW.ap.rearrange("p (k n) -> p k n", k=8)

    zt = A.alloc(D, BF16, "zt")
    m_work = A.mark()
    c_b = A.alloc(D, F32, "c_b")
    sgc = A.alloc(D, F32, "sgc")
    cact = A.alloc(D, F32, "cact")
    CT = A.alloc(D, F32, "CT")
    wst = [A.alloc(8 * 512, F32, "wst%d" % i) for i in range(2)]
    bst = [A.alloc(512, F32, "bst%d" % i) for i in range(2)]
    sec = [A.alloc(D, F32, "sec%d" % i) for i in range(2)]
    lbt = A.alloc(8, F32, "lbt")
    wct_st = A.alloc(512, F32, "wct_st")
    wist = [A.alloc(1536, F32, "wist%d" % i) for i in range(2)]

    dma("sp", c_b.ap, c_row.broadcast_to([P, D]), [], [c_b])
    dma("sp", lbt.ap, lbT, [], [lbt])
    dma("sp", gnw.ap, gnw_in, [], [gnw])
    dma("sp", validt.ap, valid_in, [], [validt])
    act(sgc.ap, c_b.ap, AF.Sigmoid, [c_b], [sgc])
    tt("dve", cact.ap, c_b.ap, sgc.ap, ALU.mult, [c_b, sgc], [cact])
    tt("dve", oml.ap, lbt.ap[:, 4:8], lbt.ap[:, 0:4], ALU.subtract, [lbt], [oml])
    act(oml.ap, oml.ap, AF.Sigmoid, [oml], [oml])
    ts("dve", noml.ap, oml.ap, -1.0, None, ALU.mult, None, [oml], [noml])
    for half in range(2):
        for k4 in range(4):
            kc = half * 4 + k4
            tr(v4(bk[half].ap)[:, k4, :], cact.ap[:, kc * P:(kc + 1) * P], identf.ap, [cact, identf], [bk[half]])
        cp("act", CT.ap[:, half * 512:(half + 1) * 512], bk[half].ap, [bk[half]], [CT])
    CT_v = v8(CT.ap)
    w_ada_v = w_ada.rearrange("(k p) n -> p k n", p=P)

    def ada_section(sidx, dst, plus_one):
        for hh in range(2):
            j = sidx * 2 + hh
            w = wst[j % 2]
            bsl = bst[j % 2]
            dma("sp", w.ap.rearrange("p (k n) -> p k n", k=8), w_ada_v[:, :, j * 512:(j + 1) * 512], [], [w])
            dma("sp", bsl.ap, b_ada[:, j * 512:(j + 1) * 512].broadcast_to([P, 512]), [], [bsl])
            pb = bk[2 + (j % 2)]
            wv = w.ap.rearrange("p (k n) -> p k n", k=8)
            for kc in range(8):
                mm(pb.ap, CT_v[:, kc, :], wv[:, kc, :], kc == 0, kc == 7, [CT, w], [pb])
            tt("dve", dst.ap[:, hh * 512:(hh + 1) * 512], pb.ap, bsl.ap, ALU.add, [pb, bsl], [dst])
        if plus_one:
            ts("dve", dst.ap, dst.ap, 1.0, None, ALU.add, None, [dst], [dst])

    ada_section(0, sec[0], False)
    ada_section(1, sec[1], True)
    S.op("dve", lambda h: h.reciprocal(out=shr1_b.ap, in_=sec[1].ap), [sec[1]], [shr1_b])
    tt("dve", shr1_b.ap, shr1_b.ap, sec[0].ap, ALU.mult, [shr1_b, sec[0]], [shr1_b])
    for half in range(2):
        for k4 in range(4):
            kc = half * 4 + k4
            tr(v4(bk[half].ap)[:, k4, :], sec[1].ap[:, kc * P:(kc + 1) * P], identf.ap, [sec[1], identf], [bk[half]])
        cp("dve", sc1p.ap[:, half * 4:(half + 1) * 4], v4(bk[half].ap)[:, :, 0], [bk[half]], [sc1p])
    for kc in range(8):
        for hh in range(2):
            st_ = wist[(kc * 2 + hh) % 2]
            dma("sp", st_.ap, w_in[kc * P:(kc + 1) * P, hh * 1536:(hh + 1) * 1536], [], [st_])
            act(w_in_v[:, kc, hh * 1536:(hh + 1) * 1536], st_.ap, AF.Identity, [st_, sc1p], [w_in_b],
                scale=sc1p.ap[:, kc:kc + 1])
    for kc in range(8):
        st_ = wist[kc % 2]
        dma("sp", st_.ap[:, 0:D], w_out[kc * P:(kc + 1) * P, :], [], [st_])
        cp("act", w_out_v[:, kc, :], st_.ap[:, 0:D], [st_], [w_out_b])
    dma("sp", wct_st.ap.rearrange("p (g t) -> p g t", g=4), sg_wT, [], [wct_st])
    tt("dve", v4(WcT.ap), v4(wct_st.ap), mask_f.ap.unsqueeze(1).to_broadcast([P, 4, P]), ALU.mult,
       [wct_st, mask_f], [WcT])
    dma("sp", sgb_b.ap, sg_b.broadcast_to([P, 512]), [], [sgb_b])
    dma("sp", lng_b.ap, sg_ln_g.broadcast_to([P, 512]), [], [lng_b])
    dma("sp", lnb_b.ap, sg_ln_b.broadcast_to([P, 512]), [], [lnb_b])
    dma("sp", ln1g_b.ap, ln1_g.broadcast_to([P, D]), [], [ln1g_b])
    dma("sp", ln1b_b.ap, ln1_b.broadcast_to([P, D]), [], [ln1b_b])
    dma("sp", RW_v, rw.rearrange("(k p) n -> p k n", p=P), [], [RW])
    dma("sp", rb_b.ap, rb.broadcast_to([P, 36]), [], [rb_b])
    S.op("pool", lambda h: h.memset(Sst.ap, 0.0), [], [Sst])

    ada_section(2, g1p_b, True)
    ada_section(3, sh2_b, False)
    ada_section(4, sc2p_b, True)
    ada_section(5, g2p_b, True)

    S.op("pool", lambda h: h.memset(zt.ap, 0.0), [], [zt])
    xpad_b = Buf("xpad")

    S.barrier()
    A.release(m_work)

    xin = [A.alloc(D, F32, "xin%d" % i) for i in range(4)]
    xb = A.alloc(D, BF16, "xb")
    xT = A.alloc(D, BF16, "xT")
    tmpA = A.alloc(512, F32, "tmpA")
    tmpB = A.alloc(512, F32, "tmpB")
    tmpC = A.alloc(512, F32, "tmpC")
    sg_ = [A.alloc(512, F32, "sg%d" % i) for i in range(2)]
    vtm_ = [A.alloc(512, BF16, "v_tm%d" % i) for i in range(3)]
    qs_ = [A.alloc(512, BF16, "qs%d" % i) for i in range(2)]
    gs_ = [A.alloc(512, BF16, "gs%d" % i) for i in range(3)]
    tu_ = [A.alloc(512, BF16, "tu%d" % i) for i in range(2)]
    gv_ = [A.alloc(512, F32, "gv%d" % i) for i in range(2)]
    yaT_ = [A.alloc(512, BF16, "yaT%d" % i) for i in range(2)]
    ybT_ = [A.alloc(512, BF16, "ybT%d" % i) for i in range(3)]
    logf = A.alloc(512, F32, "logf")
    cum = A.alloc(512, F32, "cum")
    ncol = A.alloc(16, F32, "ncol")
    ekl = A.alloc(512, F32, "ekl")
    khT = A.alloc(512, BF16, "khT")
    kh_ = [A.alloc(512, BF16, "kh%d" % i) for i in range(2)]
    dec_ = [A.alloc(4, F32, "dec%d" % i) for i in range(2)]
    vn = A.alloc(512, BF16, "vn")
    stM = (A.alloc(12, F32, "bst6M"), A.alloc(2, F32, "mvM"), A.alloc(4, F32, "rsM"))
    stT = (A.alloc(12, F32, "bst6T"), A.alloc(2, F32, "mvT"), A.alloc(4, F32, "rsT"))
    eq = [A.alloc(32, F32, "eq%d" % i) for i in range(4)]
    ek = [A.alloc(P, F32, "ek%d" % i) for i in range(4)]
    qt = A.alloc(512, BF16, "qt")
    kt = [A.alloc(P, BF16, "kt%d" % i) for i in range(4)]
    scT_ = [A.alloc(512, BF16, "scT%d" % i) for i in range(2)]
    qh_ = [A.alloc(512, BF16, "qh%d" % i) for i in range(2)]
    osq = A.alloc(512, BF16, "osq")
    rstd = A.alloc(512, F32, "rstd")
    rr = A.alloc(D, F32, "rr")
    x1 = A.alloc(D, F32, "x1")
    h2 = rr
    h2b = [A.alloc(D, BF16, "h2b%d" % i) for i in range(2)]
    h2T = A.alloc(D, F32, "h2T")
    lg_ = [A.alloc(36, F32, "lg%d" % i) for i in range(2)]
    sm = A.alloc(64, F32, "sm")
    ml = A.alloc(NE, F32, "ml")
    mx8 = A.alloc(8, F32, "mx8")
    ix8 = A.alloc(8, U32, "ix8")
    A0 = A.alloc(NE, F32, "A0")
    A1 = A.alloc(NE, F32, "A1")
    AA = A.alloc(NE, F32, "AA")
    Cc = A.alloc(NE, F32, "Cc")
    x1_bufs = [Buf("x1scr%d" % i) for i in range(NT)]
    print("arena after phase1 alloc:", A.off, "of", A.nbytes)

    bT, bA, bB, bC, bD, bE, bF, bG = bk
    bT_b8 = v8(bT.ap.bitcast(BF16))
    bD_b4 = v4(bD.ap.bitcast(BF16)[:, 0:512])
    bC4 = v4(bC.ap)
    bD4 = v4(bD.ap)
    bE4 = v4(bE.ap)
    S4 = v4(Sst.ap)
    Sb4 = v4(Sbf.ap)
    xT_v = v8(xT.ap)
    logf4, cum4, ekl4, khT4 = (v4(t.ap) for t in (logf, cum, ekl, khT))
    mask3 = mask_f.ap.unsqueeze(1).to_broadcast([P, 4, P])

    _steps = {}

    def interleave(*gens, names=None):
        items = [[g, n, 0, 0.0] for g, n in zip(gens, names or [None] * len(gens)) if g is not None]
        lens = [_steps.get(it[1]) for it in items]
        prop = all(l is not None for l in lens) and len(items) > 1
        mx = max(lens) if prop else 1
        while items:
            for it in list(items):
                if prop:
                    it[3] += _steps[it[1]] / mx
                    k = int(it[3])
                    it[3] -= k
                else:
                    k = 1
                for _ in range(k):
                    try:
                        next(it[0])
                        it[2] += 1
                    except StopIteration:
                        if it[1] is not None:
                            _steps[it[1]] = max(it[2], 1)
                        items.remove(it)
                        break

    def load_xT(src_rows, xi):
        dma("sp", xi.ap, src_rows, [], [xi])
        tt("pool", xb.ap, xi.ap, shr1_b.ap, ALU.add, [xi, shr1_b], [xb])
        yield
        for kc in range(8):
            tr(bT_b8[:, kc, :], xb.ap[:, kc * P:(kc + 1) * P], identb.ap, [xb, identb], [bT])
        cp("act", xT.ap, bT.ap.bitcast(BF16), [bT], [xT])
        yield

    def proj_fm(bank, col0):
        b4 = v4(bank.ap)
        for hb in range(4):
            for kc in range(8):
                mm(b4[:, hb, :], w_in_v[:, kc, col0 + hb * P:col0 + (hb + 1) * P], xT_v[:, kc, :],
                   kc == 0, kc == 7, [w_in_b, xT], [bank])
            yield

    def proj_tm(bank, col0):
        for kc in range(8):
            mm(bank.ap, xT_v[:, kc, :], w_in_v[:, kc, col0:col0 + 512], kc == 0, kc == 7, [xT, w_in_b], [bank])
            if kc % 2 == 1:
                yield

    def gate_state(sg, valid_col, kh, dec):
        sg4 = v4(sg.ap)
        tt("dve", logf4, sg4, oml.ap.unsqueeze(2).to_broadcast([P, 4, P]), ALU.mult, [sg, oml], [logf])
        act(logf.ap, logf.ap, AF.Ln, [logf], [logf], bias=C_ONE, scale=-1.0)
        yield
        for hd in range(4):
            S.op("dve", (lambda hd_: (lambda h: h.tensor_tensor_scan(
                out=cum4[:, hd_, :], data0=ones_f.ap, data1=logf4[:, hd_, :], initial=0.0,
                op0=ALU.mult, op1=ALU.add)))(hd), [logf, ones_f], [cum])
        yield
        tt("dve", ekl4, cum4[:, :, P - 1:P].to_broadcast([P, 4, P]), cum4, ALU.subtract, [cum], [ekl])
        act(ekl.ap, ekl.ap, AF.Exp, [ekl], [ekl])
        act(dec.ap, cum4[:, :, P - 1], AF.Exp, [cum], [dec])
        yield
        if valid_col is None:
            tt("dve", khT.ap, ekl.ap, sg.ap, ALU.mult, [ekl, sg], [khT])
        else:
            stt(khT.ap, ekl.ap, valid_col, sg.ap, ALU.mult, ALU.mult, [ekl, sg, validt], [khT])
        yield
        for hd in range(4):
            tr(bD_b4[:, hd, :], khT4[:, hd, :], identb.ap, [khT, identb], [bD])
        cp("act", kh.ap, bD.ap.bitcast(BF16)[:, 0:512], [bD], [kh])
        yield

    def state_update(v_tm, kh, dec):
        kh4 = v4(kh.ap)
        for hd in range(4):
            mm(bC4[:, hd, :], kh4[:, hd, :], v_tm.ap[:, hd * P:(hd + 1) * P], True, True, [kh, v_tm], [bC])
        yield
        for hd in range(4):
            stt(S4[:, hd, :], S4[:, hd, :], dec.ap[:, hd:hd + 1], bC4[:, hd, :], ALU.mult, ALU.add,
                [Sst, dec, bC], [Sst])
        yield

    def layernorm_tm(src, width, eps_ap, g_b, b_b, dst, stats):
        st6_, mv_, r_ = stats
        nch = width // 512
        for c in range(nch):
            S.op("dve", (lambda c_: (lambda h: h.bn_stats(out=st6_.ap[:, c_ * 6:(c_ + 1) * 6],
                                                          in_=src.ap[:, c_ * 512:(c_ + 1) * 512])))(c), [src], [st6_])
        S.op("dve", lambda h: h.bn_aggr(out=mv_.ap, in_=st6_.ap[:, 0:6 * nch]), [st6_], [mv_])
        yield
        act(r_.ap[:, 0:1], mv_.ap[:, 1:2], AF.Ln, [mv_], [r_], bias=eps_ap)
        act(r_.ap[:, 0:1], r_.ap[:, 0:1], AF.Exp, [r_], [r_], scale=-0.5)
        yield
        stt(r_.ap[:, 1:2], mv_.ap[:, 0:1], -1.0, r_.ap[:, 0:1], ALU.mult, ALU.mult, [mv_, r_], [r_])
        act(src.ap, src.ap, AF.Identity, [src, r_], [src], bias=r_.ap[:, 1:2], scale=r_.ap[:, 0:1])
        yield
        tt("pool", src.ap, src.ap, g_b.ap, ALU.mult, [src, g_b], [src])
        yield
        tt("dve", dst.ap, src.ap, b_b.ap, ALU.add, [src, b_b], [dst])
        yield

    def PF(j):
        s = j % 2
        yield from load_xT(x_prev[j * P:(j + 1) * P, :], xin[j % 4])
        yield from proj_fm(bA, 512)
        act(tmpA.ap, bA.ap, AF.Exp, [bA], [tmpA])
        act(tmpA.ap, tmpA.ap, AF.Ln, [tmpA], [tmpA], bias=C_ONE)
        act(sg_[s].ap, tmpA.ap, AF.Exp, [tmpA], [sg_[s]], scale=-1.0)
        yield
        yield from proj_tm(bB, 1024)
        cp("act", vtm_[s].ap, bB.ap, [bB], [vtm_[s]])
        yield

    def PM(j):
        s = j % 2
        yield from gate_state(sg_[s], validt.ap[:, j:j + 1], kh_[s], dec_[s])
        yield from state_update(vtm_[s], kh_[s], dec_[s])

    def zero_gen():
        for r in range(NE * CAP // P):
            dma("sp", xpad[r * P:(r + 1) * P, :], zt.ap, [zt], [Buf()])
            yield

    zg = zero_gen()
    for j in range(NPREV + 1):
        interleave(PF(j) if j < NPREV else None, PM(j - 1) if j >= 1 else None, names=["PF", "PM"])
        for _ in range(2):
            next(zg, None)
    for _ in zg:
        pass
    cp("act", Sbf.ap, Sst.ap, [Sst], [Sbf])
    if dbg:
        dma("sp", dbg_S, Sst.ap, [Sst], [Buf()])
        for jj, t_ in enumerate([shr1_b, g1p_b, sc2p_b, sh2_b, g2p_b]):
            dma("sp", dbg_par[:, jj * D:(jj + 1) * D], t_.ap, [t_], [Buf()])
    S.barrier()

    def FE(i):
        s = i % 2
        yield from load_xT(x_own[i * P:(i + 1) * P, :], xin[i % 4])
        yield from proj_fm(bA, 2048)
        act(tu_[s].ap, bA.ap, AF.Gelu, [bA], [tu_[s]])
        yield from proj_tm(bB, 2560)
        act(gv_[s].ap, bB.ap, AF.Gelu, [bB], [gv_[s]])
        yield

    def sigmoid_exp(dst, bank, sign):
        act(dst.ap, bank.ap, AF.Exp, [bank], [dst], scale=-sign)
        act(dst.ap, dst.ap, AF.Ln, [dst], [dst], bias=C_ONE)
        act(dst.ap, dst.ap, AF.Exp, [dst], [dst], scale=-1.0)

    def FX(i):
        s = i % 2
        yield from proj_fm(bA, 0)
        sigmoid_exp(tmpA, bA, 1.0)
        yield
        tt("dve", qs_[s].ap, bA.ap, tmpA.ap, ALU.mult, [bA, tmpA], [qs_[s]])
        yield from proj_fm(bB, 1536)
        sigmoid_exp(tmpB, bB, 1.0)
        yield
        tt("dve", gs_[i % 3].ap, bB.ap, tmpB.ap, ALU.mult, [bB, tmpB], [gs_[i % 3]])
        yield from proj_fm(bA, 512)
        sigmoid_exp(sg_[s], bA, -1.0)
        yield
        yield from proj_tm(bB, 1024)
        cp("act", vtm_[i % 3].ap, bB.ap, [bB], [vtm_[i % 3]])
        yield

    def M1(i):
        s = i % 2
        tu, gv, ybT = tu_[s], gv_[s], ybT_[i % 3]
        bT4 = v4(bT.ap)
        yield from layernorm_tm(gv, 512, C_EPS, lng_b, lnb_b, vn, stM)
        for g in range(4):
            mm(bT4[:, g, :], vn.ap[:, g * P:(g + 1) * P], v4(WcT.ap)[:, g, :], True, True, [vn, WcT], [bT])
        tt("dve", tmpC.ap, bT.ap, sgb_b.ap, ALU.add, [bT, sgb_b], [tmpC])
        yield
        tt("dve", ybT.ap, tu.ap, tmpC.ap, ALU.mult, [tu, tmpC], [ybT])
        yield

    def M2a(i):
        s = i % 2
        sg, qs, kh, dec, scT, qh = sg_[s], qs_[s], kh_[s], dec_[s], scT_[s], qh_[s]
        sg4, qs4, qt4, scT4, qh4 = (v4(t.ap) for t in (sg, qs, qt, scT, qh))
        yield from gate_state(sg, None, kh, dec)
        ts("dve", ncol.ap.rearrange("p (h i) -> p h i", h=4)[:, :, 1:4],
           cum.ap.rearrange("p (h b j) -> p h b j", h=4, b=4)[:, :, 0:3, 31], -1.0, None, ALU.mult, None, [cum], [ncol])
        yield
        n = 0
        for hd in range(4):
            for I in range(4):
                e_q = eq[n % 4]
                e_k = ek[n % 4]
                k_t = kt[n % 4]
                n += 1
                M_ = 32 * (I + 1)
                bq = C_ZERO if I == 0 else ncol.ap[:, hd * 4 + I:hd * 4 + I + 1]
                bkk = C_ZERO if I == 0 else cum4[:, hd, 32 * I - 1:32 * I]
                act(e_q.ap, cum4[:, hd, 32 * I:32 * I + 32], AF.Exp, [cum, ncol], [e_q], bias=bq)
                act(e_k.ap[:, 0:M_], cum4[:, hd, 0:M_], AF.Exp, [cum], [e_k], bias=bkk, scale=-1.0)
                stt(qt4[:, hd, 32 * I:32 * I + 32], e_q.ap, oml.ap[:, hd:hd + 1], qs4[:, hd, 32 * I:32 * I + 32],
                    ALU.mult, ALU.mult, [e_q, oml, qs], [qt])
                tt("pool", k_t.ap[:, 0:M_], e_k.ap[:, 0:M_], sg4[:, hd, 0:M_], ALU.mult, [e_k, sg], [k_t])
                mm(bD4[0:M_, hd, 32 * I:32 * I + 32], k_t.ap[:, 0:M_], qt4[:, hd, 32 * I:32 * I + 32], True, True,
                   [k_t, qt], [bD])
                yield
        tt("dve", scT4, bD4, mask3, ALU.mult, [bD, mask_f], [scT])
        act(ekl.ap, cum.ap, AF.Exp, [cum], [ekl])
        yield
        for hd in range(4):
            stt(qh4[:, hd, :], ekl4[:, hd, :], oml.ap[:, hd:hd + 1], qs4[:, hd, :], ALU.mult, ALU.mult,
                [ekl, oml, qs], [qh])
        yield

    def M2b(i):
        s = i % 2
        v_tm, gs, kh, dec, scT, qh, yaT = vtm_[i % 3], gs_[i % 3], kh_[s], dec_[s], scT_[s], qh_[s], yaT_[s]
        scT4, qh4 = v4(scT.ap), v4(qh.ap)
        for hd in range(4):
            mm(bE4[:, hd, :], v_tm.ap[:, hd * P:(hd + 1) * P], scT4[:, hd, :], True, False, [v_tm, scT], [bE])
            mm(bE4[:, hd, :], Sb4[:, hd, :], qh4[:, hd, :], False, True, [Sbf, qh], [bE])
        yield
        yield from state_update(v_tm, kh, dec)
        cp("act", Sbf.ap, Sst.ap, [Sst], [Sbf])
        act(osq.ap, bE.ap, AF.Square, [bE], [osq])
        yield
        mm(bC.ap, ones_b.ap, osq.ap, True, True, [ones_b, osq], [bC])
        yield
        act(rstd.ap, bC.ap, AF.Ln, [bC], [rstd], bias=C_REPS, scale=1.0 / 128.0)
        yield
        act(rstd.ap, rstd.ap, AF.Exp, [rstd], [rstd], scale=-0.5)
        yield
        stt(rstd.ap, bE.ap, gnw.ap[:, 0:1], rstd.ap, ALU.mult, ALU.mult, [bE, gnw, rstd], [rstd])
        yield
        tt("pool", yaT.ap, rstd.ap, gs.ap, ALU.mult, [rstd, gs], [yaT])
        yield

    def T1(i):
        lg = lg_[i % 2]
        s = i % 2
        xi = xin[i % 4]
        yaT4, ybT4 = v4(yaT_[s].ap), v4(ybT_[i % 3].ap)
        for half, pb in ((0, bF), (1, bG)):
            for k in range(8):
                lhs = yaT4[:, k, :] if k < 4 else ybT4[:, k - 4, :]
                mm(pb.ap, lhs, w_out_v[:, k, half * 512:(half + 1) * 512], k == 0, k == 7,
                   [yaT_[s], ybT_[i % 3], w_out_b], [pb])
                if k % 4 == 3:
                    yield
        for half, pb in ((0, bF), (1, bG)):
            tt("dve", rr.ap[:, half * 512:(half + 1) * 512], pb.ap, g1p_b.ap[:, half * 512:(half + 1) * 512],
               ALU.mult, [pb, g1p_b], [rr])
            yield
        stt(rr.ap, xi.ap, ALPHA, rr.ap, ALU.mult, ALU.add, [xi, rr], [rr])
        yield
        yield from layernorm_tm(rr, D, C_EPS, ln1g_b, ln1b_b, x1, stT)
        dma("sp", x1scr[i * P:(i + 1) * P, :], x1.ap, [x1], [x1_bufs[i]])
        hb_ = h2b[i % 2]
        tt("pool", h2.ap, x1.ap, sc2p_b.ap, ALU.mult, [x1, sc2p_b], [h2])
        yield
        tt("dve", h2.ap, h2.ap, sh2_b.ap, ALU.add, [h2, sh2_b], [h2])
        cp("act", hb_.ap, h2.ap, [h2], [hb_])
        yield
        for half, pb in ((0, bF), (1, bG)):
            for k4 in range(4):
                kc = half * 4 + k4
                tr(v4(pb.ap)[:, k4, :], h2.ap[:, kc * P:(kc + 1) * P], identf.ap, [h2, identf], [pb])
            cp("act", h2T.ap[:, half * 512:(half + 1) * 512], pb.ap, [pb], [h2T])
            yield
        h2T_v = v8(h2T.ap)
        for kc in range(8):
            mm(bF.ap[:, 0:36], h2T_v[:, kc, :], RW_v[:, kc, :], kc == 0, kc == 7, [h2T, RW], [bF])
        yield
        tt("dve", lg.ap, bF.ap[:, 0:36], rb_b.ap, ALU.add, [bF, rb_b], [lg])
        yield

    def T2(i):
        lg = lg_[i % 2]
        hb_ = h2b[i % 2]
        red(sm.ap[:, 0:1], lg.ap[:, 0:4], ALU.max, [lg], [sm])
        ts("dve", sm.ap[:, 1:2], sm.ap[:, 0:1], -1.0, None, ALU.mult, None, [sm], [sm])
        yield
        ts("dve", sm.ap[:, 4:8], lg.ap[:, 0:4], sm.ap[:, 0:1], None, ALU.is_equal, None, [lg, sm], [sm])
        act(sm.ap[:, 12:16], lg.ap[:, 0:4], AF.Exp, [lg, sm], [sm], bias=sm.ap[:, 1:2])
        yield
        red(sm.ap[:, 2:3], sm.ap[:, 12:16], ALU.add, [sm], [sm])
        S.op("dve", lambda h: h.reciprocal(out=sm.ap[:, 3:4], in_=sm.ap[:, 2:3]), [sm], [sm])
        yield
        ts("dve", sm.ap[:, 8:12], sm.ap[:, 4:8], 1.0, 1e30, ALU.subtract, ALU.mult, [sm], [sm])
        for g in range(4):
            ts("dve", ml.ap[:, g * 8:(g + 1) * 8], lg.ap[:, 4 + g * 8:4 + (g + 1) * 8], sm.ap[:, 8 + g:9 + g], None,
               ALU.add, None, [lg, sm], [ml])
        yield
        S.op("dve", lambda h: h.max(out=mx8.ap, in_=ml.ap), [ml], [mx8])
        S.op("dve", lambda h: h.max_index(out=ix8.ap, in_max=mx8.ap, in_values=ml.ap), [ml, mx8], [ix8])
        yield
        tt("dve", sm.ap[:, 16:17], mx8.ap[:, 1:2], mx8.ap[:, 0:1], ALU.subtract, [mx8], [sm])
        act(sm.ap[:, 17:18], sm.ap[:, 16:17], AF.Exp, [sm], [sm])
        yield
        ts("dve", sm.ap[:, 18:19], sm.ap[:, 17:18], 1.0, None, ALU.add, None, [sm], [sm])
        S.op("dve", lambda h: h.reciprocal(out=sm.ap[:, 19:20], in_=sm.ap[:, 18:19]), [sm], [sm])
        yield
        tt("dve", wgt0.ap[:, i:i + 1], sm.ap[:, 3:4], sm.ap[:, 19:20], ALU.mult, [sm], [wgt0])
        tt("dve", wgt1.ap[:, i:i + 1], wgt0.ap[:, i:i + 1], sm.ap[:, 17:18], ALU.mult, [sm, wgt0], [wgt1])
        yield
        cp("dve", sm.ap[:, 20:22], ix8.ap[:, 0:2], [ix8], [sm])
        ts("dve", A0.ap, eidx.ap, sm.ap[:, 20:21], None, ALU.is_equal, None, [eidx, sm], [A0])
        ts("dve", A1.ap, eidx.ap, sm.ap[:, 21:22], None, ALU.is_equal, None, [eidx, sm], [A1])
        tt("dve", AA.ap, A0.ap, A1.ap, ALU.add, [A0, A1], [AA])
        yield
        mm(bT.ap[:, 0:32], ustrict.ap, AA.ap, True, True, [ustrict, AA], [bT])
        mm(bT.ap[:, 64:96], ones_f.ap, AA.ap, True, True, [ones_f, AA], [bT])
        tt("dve", Cc.ap, bT.ap[:, 0:32], cntbase.ap, ALU.add, [bT, cntbase], [Cc])
        tt("dve", cntbase.ap, cntbase.ap, bT.ap[:, 64:96], ALU.add, [cntbase, bT], [cntbase])
        yield
        ts("dve", Cc.ap, Cc.ap, float(CAP - 1), None, ALU.min, None, [Cc], [Cc])
        tt("dve", Cc.ap, Cc.ap, ebase.ap, ALU.add, [Cc, ebase], [Cc])
        yield
        tt("dve", A0.ap, A0.ap, Cc.ap, ALU.mult, [A0, Cc], [A0])
        tt("dve", A1.ap, A1.ap, Cc.ap, ALU.mult, [A1, Cc], [A1])
        red(sm.ap[:, 22:23], A0.ap, ALU.add, [A0], [sm])
        red(sm.ap[:, 23:24], A1.ap, ALU.add, [A1], [sm])
        yield
        ts("dve", sm.ap[:, 22:24], sm.ap[:, 22:24], 0.0, float(NE * CAP - 1), ALU.max, ALU.min, [sm], [sm])
        cp("dve", slot0.ap[:, i:i + 1], sm.ap[:, 22:23], [sm], [slot0])
        cp("dve", slot1.ap[:, i:i + 1], sm.ap[:, 23:24], [sm], [slot1])
        yield
        for sl in (slot0, slot1):
            S.dma("pool", (lambda sl_, hb__, ii: (lambda h: h.indirect_dma_start(
                out=xpad, out_offset=bass.IndirectOffsetOnAxis(ap=sl_.ap[:, ii:ii + 1], axis=0),
                in_=hb__.ap, in_offset=None)))(sl, hb_, i), [hb_, sl], [Buf()])
        yield

    def stage(g, k):
        return g(k) if 0 <= k < NT else None

    for t in range(NT + 5):
        if t < NT:
            interleave(FE(t))
        interleave(stage(M2a, t - 1), stage(FX, t), stage(T1, t - 3), stage(M2b, t - 2), stage(M1, t - 1),
                   stage(T2, t - 4), names=["M2a", "FX", "T1", "M2b", "M1", "T2"])

    if dbg:
        dbgm = A.alloc(4 * NT + NE, F32, "dbgm")
        cp("dve", dbgm.ap[:, 0:NT], slot0.ap, [slot0], [dbgm])
        cp("dve", dbgm.ap[:, NT:2 * NT], slot1.ap, [slot1], [dbgm])
        cp("dve", dbgm.ap[:, 2 * NT:3 * NT], wgt0.ap, [wgt0], [dbgm])
        cp("dve", dbgm.ap[:, 3 * NT:4 * NT], wgt1.ap, [wgt1], [dbgm])
        cp("dve", dbgm.ap[:, 4 * NT:4 * NT + NE], cntbase.ap, [cntbase], [dbgm])
        dma("sp", dbg_misc, dbgm.ap, [dbgm], [Buf()])
    S.barrier()
    A.release(m_persist)
    if stop == "p1":
        S.run()
        for cm in reversed(bank_cms):
            cm.__exit__(None, None, None)
        arena_cm.__exit__(None, None, None)
        S.close()
        return nc

    wu = [A.alloc(8 * 1024, BF16, "wu%d" % i) for i in range(3)]
    wd = [A.alloc(4 * 1024, BF16, "wd%d" % i) for i in range(3)]
    Xe = [[A.alloc(D, BF16, "Xe%d_%d" % (i, s)) for s in range(NSB)] for i in range(2)]
    XeT = [A.alloc(8 * CAP, BF16, "XeT%d" % i) for i in range(2)]
    sgt = [A.alloc(CAP, F32, "sgt%d" % i) for i in range(2)]
    gt_ = [A.alloc(CAP, F32, "gt%d" % i) for i in range(2)]
    actT = [A.alloc(4 * CAP, BF16, "actT%d" % i) for i in range(2)]
    Yt = [A.alloc(D, BF16, "Yt%d" % i) for i in range(6)]
    ypad_b = Buf("ypad")
    print("arena after phase2 alloc:", A.off, "of", A.nbytes)
    w_up_v = w_up.rearrange("e (k p) n -> e p k n", p=P)
    w_down_v = w_down.rearrange("e (k p) n -> e p k n", p=P)
    stg = [A.alloc(2 * 1024, F32, "stg%d" % i) for i in range(8)]
    print("arena after phase2 alloc (+staging):", A.off, "of", A.nbytes)
    stg_n = [0]

    def wload(e):
        bw = e % 3
        wuv = wu[bw].ap.rearrange("p (k n) -> p k n", k=8)
        wdv = wd[bw].ap.rearrange("p (k n) -> p k n", k=4)
        chunks = [(wuv[:, 2 * k:2 * k + 2, :], w_up_v[e, :, 2 * k:2 * k + 2, :], wu[bw]) for k in range(4)] + \
                 [(wdv[:, 2 * k:2 * k + 2, :], w_down_v[e, :, 2 * k:2 * k + 2, :], wd[bw]) for k in range(2)]
        pend = []
        for ci, (dst, src, dtl) in enumerate(chunks):
            st = stg[stg_n[0] % 8]
            stg_n[0] += 1
            dma("sp", st.ap.rearrange("p (k n) -> p k n", k=2), src, [], [st])
            pend.append((dst, st, dtl, ci))
            yield
            if len(pend) > 4:
                d_, s_, t_, c_ = pend.pop(0)
                cp("act" if c_ % 2 == 0 else "dve", d_, s_.ap.rearrange("p (k n) -> p k n", k=2), [s_], [t_])
                yield
        for d_, s_, t_, c_ in pend:
            cp("act" if c_ % 2 == 0 else "dve", d_, s_.ap.rearrange("p (k n) -> p k n", k=2), [s_], [t_])
            yield

    yn = [0]

    def compute(e):
        bu = e % 2
        bw = e % 3
        wuv = wu[bw].ap.rearrange("p (k n) -> p k n", k=8)
        wdv = wd[bw].ap.rearrange("p (k n) -> p k n", k=4)
        xet = XeT[bu]
        xetv = xet.ap.rearrange("p (k s) -> p k s", k=8)
        for sb_ in range(NSB):
            xe = Xe[bu][sb_]
            dma("act", xe.ap, xpad[e * CAP + sb_ * P:e * CAP + (sb_ + 1) * P, :], [xpad_b], [xe])
            pb = bk[sb_ % 2]
            pb8 = v8(pb.ap.bitcast(BF16))
            for kc in range(8):
                tr(pb8[:, kc, :], xe.ap[:, kc * P:(kc + 1) * P], identb.ap, [xe, identb], [pb])
            cp("act" if sb_ % 2 == 0 else "dve", xetv[:, :, sb_ * P:(sb_ + 1) * P], pb8, [pb], [xet])
            yield
        at = actT[bu]
        atv = at.ap.rearrange("p (k s) -> p k s", k=4)
        for cb in range(4):
            pg = bk[2 + (cb % 2)]
            pu = bk[4 + (cb % 2)]
            for kc in range(8):
                mm(pg.ap[:, 0:CAP], wuv[:, kc, cb * P:(cb + 1) * P], xetv[:, kc, :], kc == 0, kc == 7, [wu[bw], xet], [pg])
            yield
            for kc in range(8):
                mm(pu.ap[:, 0:CAP], wuv[:, kc, 512 + cb * P:512 + (cb + 1) * P], xetv[:, kc, :], kc == 0, kc == 7,
                   [wu[bw], xet], [pu])
            yield
            s_ = sgt[cb % 2]
            g_ = gt_[cb % 2]
            act(s_.ap, pg.ap[:, 0:CAP], AF.Sigmoid, [pg], [s_])
            tt("dve", g_.ap, pg.ap[:, 0:CAP], s_.ap, ALU.mult, [pg, s_], [g_])
            tt("dve", atv[:, cb, :], pu.ap[:, 0:CAP], g_.ap, ALU.mult, [pu, g_], [at])
            yield
        for sb_ in range(NSB):
            y = Yt[yn[0] % 6]
            yn[0] += 1
            for half in range(2):
                pb = bk[6 + half]
                for hc in range(4):
                    mm(pb.ap, atv[:, hc, sb_ * P:(sb_ + 1) * P], wdv[:, hc, half * 512:(half + 1) * 512],
                       hc == 0, hc == 3, [at, wd[bw]], [pb])
                cp("act" if half == 0 else "dve", y.ap[:, half * 512:(half + 1) * 512], pb.ap, [pb], [y])
                yield
            dma("act", ypad[e * CAP + sb_ * P:e * CAP + (sb_ + 1) * P, :], y.ap, [y], [Buf()])

    interleave(wload(0))
    for e in range(NE):
        interleave(compute(e), wload(e + 1) if e + 1 < NE else None, names=["cmp", "wl"])

    if dbg:
        for r in range(NE * CAP // P):
            dma("sp", dbg_ypad[r * P:(r + 1) * P, :], ypad[r * P:(r + 1) * P, :], [ypad_b], [Buf()])
            dma("sp", dbg_xpad[r * P:(r + 1) * P, :], xpad[r * P:(r + 1) * P, :], [xpad_b], [Buf()])
        dma("sp", dbg_wu, wu[1].ap, [wu[1]], [Buf()])
    S.barrier()
    A.release(m_persist)

    NW3 = 4
    Y0 = [A.alloc(D, BF16, "Y0_%d" % i) for i in range(NW3)]
    Y1 = [A.alloc(D, BF16, "Y1_%d" % i) for i in range(NW3)]
    x1t = [A.alloc(D, F32, "x1t%d" % i) for i in range(NW3)]
    mt = [A.alloc(D, F32, "mt%d" % i) for i in range(NW3)]
    ot = [A.alloc(D, F32, "ot%d" % i) for i in range(NW3)]
    st3 = [(A.alloc(12, F32, "bst6_3%d" % i), A.alloc(2, F32, "mv_3%d" % i), A.alloc(4, F32, "rs_3%d" % i))
           for i in range(NW3)]

    def P3(i):
        p2 = i % NW3
        for sl, yt in ((slot0, Y0[p2]), (slot1, Y1[p2])):
            S.dma("pool", (lambda sl_, yt_, ii: (lambda h: h.indirect_dma_start(
                out=yt_.ap, out_offset=None, in_=ypad,
                in_offset=bass.IndirectOffsetOnAxis(ap=sl_.ap[:, ii:ii + 1], axis=0))))(sl, yt, i), [sl], [yt])
        dma("sp", x1t[p2].ap, x1scr[i * P:(i + 1) * P, :], [x1_bufs[i]], [x1t[p2]])
        yield
        m = mt[p2]
        act(m.ap, Y0[p2].ap, AF.Identity, [Y0[p2], wgt0], [m], scale=wgt0.ap[:, i:i + 1])
        yield
        stt(m.ap, Y1[p2].ap, wgt1.ap[:, i:i + 1], m.ap, ALU.mult, ALU.add, [Y1[p2], wgt1, m], [m])
        yield
        tt("pool", m.ap, m.ap, g2p_b.ap, ALU.mult, [m, g2p_b], [m])
        yield
        stt(m.ap, x1t[p2].ap, ALPHA, m.ap, ALU.mult, ALU.add, [x1t[p2], m], [m])
        yield
        yield from layernorm_tm(m, D, C_EPS, ln2g_b, ln2b_b, ot[p2], st3[p2])
        dma("sp", out[i * P:(i + 1) * P, :], ot[p2].ap, [ot[p2]], [Buf()])
        yield

    for i in range(0, NT, NW3):
        interleave(*[P3(i + w) for w in range(NW3)])
    S.barrier(["sp"])
    S.run()
    for cm in reversed(bank_cms):
        cm.__exit__(None, None, None)
    arena_cm.__exit__(None, None, None)
    S.close()
    return nc


def kernel(x, c, w_ada, b_ada, w_in, lb_logits, hg_norm_w, sg_ln_g, sg_ln_b, sg_w, sg_b,
           w_out, ln1_g, ln1_b, router_group_w, router_group_b, router_expert_w,
           router_expert_b, w_up, w_down, ln2_g, ln2_b):
    f = lambda a: np.ascontiguousarray(np.asarray(a, dtype=np.float32))
    x = f(x); c = f(c)
    B, SEQ, _ = x.shape
    QT = SEQ // 4
    rw = np.concatenate([f(router_group_w)[0], f(router_expert_w)[0].transpose(1, 0, 2).reshape(D, 32)], axis=1)
    rb = np.concatenate([f(router_group_b)[0], f(router_expert_b)[0].reshape(32)])[None, :]
    shared = {
        "w_ada": f(w_ada)[0], "b_ada": f(b_ada), "w_in": f(w_in)[0],
        "lbT": f(np.asarray(lb_logits).reshape(2, 4, P).transpose(2, 0, 1).reshape(P, 8)),
        "gnw": f(np.asarray(hg_norm_w).reshape(P, 1)),
        "sg_ln_g": f(sg_ln_g), "sg_ln_b": f(sg_ln_b),
        "sg_wT": f(np.asarray(sg_w)[0].transpose(2, 0, 1)),
        "sg_b": f(np.asarray(sg_b).reshape(1, 512)),
        "w_out": f(w_out)[0], "ln1_g": f(ln1_g), "ln1_b": f(ln1_b),
        "rw": f(rw), "rb": f(rb), "w_up": f(w_up)[0], "w_down": f(w_down)[0],
        "ln2_g": f(ln2_g), "ln2_b": f(ln2_b),
    }
    in_maps = []
    for core in range(8):
        b, q = core // 4, core % 4
        xo = x[b, q * QT:(q + 1) * QT, :]
        xp = np.zeros((NPREV * P, D), np.float32)
        val = np.zeros((P, NPREV), np.float32)
        npv = q * QT
        if npv:
            xp[NPREV * P - npv:, :] = x[b, :npv, :]
            val[:, NPREV - npv // P:] = 1.0
        d = dict(shared)
        d.update({"x_own": np.ascontiguousarray(xo), "x_prev": xp, "valid": val,
                  "c_row": np.ascontiguousarray(c[b:b + 1, :])})
        in_maps.append(d)
    if _DBG:
        return in_maps
    nc = build_nc()
    res = run_bass_kernel_spmd(nc, in_maps, core_ids=list(range(8)))
    outp = np.zeros((B, SEQ, D), np.float32)
    for core in range(8):
        b, q = core // 4, core % 4
        outp[b, q * QT:(q + 1) * QT, :] = np.asarray(res.results[core]["out"], dtype=np.float32)
    return outp
```

```python
import numpy as np
import concourse.bass as bass
import concourse.mybir as mybir
from concourse.bass_utils import run_bass_kernel_spmd

F32 = mybir.dt.float32
BF16 = mybir.dt.bfloat16
I32 = mybir.dt.int32
U32 = mybir.dt.uint32
AF = mybir.ActivationFunctionType
ALU = mybir.AluOpType
AX = mybir.AxisListType

P = 128
D = 1024
NT = 16
NPREV = 48
NE = 32
CAP = 384
NSB = CAP // 128
HID = 512
ALPHA = 2.0 ** 0.25
LN_EPS = 1e-5
RMS_EPS = 1e-6
SAME_ENG_SYNC = True
_DBG = False


class Buf:
    __slots__ = ("name", "writer", "readers")

    def __init__(self, name=""):
        self.name = name
        self.writer = None
        self.readers = []


class Tl:
    __slots__ = ("ap", "b")

    def __init__(self, ap, name=""):
        self.ap = ap
        self.b = Buf(name)


def _b(x):
    return x.b if isinstance(x, Tl) else x


class Sched:
    def __init__(self, nc, n_dma_sems=24, n_sw_sems=64):
        self.nc = nc
        self.n_sw_sems = n_sw_sems
        self.sw_sems = []
        self.sw_next = 0
        self.engs = ["pe", "act", "dve", "pool", "sp"]
        self.prog = {e: [] for e in self.engs}
        self.count = {e: 0 for e in self.engs}
        self.sem = {}
        self.known = {e: {} for e in self.engs}
        self.n_dma_sems = n_dma_sems
        self.dma_sems = []
        self.dma_sem_val = []
        self.dma_rr = 0
        self._cms = []

    def open(self):
        for e in ["pe", "act", "dve", "pool"]:
            cm = self.nc.semaphore("s_" + e)
            self.sem[e] = cm.__enter__()
            self._cms.append(cm)
        for i in range(self.n_dma_sems):
            cm = self.nc.semaphore("s_dma%d" % i)
            self.dma_sems.append(cm.__enter__())
            self.dma_sem_val.append(0)
            self._cms.append(cm)
        for i in range(self.n_sw_sems):
            cm = self.nc.semaphore("s_sw%d" % i)
            self.sw_sems.append(cm.__enter__())
            self._cms.append(cm)

    def close(self):
        for cm in reversed(self._cms):
            cm.__exit__(None, None, None)

    def _semh(self, key):
        if isinstance(key, tuple):
            return self.sw_sems[key[1]] if key[0] == "sw" else self.dma_sems[key[1]]
        return self.sem[key]

    def _collect(self, eng, reads, writes):
        need = {}

        def add(ev, raw):
            if ev is None:
                return
            k, v = ev
            if k == eng:
                if eng == "pe" or not SAME_ENG_SYNC or not raw:
                    return
            if self.known[eng].get(k, 0) >= v:
                return
            if need.get(k, 0) < v:
                need[k] = v
        for b in reads:
            add(_b(b).writer, True)
        for b in writes:
            bb = _b(b)
            add(bb.writer, False)
            for r in bb.readers:
                add(r, False)
        return need

    def _emit_waits(self, eng, need):
        waits = []
        for k, v in need.items():
            self.known[eng][k] = v
            waits.append((self._semh(k), v))
        return waits

    def op(self, eng, fn, reads=(), writes=()):
        need = self._collect(eng, reads, writes)
        waits = self._emit_waits(eng, need)
        self.count[eng] += 1
        idx = self.count[eng]
        sem = self.sem[eng]

        def thunk(h, waits=waits, fn=fn, sem=sem):
            for s, v in waits:
                h.wait_ge(s, v)
            fn(h).then_inc(sem, 1)
        self.prog[eng].append(thunk)
        ev = (eng, idx)
        for b in reads:
            _b(b).readers.append(ev)
        for b in writes:
            bb = _b(b)
            bb.writer = ev
            bb.readers = []
        return ev

    def dma(self, q, fn, reads=(), writes=()):
        if q == "pool":
            return self._dma_sw(fn, reads, writes)
        need = self._collect(q, reads, writes)
        i = self.dma_rr
        self.dma_rr = (self.dma_rr + 1) % self.n_dma_sems
        key = ("dma", i)
        prev = self.dma_sem_val[i]
        if prev > 0 and self.known[q].get(key, 0) < prev:
            need[key] = max(need.get(key, 0), prev)
        waits = self._emit_waits(q, need)
        val = prev + 16
        self.dma_sem_val[i] = val
        sem = self.dma_sems[i]

        def thunk(h, waits=waits, fn=fn, sem=sem):
            for s, v in waits:
                h.wait_ge(s, v)
            fn(h).then_inc(sem, 16)
        self.prog[q].append(thunk)
        ev = (key, val)
        for b in reads:
            _b(b).readers.append(ev)
        for b in writes:
            bb = _b(b)
            bb.writer = ev
            bb.readers = []
        return ev

    def _dma_sw(self, fn, reads, writes):
        q = "pool"
        need = self._collect(q, reads, writes)
        waits = self._emit_waits(q, need)
        i = self.sw_next
        self.sw_next += 1
        assert i < self.n_sw_sems, "out of software-DMA semaphores"
        sem = self.sw_sems[i]

        def thunk(h, waits=waits, fn=fn, sem=sem):
            for s, v in waits:
                h.wait_ge(s, v)
            fn(h).then_inc(sem, 16)
        self.prog[q].append(thunk)
        ev = (("sw", i), 16)
        for b in reads:
            _b(b).readers.append(ev)
        for b in writes:
            bb = _b(b)
            bb.writer = ev
            bb.readers = []
        return ev

    def barrier(self, engs=None):
        for e in (engs or self.engs):
            need = {}
            for o in ["pe", "act", "dve", "pool"]:
                if o != e and self.count[o] > self.known[e].get(o, 0):
                    need[o] = self.count[o]
            for i in range(self.n_dma_sems):
                key = ("dma", i)
                v = self.dma_sem_val[i]
                if v > self.known[e].get(key, 0):
                    need[key] = v
            for i in range(self.sw_next):
                key = ("sw", i)
                if self.known[e].get(key, 0) < 16:
                    need[key] = 16
            waits = self._emit_waits(e, need)

            def thunk(h, waits=waits):
                for s, v in waits:
                    h.wait_ge(s, v)
            self.prog[e].append(thunk)

    def run(self):
        nc = self.nc
        with nc.Block() as block:
            @block.tensor
            def _(h):
                for t in self.prog["pe"]:
                    t(h)

            @block.scalar
            def _(h):
                for t in self.prog["act"]:
                    t(h)

            @block.vector
            def _(h):
                for t in self.prog["dve"]:
                    t(h)

            @block.gpsimd
            def _(h):
                for t in self.prog["pool"]:
                    t(h)

            @block.sync
            def _(h):
                for t in self.prog["sp"]:
                    t(h)


class Arena:
    def __init__(self, ap_f32, nbytes):
        self.ap = ap_f32
        self.nbytes = nbytes
        self.off = 0
        self.peak = 0

    def alloc(self, n, dt, name=""):
        sz = {F32: 4, BF16: 2, I32: 4, U32: 4}[dt] * n
        sz = (sz + 31) // 32 * 32
        assert self.off + sz <= self.nbytes, ("arena overflow", name, self.off, sz, self.nbytes)
        v = self.ap[:, self.off // 4:(self.off + sz) // 4]
        if dt != F32:
            v = v.bitcast(dt)
        self.off += sz
        self.peak = max(self.peak, self.off)
        return Tl(v[:, 0:n], name)

    def mark(self):
        return self.off

    def release(self, m):
        self.off = m


def build_nc(dbg=False, stop=None):
    nc = bass.Bass("TRN2", target_bir_lowering=False)

    def din(name, shape, dt=F32):
        return nc.dram_tensor(name, shape, dt, kind="ExternalInput").ap()

    x_own = din("x_own", [NT * P, D])
    x_prev = din("x_prev", [NPREV * P, D])
    valid_in = din("valid", [P, NPREV])
    c_row = din("c_row", [1, D])
    w_ada = din("w_ada", [D, 6 * D])
    b_ada = din("b_ada", [1, 6 * D])
    w_in = din("w_in", [D, 3072])
    lbT = din("lbT", [P, 8])
    gnw_in = din("gnw", [P, 1])
    sg_ln_g = din("sg_ln_g", [1, 512])
    sg_ln_b = din("sg_ln_b", [1, 512])
    sg_wT = din("sg_wT", [P, 4, P])
    sg_b = din("sg_b", [1, 512])
    w_out = din("w_out", [D, D])
    ln1_g = din("ln1_g", [1, D])
    ln1_b = din("ln1_b", [1, D])
    rw = din("rw", [D, 36])
    rb = din("rb", [1, 36])
    w_up = din("w_up", [NE, D, 2 * HID])
    w_down = din("w_down", [NE, HID, D])
    ln2_g = din("ln2_g", [1, D])
    ln2_b = din("ln2_b", [1, D])
    out = nc.dram_tensor("out", [NT * P, D], F32, kind="ExternalOutput").ap()

    sk = "ExternalOutput" if dbg else "Internal"
    xpad = nc.dram_tensor("xpad", [NE * CAP, D], BF16, kind="Internal").ap()
    ypad = nc.dram_tensor("ypad", [NE * CAP, D], BF16, kind="Internal").ap()
    if dbg:
        dbg_ypad = nc.dram_tensor("dbg_ypad", [NE * CAP, D], BF16, kind="ExternalOutput").ap()
        dbg_xpad = nc.dram_tensor("dbg_xpad", [NE * CAP, D], BF16, kind="ExternalOutput").ap()
        dbg_wu = nc.dram_tensor("dbg_wu", [P, 8 * 1024], BF16, kind="ExternalOutput").ap()
    x1scr = nc.dram_tensor("x1scr", [NT * P, D], F32, kind=sk).ap()
    if dbg:
        dbg_S = nc.dram_tensor("dbg_S", [P, 512], F32, kind="ExternalOutput").ap()
        dbg_misc = nc.dram_tensor("dbg_misc", [P, 4 * NT + NE], F32, kind="ExternalOutput").ap()
        dbg_par = nc.dram_tensor("dbg_par", [P, 5 * D], F32, kind="ExternalOutput").ap()

    S = Sched(nc)
    S.open()
    ARENA_BYTES = 211968
    arena_cm = nc.sbuf_tensor("arena", [P, ARENA_BYTES // 4], F32)
    arena_t = arena_cm.__enter__()
    A = Arena(arena_t[:], ARENA_BYTES)
    bank_cms = [nc.psum_tensor("bank%d" % i, [P, 512], F32) for i in range(8)]
    bk = [Tl(cm.__enter__()[:], "bank%d" % i) for i, cm in enumerate(bank_cms)]

    def act(out_, in_, func, reads, writes, bias=None, scale=None):
        kw = {}
        if bias is not None:
            kw["bias"] = bias
        if scale is not None:
            kw["scale"] = scale
        S.op("act", lambda h: h.activation(out=out_, in_=in_, func=func, **kw), reads, writes)

    def tt(eng, out_, in0, in1, op, reads, writes):
        S.op(eng, lambda h: h.tensor_tensor(out=out_, in0=in0, in1=in1, op=op), reads, writes)

    def ts(eng, out_, in0, s1, s2, op0, op1, reads, writes):
        if op1 is None:
            S.op(eng, lambda h: h.tensor_scalar(out=out_, in0=in0, scalar1=s1, scalar2=None, op0=op0), reads, writes)
        else:
            S.op(eng, lambda h: h.tensor_scalar(out=out_, in0=in0, scalar1=s1, scalar2=s2, op0=op0, op1=op1), reads, writes)

    def stt(out_, in0, scalar, in1, op0, op1, reads, writes):
        S.op("dve", lambda h: h.scalar_tensor_tensor(out=out_, in0=in0, scalar=scalar, in1=in1, op0=op0, op1=op1),
             reads, writes)

    def cp(eng, out_, in_, reads, writes):
        if eng == "act":
            S.op("act", lambda h: h.copy(out=out_, in_=in_), reads, writes)
        else:
            S.op(eng, lambda h: h.tensor_copy(out=out_, in_=in_), reads, writes)

    def mm(out_, lhsT, rhs, start, stop, reads, writes):
        S.op("pe", lambda h: h.matmul(out_, lhsT=lhsT, rhs=rhs, start=start, stop=stop), reads, writes)

    def tr(out_, in_, ident, reads, writes):
        S.op("pe", lambda h: h.transpose(out=out_, in_=in_, identity=ident), reads, writes)

    def dma(q, out_, in_, reads, writes):
        S.dma(q, lambda h: h.dma_start(out=out_, in_=in_), reads, writes)

    def red(out_, in_, op, reads, writes):
        S.op("dve", lambda h: h.tensor_reduce(out=out_, in_=in_, axis=AX.X, op=op), reads, writes)

    def v4(ap):
        return ap.rearrange("p (h t) -> p h t", h=4)

    def v8(ap):
        return ap.rearrange("p (k t) -> p k t", k=8)

    identf = A.alloc(P, F32, "identf")
    identb = A.alloc(P, BF16, "identb")
    mask_f = A.alloc(P, F32, "mask_f")
    ustrict = A.alloc(P, F32, "ustrict")
    ones_f = A.alloc(P, F32, "ones_f")
    ones_b = A.alloc(P, BF16, "ones_b")
    io_pj = A.alloc(P, F32, "io_pj")
    ebase = A.alloc(NE, F32, "ebase")
    eidx = A.alloc(NE, F32, "eidx")
    consts = A.alloc(8, F32, "consts")
    slot0 = A.alloc(NT, I32, "slot0")
    slot1 = A.alloc(NT, I32, "slot1")
    wgt0 = A.alloc(NT, F32, "wgt0")
    wgt1 = A.alloc(NT, F32, "wgt1")
    cntbase = A.alloc(NE, F32, "cntbase")
    ln2g_b = A.alloc(D, F32, "ln2g_b")
    ln2b_b = A.alloc(D, F32, "ln2b_b")
    g2p_b = A.alloc(D, F32, "g2p_b")

    S.op("pool", lambda h: h.iota(io_pj.ap, pattern=[[1, P]], base=0, channel_multiplier=-1,
                                  allow_small_or_imprecise_dtypes=True), [], [io_pj])
    S.op("dve", lambda h: h.tensor_single_scalar(out=identf.ap, in_=io_pj.ap, scalar=0.0, op=ALU.is_equal), [io_pj], [identf])
    S.op("dve", lambda h: h.tensor_single_scalar(out=mask_f.ap, in_=io_pj.ap, scalar=0.0, op=ALU.is_ge), [io_pj], [mask_f])
    S.op("dve", lambda h: h.tensor_single_scalar(out=ustrict.ap, in_=io_pj.ap, scalar=0.0, op=ALU.is_gt), [io_pj], [ustrict])
    cp("dve", identb.ap, identf.ap, [identf], [identb])
    S.op("pool", lambda h: h.memset(ones_f.ap, 1.0), [], [ones_f])
    S.op("pool", lambda h: h.memset(ones_b.ap, 1.0), [], [ones_b])
    S.op("pool", lambda h: h.iota(ebase.ap, pattern=[[CAP, NE]], base=0, channel_multiplier=0,
                                  allow_small_or_imprecise_dtypes=True), [], [ebase])
    S.op("pool", lambda h: h.iota(eidx.ap, pattern=[[1, NE]], base=0, channel_multiplier=0,
                                  allow_small_or_imprecise_dtypes=True), [], [eidx])
    S.op("pool", lambda h: h.memset(cntbase.ap, 0.0), [], [cntbase])
    for j, val in enumerate([1.0, 4 * LN_EPS, LN_EPS, RMS_EPS, 0.0]):
        S.op("pool", (lambda v, jj: (lambda h: h.memset(consts.ap[:, jj:jj + 1], v)))(val, j), [], [consts])
    C_ONE = consts.ap[:, 0:1]
    C_4EPS = consts.ap[:, 1:2]
    C_EPS = consts.ap[:, 2:3]
    C_REPS = consts.ap[:, 3:4]
    C_ZERO = consts.ap[:, 4:5]
    for b_ in bk:
        S.op("dve", (lambda bb: (lambda h: h.memset(bb.ap, 0.0)))(b_), [], [b_])
    dma("sp", ln2g_b.ap, ln2_g.broadcast_to([P, D]), [], [ln2g_b])
    dma("sp", ln2b_b.ap, ln2_b.broadcast_to([P, D]), [], [ln2b_b])

    m_persist = A.mark()
    w_in_b = A.alloc(8 * 3072, BF16, "w_in_b")
    w_out_b = A.alloc(8 * D, BF16, "w_out_b")
    shr1_b = A.alloc(D, F32, "shr1_b")
    g1p_b = A.alloc(D, F32, "g1p_b")
    sc2p_b = A.alloc(D, F32, "sc2p_b")
    sh2_b = A.alloc(D, F32, "sh2_b")
    ln1g_b = A.alloc(D, F32, "ln1g_b")
    ln1b_b = A.alloc(D, F32, "ln1b_b")
    lng_b = A.alloc(512, F32, "lng_b")
    lnb_b = A.alloc(512, F32, "lnb_b")
    WcT = A.alloc(512, BF16, "WcT")
    sgb_b = A.alloc(512, F32, "sgb_b")
    RW = A.alloc(8 * 36, F32, "RW")
    rb_b = A.alloc(36, F32, "rb_b")
    oml = A.alloc(4, F32, "oml")
    noml = A.alloc(4, F32, "noml")
    gnw = A.alloc(1, F32, "gnw")
    Sst = A.alloc(512, F32, "Sst")
    Sbf = A.alloc(512, BF16, "Sbf")
    sc1p = A.alloc(8, F32, "sc1p")
    validt = A.alloc(NPREV, F32, "validt")
    w_in_v = w_in_b.ap.rearrange("p (k n) -> p k n", k=8)
    w_out_v = w_out_b.ap.rearrange("p (k n) -> p k n", k=8)
    RW_v = RW.ap.rearrange("p (k n) -> p k n", k=8)

    zt = A.alloc(D, BF16, "zt")
    m_work = A.mark()
    c_b = A.alloc(D, F32, "c_b")
    sgc = A.alloc(D, F32, "sgc")
    cact = A.alloc(D, F32, "cact")
    CT = A.alloc(D, F32, "CT")
    wst = [A.alloc(8 * 512, F32, "wst%d" % i) for i in range(2)]
    bst = [A.alloc(512, F32, "bst%d" % i) for i in range(2)]
    sec = [A.alloc(D, F32, "sec%d" % i) for i in range(2)]
    lbt = A.alloc(8, F32, "lbt")
    wct_st = A.alloc(512, F32, "wct_st")
    wist = [A.alloc(1536, F32, "wist%d" % i) for i in range(2)]

    dma("sp", c_b.ap, c_row.broadcast_to([P, D]), [], [c_b])
    dma("sp", lbt.ap, lbT, [], [lbt])
    dma("sp", gnw.ap, gnw_in, [], [gnw])
    dma("sp", validt.ap, valid_in, [], [validt])
    act(sgc.ap, c_b.ap, AF.Sigmoid, [c_b], [sgc])
    tt("dve", cact.ap, c_b.ap, sgc.ap, ALU.mult, [c_b, sgc], [cact])
    tt("dve", oml.ap, lbt.ap[:, 4:8], lbt.ap[:, 0:4], ALU.subtract, [lbt], [oml])
    act(oml.ap, oml.ap, AF.Sigmoid, [oml], [oml])
    ts("dve", noml.ap, oml.ap, -1.0, None, ALU.mult, None, [oml], [noml])
    for half in range(2):
        for k4 in range(4):
            kc = half * 4 + k4
            tr(v4(bk[half].ap)[:, k4, :], cact.ap[:, kc * P:(kc + 1) * P], identf.ap, [cact, identf], [bk[half]])
        cp("act", CT.ap[:, half * 512:(half + 1) * 512], bk[half].ap, [bk[half]], [CT])
    CT_v = v8(CT.ap)
    w_ada_v = w_ada.rearrange("(k p) n -> p k n", p=P)

    def ada_section(sidx, dst, plus_one):
        for hh in range(2):
            j = sidx * 2 + hh
            w = wst[j % 2]
            bsl = bst[j % 2]
            dma("sp", w.ap.rearrange("p (k n) -> p k n", k=8), w_ada_v[:, :, j * 512:(j + 1) * 512], [], [w])
            dma("sp", bsl.ap, b_ada[:, j * 512:(j + 1) * 512].broadcast_to([P, 512]), [], [bsl])
            pb = bk[2 + (j % 2)]
            wv = w.ap.rearrange("p (k n) -> p k n", k=8)
            for kc in range(8):
                mm(pb.ap, CT_v[:, kc, :], wv[:, kc, :], kc == 0, kc == 7, [CT, w], [pb])
            tt("dve", dst.ap[:, hh * 512:(hh + 1) * 512], pb.ap, bsl.ap, ALU.add, [pb, bsl], [dst])
        if plus_one:
            ts("dve", dst.ap, dst.ap, 1.0, None, ALU.add, None, [dst], [dst])

    ada_section(0, sec[0], False)
    ada_section(1, sec[1], True)
    S.op("dve", lambda h: h.reciprocal(out=shr1_b.ap, in_=sec[1].ap), [sec[1]], [shr1_b])
    tt("dve", shr1_b.ap, shr1_b.ap, sec[0].ap, ALU.mult, [shr1_b, sec[0]], [shr1_b])
    for half in range(2):
        for k4 in range(4):
            kc = half * 4 + k4
            tr(v4(bk[half].ap)[:, k4, :], sec[1].ap[:, kc * P:(kc + 1) * P], identf.ap, [sec[1], identf], [bk[half]])
        cp("dve", sc1p.ap[:, half * 4:(half + 1) * 4], v4(bk[half].ap)[:, :, 0], [bk[half]], [sc1p])
    for kc in range(8):
        for hh in range(2):
            st_ = wist[(kc * 2 + hh) % 2]
            dma("sp", st_.ap, w_in[kc * P:(kc + 1) * P, hh * 1536:(hh + 1) * 1536], [], [st_])
            act(w_in_v[:, kc, hh * 1536:(hh + 1) * 1536], st_.ap, AF.Identity, [st_, sc1p], [w_in_b],
                scale=sc1p.ap[:, kc:kc + 1])
    for kc in range(8):
        st_ = wist[kc % 2]
        dma("sp", st_.ap[:, 0:D], w_out[kc * P:(kc + 1) * P, :], [], [st_])
        cp("act", w_out_v[:, kc, :], st_.ap[:, 0:D], [st_], [w_out_b])
    dma("sp", wct_st.ap.rearrange("p (g t) -> p g t", g=4), sg_wT, [], [wct_st])
    tt("dve", v4(WcT.ap), v4(wct_st.ap), mask_f.ap.unsqueeze(1).to_broadcast([P, 4, P]), ALU.mult,
       [wct_st, mask_f], [WcT])
    dma("sp", sgb_b.ap, sg_b.broadcast_to([P, 512]), [], [sgb_b])
    dma("sp", lng_b.ap, sg_ln_g.broadcast_to([P, 512]), [], [lng_b])
    dma("sp", lnb_b.ap, sg_ln_b.broadcast_to([P, 512]), [], [lnb_b])
    dma("sp", ln1g_b.ap, ln1_g.broadcast_to([P, D]), [], [ln1g_b])
    dma("sp", ln1b_b.ap, ln1_b.broadcast_to([P, D]), [], [ln1b_b])
    dma("sp", RW_v, rw.rearrange("(k p) n -> p k n", p=P), [], [RW])
    dma("sp", rb_b.ap, rb.broadcast_to([P, 36]), [], [rb_b])
    S.op("pool", lambda h: h.memset(Sst.ap, 0.0), [], [Sst])

    ada_section(2, g1p_b, True)
    ada_section(3, sh2_b, False)
    ada_section(4, sc2p_b, True)
    ada_section(5, g2p_b, True)

    S.op("pool", lambda h: h.memset(zt.ap, 0.0), [], [zt])
    xpad_b = Buf("xpad")

    S.barrier()
    A.release(m_work)

    xin = [A.alloc(D, F32, "xin%d" % i) for i in range(4)]
    xb = A.alloc(D, BF16, "xb")
    xT = A.alloc(D, BF16, "xT")
    tmpA = A.alloc(512, F32, "tmpA")
    tmpB = A.alloc(512, F32, "tmpB")
    tmpC = A.alloc(512, F32, "tmpC")
    sg_ = [A.alloc(512, F32, "sg%d" % i) for i in range(2)]
    vtm_ = [A.alloc(512, BF16, "v_tm%d" % i) for i in range(3)]
    qs_ = [A.alloc(512, BF16, "qs%d" % i) for i in range(2)]
    gs_ = [A.alloc(512, BF16, "gs%d" % i) for i in range(3)]
    tu_ = [A.alloc(512, BF16, "tu%d" % i) for i in range(2)]
    gv_ = [A.alloc(512, F32, "gv%d" % i) for i in range(2)]
    yaT_ = [A.alloc(512, BF16, "yaT%d" % i) for i in range(2)]
    ybT_ = [A.alloc(512, BF16, "ybT%d" % i) for i in range(3)]
    logf = A.alloc(512, F32, "logf")
    cum = A.alloc(512, F32, "cum")
    ncol = A.alloc(16, F32, "ncol")
    ekl = A.alloc(512, F32, "ekl")
    khT = A.alloc(512, BF16, "khT")
    kh_ = [A.alloc(512, BF16, "kh%d" % i) for i in range(2)]
    dec_ = [A.alloc(4, F32, "dec%d" % i) for i in range(2)]
    vn = A.alloc(512, BF16, "vn")
    stM = (A.alloc(12, F32, "bst6M"), A.alloc(2, F32, "mvM"), A.alloc(4, F32, "rsM"))
    stT = (A.alloc(12, F32, "bst6T"), A.alloc(2, F32, "mvT"), A.alloc(4, F32, "rsT"))
    eq = [A.alloc(32, F32, "eq%d" % i) for i in range(4)]
    ek = [A.alloc(P, F32, "ek%d" % i) for i in range(4)]
    qt = A.alloc(512, BF16, "qt")
    kt = [A.alloc(P, BF16, "kt%d" % i) for i in range(4)]
    scT_ = [A.alloc(512, BF16, "scT%d" % i) for i in range(2)]
    qh_ = [A.alloc(512, BF16, "qh%d" % i) for i in range(2)]
    osq = A.alloc(512, BF16, "osq")
    rstd = A.alloc(512, F32, "rstd")
    rr = A.alloc(D, F32, "rr")
    x1 = A.alloc(D, F32, "x1")
    h2 = rr
    h2b = [A.alloc(D, BF16, "h2b%d" % i) for i in range(2)]
    h2T = A.alloc(D, F32, "h2T")
    lg_ = [A.alloc(36, F32, "lg%d" % i) for i in range(2)]
    sm = A.alloc(64, F32, "sm")
    ml = A.alloc(NE, F32, "ml")
    mx8 = A.alloc(8, F32, "mx8")
    ix8 = A.alloc(8, U32, "ix8")
    A0 = A.alloc(NE, F32, "A0")
    A1 = A.alloc(NE, F32, "A1")
    AA = A.alloc(NE, F32, "AA")
    Cc = A.alloc(NE, F32, "Cc")
    x1_bufs = [Buf("x1scr%d" % i) for i in range(NT)]
    print("arena after phase1 alloc:", A.off, "of", A.nbytes)

    bT, bA, bB, bC, bD, bE, bF, bG = bk
    bT_b8 = v8(bT.ap.bitcast(BF16))
    bD_b4 = v4(bD.ap.bitcast(BF16)[:, 0:512])
    bC4 = v4(bC.ap)
    bD4 = v4(bD.ap)
    bE4 = v4(bE.ap)
    S4 = v4(Sst.ap)
    Sb4 = v4(Sbf.ap)
    xT_v = v8(xT.ap)
    logf4, cum4, ekl4, khT4 = (v4(t.ap) for t in (logf, cum, ekl, khT))
    mask3 = mask_f.ap.unsqueeze(1).to_broadcast([P, 4, P])

    _steps = {}

    def interleave(*gens, names=None):
        items = [[g, n, 0, 0.0] for g, n in zip(gens, names or [None] * len(gens)) if g is not None]
        lens = [_steps.get(it[1]) for it in items]
        prop = all(l is not None for l in lens) and len(items) > 1
        mx = max(lens) if prop else 1
        while items:
            for it in list(items):
                if prop:
                    it[3] += _steps[it[1]] / mx
                    k = int(it[3])
                    it[3] -= k
                else:
                    k = 1
                for _ in range(k):
                    try:
                        next(it[0])
                        it[2] += 1
                    except StopIteration:
                        if it[1] is not None:
                            _steps[it[1]] = max(it[2], 1)
                        items.remove(it)
                        break

    def load_xT(src_rows, xi):
        dma("sp", xi.ap, src_rows, [], [xi])
        tt("pool", xb.ap, xi.ap, shr1_b.ap, ALU.add, [xi, shr1_b], [xb])
        yield
        for kc in range(8):
            tr(bT_b8[:, kc, :], xb.ap[:, kc * P:(kc + 1) * P], identb.ap, [xb, identb], [bT])
        cp("act", xT.ap, bT.ap.bitcast(BF16), [bT], [xT])
        yield

    def proj_fm(bank, col0):
        b4 = v4(bank.ap)
        for hb in range(4):
            for kc in range(8):
                mm(b4[:, hb, :], w_in_v[:, kc, col0 + hb * P:col0 + (hb + 1) * P], xT_v[:, kc, :],
                   kc == 0, kc == 7, [w_in_b, xT], [bank])
            yield

    def proj_tm(bank, col0):
        for kc in range(8):
            mm(bank.ap, xT_v[:, kc, :], w_in_v[:, kc, col0:col0 + 512], kc == 0, kc == 7, [xT, w_in_b], [bank])
            if kc % 2 == 1:
                yield

    def gate_state(sg, valid_col, kh, dec):
        sg4 = v4(sg.ap)
        tt("dve", logf4, sg4, oml.ap.unsqueeze(2).to_broadcast([P, 4, P]), ALU.mult, [sg, oml], [logf])
        act(logf.ap, logf.ap, AF.Ln, [logf], [logf], bias=C_ONE, scale=-1.0)
        yield
        for hd in range(4):
            S.op("dve", (lambda hd_: (lambda h: h.tensor_tensor_scan(
                out=cum4[:, hd_, :], data0=ones_f.ap, data1=logf4[:, hd_, :], initial=0.0,
                op0=ALU.mult, op1=ALU.add)))(hd), [logf, ones_f], [cum])
        yield
        tt("dve", ekl4, cum4[:, :, P - 1:P].to_broadcast([P, 4, P]), cum4, ALU.subtract, [cum], [ekl])
        act(ekl.ap, ekl.ap, AF.Exp, [ekl], [ekl])
        act(dec.ap, cum4[:, :, P - 1], AF.Exp, [cum], [dec])
        yield
        if valid_col is None:
            tt("dve", khT.ap, ekl.ap, sg.ap, ALU.mult, [ekl, sg], [khT])
        else:
            stt(khT.ap, ekl.ap, valid_col, sg.ap, ALU.mult, ALU.mult, [ekl, sg, validt], [khT])
        yield
        for hd in range(4):
            tr(bD_b4[:, hd, :], khT4[:, hd, :], identb.ap, [khT, identb], [bD])
        cp("act", kh.ap, bD.ap.bitcast(BF16)[:, 0:512], [bD], [kh])
        yield

    def state_update(v_tm, kh, dec):
        kh4 = v4(kh.ap)
        for hd in range(4):
            mm(bC4[:, hd, :], kh4[:, hd, :], v_tm.ap[:, hd * P:(hd + 1) * P], True, True, [kh, v_tm], [bC])
        yield
        for hd in range(4):
            stt(S4[:, hd, :], S4[:, hd, :], dec.ap[:, hd:hd + 1], bC4[:, hd, :], ALU.mult, ALU.add,
                [Sst, dec, bC], [Sst])
        yield

    def layernorm_tm(src, width, eps_ap, g_b, b_b, dst, stats):
        st6_, mv_, r_ = stats
        nch = width // 512
        for c in range(nch):
            S.op("dve", (lambda c_: (lambda h: h.bn_stats(out=st6_.ap[:, c_ * 6:(c_ + 1) * 6],
                                                          in_=src.ap[:, c_ * 512:(c_ + 1) * 512])))(c), [src], [st6_])
        S.op("dve", lambda h: h.bn_aggr(out=mv_.ap, in_=st6_.ap[:, 0:6 * nch]), [st6_], [mv_])
        yield
        act(r_.ap[:, 0:1], mv_.ap[:, 1:2], AF.Ln, [mv_], [r_], bias=eps_ap)
        act(r_.ap[:, 0:1], r_.ap[:, 0:1], AF.Exp, [r_], [r_], scale=-0.5)
        yield
        stt(r_.ap[:, 1:2], mv_.ap[:, 0:1], -1.0, r_.ap[:, 0:1], ALU.mult, ALU.mult, [mv_, r_], [r_])
        act(src.ap, src.ap, AF.Identity, [src, r_], [src], bias=r_.ap[:, 1:2], scale=r_.ap[:, 0:1])
        yield
        tt("pool", src.ap, src.ap, g_b.ap, ALU.mult, [src, g_b], [src])
        yield
        tt("dve", dst.ap, src.ap, b_b.ap, ALU.add, [src, b_b], [dst])
        yield

    def PF(j):
        s = j % 2
        yield from load_xT(x_prev[j * P:(j + 1) * P, :], xin[j % 4])
        yield from proj_fm(bA, 512)
        act(tmpA.ap, bA.ap, AF.Exp, [bA], [tmpA])
        act(tmpA.ap, tmpA.ap, AF.Ln, [tmpA], [tmpA], bias=C_ONE)
        act(sg_[s].ap, tmpA.ap, AF.Exp, [tmpA], [sg_[s]], scale=-1.0)
        yield
        yield from proj_tm(bB, 1024)
        cp("act", vtm_[s].ap, bB.ap, [bB], [vtm_[s]])
        yield

    def PM(j):
        s = j % 2
        yield from gate_state(sg_[s], validt.ap[:, j:j + 1], kh_[s], dec_[s])
        yield from state_update(vtm_[s], kh_[s], dec_[s])

    def zero_gen():
        for r in range(NE * CAP // P):
            dma("sp", xpad[r * P:(r + 1) * P, :], zt.ap, [zt], [Buf()])
            yield

    zg = zero_gen()
    for j in range(NPREV + 1):
        interleave(PF(j) if j < NPREV else None, PM(j - 1) if j >= 1 else None, names=["PF", "PM"])
        for _ in range(2):
            next(zg, None)
    for _ in zg:
        pass
    cp("act", Sbf.ap, Sst.ap, [Sst], [Sbf])
    if dbg:
        dma("sp", dbg_S, Sst.ap, [Sst], [Buf()])
        for jj, t_ in enumerate([shr1_b, g1p_b, sc2p_b, sh2_b, g2p_b]):
            dma("sp", dbg_par[:, jj * D:(jj + 1) * D], t_.ap, [t_], [Buf()])
    S.barrier()

    def FE(i):
        s = i % 2
        yield from load_xT(x_own[i * P:(i + 1) * P, :], xin[i % 4])
        yield from proj_fm(bA, 2048)
        act(tu_[s].ap, bA.ap, AF.Gelu, [bA], [tu_[s]])
        yield from proj_tm(bB, 2560)
        act(gv_[s].ap, bB.ap, AF.Gelu, [bB], [gv_[s]])
        yield

    def sigmoid_exp(dst, bank, sign):
        act(dst.ap, bank.ap, AF.Exp, [bank], [dst], scale=-sign)
        act(dst.ap, dst.ap, AF.Ln, [dst], [dst], bias=C_ONE)
        act(dst.ap, dst.ap, AF.Exp, [dst], [dst], scale=-1.0)

    def FX(i):
        s = i % 2
        yield from proj_fm(bA, 0)
        sigmoid_exp(tmpA, bA, 1.0)
        yield
        tt("dve", qs_[s].ap, bA.ap, tmpA.ap, ALU.mult, [bA, tmpA], [qs_[s]])
        yield from proj_fm(bB, 1536)
        sigmoid_exp(tmpB, bB, 1.0)
        yield
        tt("dve", gs_[i % 3].ap, bB.ap, tmpB.ap, ALU.mult, [bB, tmpB], [gs_[i % 3]])
        yield from proj_fm(bA, 512)
        sigmoid_exp(sg_[s], bA, -1.0)
        yield
        yield from proj_tm(bB, 1024)
        cp("act", vtm_[i % 3].ap, bB.ap, [bB], [vtm_[i % 3]])
        yield

    def M1(i):
        s = i % 2
        tu, gv, ybT = tu_[s], gv_[s], ybT_[i % 3]
        bT4 = v4(bT.ap)
        yield from layernorm_tm(gv, 512, C_EPS, lng_b, lnb_b, vn, stM)
        for g in range(4):
            mm(bT4[:, g, :], vn.ap[:, g * P:(g + 1) * P], v4(WcT.ap)[:, g, :], True, True, [vn, WcT], [bT])
        tt("dve", tmpC.ap, bT.ap, sgb_b.ap, ALU.add, [bT, sgb_b], [tmpC])
        yield
        tt("dve", ybT.ap, tu.ap, tmpC.ap, ALU.mult, [tu, tmpC], [ybT])
        yield

    def M2a(i):
        s = i % 2
        sg, qs, kh, dec, scT, qh = sg_[s], qs_[s], kh_[s], dec_[s], scT_[s], qh_[s]
        sg4, qs4, qt4, scT4, qh4 = (v4(t.ap) for t in (sg, qs, qt, scT, qh))
        yield from gate_state(sg, None, kh, dec)
        ts("dve", ncol.ap.rearrange("p (h i) -> p h i", h=4)[:, :, 1:4],
           cum.ap.rearrange("p (h b j) -> p h b j", h=4, b=4)[:, :, 0:3, 31], -1.0, None, ALU.mult, None, [cum], [ncol])
        yield
        n = 0
        for hd in range(4):
            for I in range(4):
                e_q = eq[n % 4]
                e_k = ek[n % 4]
                k_t = kt[n % 4]
                n += 1
                M_ = 32 * (I + 1)
                bq = C_ZERO if I == 0 else ncol.ap[:, hd * 4 + I:hd * 4 + I + 1]
                bkk = C_ZERO if I == 0 else cum4[:, hd, 32 * I - 1:32 * I]
                act(e_q.ap, cum4[:, hd, 32 * I:32 * I + 32], AF.Exp, [cum, ncol], [e_q], bias=bq)
                act(e_k.ap[:, 0:M_], cum4[:, hd, 0:M_], AF.Exp, [cum], [e_k], bias=bkk, scale=-1.0)
                stt(qt4[:, hd, 32 * I:32 * I + 32], e_q.ap, oml.ap[:, hd:hd + 1], qs4[:, hd, 32 * I:32 * I + 32],
                    ALU.mult, ALU.mult, [e_q, oml, qs], [qt])
                tt("pool", k_t.ap[:, 0:M_], e_k.ap[:, 0:M_], sg4[:, hd, 0:M_], ALU.mult, [e_k, sg], [k_t])
                mm(bD4[0:M_, hd, 32 * I:32 * I + 32], k_t.ap[:, 0:M_], qt4[:, hd, 32 * I:32 * I + 32], True, True,
                   [k_t, qt], [bD])
                yield
        tt("dve", scT4, bD4, mask3, ALU.mult, [bD, mask_f], [scT])
        act(ekl.ap, cum.ap, AF.Exp, [cum], [ekl])
        yield
        for hd in range(4):
            stt(qh4[:, hd, :], ekl4[:, hd, :], oml.ap[:, hd:hd + 1], qs4[:, hd, :], ALU.mult, ALU.mult,
                [ekl, oml, qs], [qh])
        yield

    def M2b(i):
        s = i % 2
        v_tm, gs, kh, dec, scT, qh, yaT = vtm_[i % 3], gs_[i % 3], kh_[s], dec_[s], scT_[s], qh_[s], yaT_[s]
        scT4, qh4 = v4(scT.ap), v4(qh.ap)
        for hd in range(4):
            mm(bE4[:, hd, :], v_tm.ap[:, hd * P:(hd + 1) * P], scT4[:, hd, :], True, False, [v_tm, scT], [bE])
            mm(bE4[:, hd, :], Sb4[:, hd, :], qh4[:, hd, :], False, True, [Sbf, qh], [bE])
        yield
        yield from state_update(v_tm, kh, dec)
        cp("act", Sbf.ap, Sst.ap, [Sst], [Sbf])
        act(osq.ap, bE.ap, AF.Square, [bE], [osq])
        yield
        mm(bC.ap, ones_b.ap, osq.ap, True, True, [ones_b, osq], [bC])
        yield
        act(rstd.ap, bC.ap, AF.Ln, [bC], [rstd], bias=C_REPS, scale=1.0 / 128.0)
        yield
        act(rstd.ap, rstd.ap, AF.Exp, [rstd], [rstd], scale=-0.5)
        yield
        stt(rstd.ap, bE.ap, gnw.ap[:, 0:1], rstd.ap, ALU.mult, ALU.mult, [bE, gnw, rstd], [rstd])
        yield
        tt("pool", yaT.ap, rstd.ap, gs.ap, ALU.mult, [rstd, gs], [yaT])
        yield

    def T1(i):
        lg = lg_[i % 2]
        s = i % 2
        xi = xin[i % 4]
        yaT4, ybT4 = v4(yaT_[s].ap), v4(ybT_[i % 3].ap)
        for half, pb in ((0, bF), (1, bG)):
            for k in range(8):
                lhs = yaT4[:, k, :] if k < 4 else ybT4[:, k - 4, :]
                mm(pb.ap, lhs, w_out_v[:, k, half * 512:(half + 1) * 512], k == 0, k == 7,
                   [yaT_[s], ybT_[i % 3], w_out_b], [pb])
                if k % 4 == 3:
                    yield
        for half, pb in ((0, bF), (1, bG)):
            tt("dve", rr.ap[:, half * 512:(half + 1) * 512], pb.ap, g1p_b.ap[:, half * 512:(half + 1) * 512],
               ALU.mult, [pb, g1p_b], [rr])
            yield
        stt(rr.ap, xi.ap, ALPHA, rr.ap, ALU.mult, ALU.add, [xi, rr], [rr])
        yield
        yield from layernorm_tm(rr, D, C_EPS, ln1g_b, ln1b_b, x1, stT)
        dma("sp", x1scr[i * P:(i + 1) * P, :], x1.ap, [x1], [x1_bufs[i]])
        hb_ = h2b[i % 2]
        tt("pool", h2.ap, x1.ap, sc2p_b.ap, ALU.mult, [x1, sc2p_b], [h2])
        yield
        tt("dve", h2.ap, h2.ap, sh2_b.ap, ALU.add, [h2, sh2_b], [h2])
        cp("act", hb_.ap, h2.ap, [h2], [hb_])
        yield
        for half, pb in ((0, bF), (1, bG)):
            for k4 in range(4):
                kc = half * 4 + k4
                tr(v4(pb.ap)[:, k4, :], h2.ap[:, kc * P:(kc + 1) * P], identf.ap, [h2, identf], [pb])
            cp("act", h2T.ap[:, half * 512:(half + 1) * 512], pb.ap, [pb], [h2T])
            yield
        h2T_v = v8(h2T.ap)
        for kc in range(8):
            mm(bF.ap[:, 0:36], h2T_v[:, kc, :], RW_v[:, kc, :], kc == 0, kc == 7, [h2T, RW], [bF])
        yield
        tt("dve", lg.ap, bF.ap[:, 0:36], rb_b.ap, ALU.add, [bF, rb_b], [lg])
        yield

    def T2(i):
        lg = lg_[i % 2]
        hb_ = h2b[i % 2]
        red(sm.ap[:, 0:1], lg.ap[:, 0:4], ALU.max, [lg], [sm])
        ts("dve", sm.ap[:, 1:2], sm.ap[:, 0:1], -1.0, None, ALU.mult, None, [sm], [sm])
        yield
        ts("dve", sm.ap[:, 4:8], lg.ap[:, 0:4], sm.ap[:, 0:1], None, ALU.is_equal, None, [lg, sm], [sm])
        act(sm.ap[:, 12:16], lg.ap[:, 0:4], AF.Exp, [lg, sm], [sm], bias=sm.ap[:, 1:2])
        yield
        red(sm.ap[:, 2:3], sm.ap[:, 12:16], ALU.add, [sm], [sm])
        S.op("dve", lambda h: h.reciprocal(out=sm.ap[:, 3:4], in_=sm.ap[:, 2:3]), [sm], [sm])
        yield
        ts("dve", sm.ap[:, 8:12], sm.ap[:, 4:8], 1.0, 1e30, ALU.subtract, ALU.mult, [sm], [sm])
        for g in range(4):
            ts("dve", ml.ap[:, g * 8:(g + 1) * 8], lg.ap[:, 4 + g * 8:4 + (g + 1) * 8], sm.ap[:, 8 + g:9 + g], None,
               ALU.add, None, [lg, sm], [ml])
        yield
        S.op("dve", lambda h: h.max(out=mx8.ap, in_=ml.ap), [ml], [mx8])
        S.op("dve", lambda h: h.max_index(out=ix8.ap, in_max=mx8.ap, in_values=ml.ap), [ml, mx8], [ix8])
        yield
        tt("dve", sm.ap[:, 16:17], mx8.ap[:, 1:2], mx8.ap[:, 0:1], ALU.subtract, [mx8], [sm])
        act(sm.ap[:, 17:18], sm.ap[:, 16:17], AF.Exp, [sm], [sm])
        yield
        ts("dve", sm.ap[:, 18:19], sm.ap[:, 17:18], 1.0, None, ALU.add, None, [sm], [sm])
        S.op("dve", lambda h: h.reciprocal(out=sm.ap[:, 19:20], in_=sm.ap[:, 18:19]), [sm], [sm])
        yield
        tt("dve", wgt0.ap[:, i:i + 1], sm.ap[:, 3:4], sm.ap[:, 19:20], ALU.mult, [sm], [wgt0])
        tt("dve", wgt1.ap[:, i:i + 1], wgt0.ap[:, i:i + 1], sm.ap[:, 17:18], ALU.mult, [sm, wgt0], [wgt1])
        yield
        cp("dve", sm.ap[:, 20:22], ix8.ap[:, 0:2], [ix8], [sm])
        ts("dve", A0.ap, eidx.ap, sm.ap[:, 20:21], None, ALU.is_equal, None, [eidx, sm], [A0])
        ts("dve", A1.ap, eidx.ap, sm.ap[:, 21:22], None, ALU.is_equal, None, [eidx, sm], [A1])
        tt("dve", AA.ap, A0.ap, A1.ap, ALU.add, [A0, A1], [AA])
        yield
        mm(bT.ap[:, 0:32], ustrict.ap, AA.ap, True, True, [ustrict, AA], [bT])
        mm(bT.ap[:, 64:96], ones_f.ap, AA.ap, True, True, [ones_f, AA], [bT])
        tt("dve", Cc.ap, bT.ap[:, 0:32], cntbase.ap, ALU.add, [bT, cntbase], [Cc])
        tt("dve", cntbase.ap, cntbase.ap, bT.ap[:, 64:96], ALU.add, [cntbase, bT], [cntbase])
        yield
        ts("dve", Cc.ap, Cc.ap, float(CAP - 1), None, ALU.min, None, [Cc], [Cc])
        tt("dve", Cc.ap, Cc.ap, ebase.ap, ALU.add, [Cc, ebase], [Cc])
        yield
        tt("dve", A0.ap, A0.ap, Cc.ap, ALU.mult, [A0, Cc], [A0])
        tt("dve", A1.ap, A1.ap, Cc.ap, ALU.mult, [A1, Cc], [A1])
        red(sm.ap[:, 22:23], A0.ap, ALU.add, [A0], [sm])
        red(sm.ap[:, 23:24], A1.ap, ALU.add, [A1], [sm])
        yield
        ts("dve", sm.ap[:, 22:24], sm.ap[:, 22:24], 0.0, float(NE * CAP - 1), ALU.max, ALU.min, [sm], [sm])
        cp("dve", slot0.ap[:, i:i + 1], sm.ap[:, 22:23], [sm], [slot0])
        cp("dve", slot1.ap[:, i:i + 1], sm.ap[:, 23:24], [sm], [slot1])
        yield
        for sl in (slot0, slot1):
            S.dma("pool", (lambda sl_, hb__, ii: (lambda h: h.indirect_dma_start(
                out=xpad, out_offset=bass.IndirectOffsetOnAxis(ap=sl_.ap[:, ii:ii + 1], axis=0),
                in_=hb__.ap, in_offset=None)))(sl, hb_, i), [hb_, sl], [Buf()])
        yield

    def stage(g, k):
        return g(k) if 0 <= k < NT else None

    for t in range(NT + 5):
        if t < NT:
            interleave(FE(t))
        interleave(stage(M2a, t - 1), stage(FX, t), stage(T1, t - 3), stage(M2b, t - 2), stage(M1, t - 1),
                   stage(T2, t - 4), names=["M2a", "FX", "T1", "M2b", "M1", "T2"])

    if dbg:
        dbgm = A.alloc(4 * NT + NE, F32, "dbgm")
        cp("dve", dbgm.ap[:, 0:NT], slot0.ap, [slot0], [dbgm])
        cp("dve", dbgm.ap[:, NT:2 * NT], slot1.ap, [slot1], [dbgm])
        cp("dve", dbgm.ap[:, 2 * NT:3 * NT], wgt0.ap, [wgt0], [dbgm])
        cp("dve", dbgm.ap[:, 3 * NT:4 * NT], wgt1.ap, [wgt1], [dbgm])
        cp("dve", dbgm.ap[:, 4 * NT:4 * NT + NE], cntbase.ap, [cntbase], [dbgm])
        dma("sp", dbg_misc, dbgm.ap, [dbgm], [Buf()])
    S.barrier()
    A.release(m_persist)
    if stop == "p1":
        S.run()
        for cm in reversed(bank_cms):
            cm.__exit__(None, None, None)
        arena_cm.__exit__(None, None, None)
        S.close()
        return nc

    wu = [A.alloc(8 * 1024, BF16, "wu%d" % i) for i in range(3)]
    wd = [A.alloc(4 * 1024, BF16, "wd%d" % i) for i in range(3)]
    Xe = [[A.alloc(D, BF16, "Xe%d_%d" % (i, s)) for s in range(NSB)] for i in range(2)]
    XeT = [A.alloc(8 * CAP, BF16, "XeT%d" % i) for i in range(2)]
    sgt = [A.alloc(CAP, F32, "sgt%d" % i) for i in range(2)]
    gt_ = [A.alloc(CAP, F32, "gt%d" % i) for i in range(2)]
    actT = [A.alloc(4 * CAP, BF16, "actT%d" % i) for i in range(2)]
    Yt = [A.alloc(D, BF16, "Yt%d" % i) for i in range(6)]
    ypad_b = Buf("ypad")
    print("arena after phase2 alloc:", A.off, "of", A.nbytes)
    w_up_v = w_up.rearrange("e (k p) n -> e p k n", p=P)
    w_down_v = w_down.rearrange("e (k p) n -> e p k n", p=P)
    stg = [A.alloc(2 * 1024, F32, "stg%d" % i) for i in range(4)]
    print("arena after phase2 alloc (+staging):", A.off, "of", A.nbytes)
    stg_n = [0]

    def wload(e):
        bw = e % 3
        wuv = wu[bw].ap.rearrange("p (k n) -> p k n", k=8)
        wdv = wd[bw].ap.rearrange("p (k n) -> p k n", k=4)
        chunks = [(wuv[:, 2 * k:2 * k + 2, :], w_up_v[e, :, 2 * k:2 * k + 2, :], wu[bw]) for k in range(4)] + \
                 [(wdv[:, 2 * k:2 * k + 2, :], w_down_v[e, :, 2 * k:2 * k + 2, :], wd[bw]) for k in range(2)]
        pend = []
        for ci, (dst, src, dtl) in enumerate(chunks):
            st = stg[stg_n[0] % 4]
            stg_n[0] += 1
            dma("sp", st.ap.rearrange("p (k n) -> p k n", k=2), src, [], [st])
            pend.append((dst, st, dtl, ci))
            yield
            if len(pend) > 2:
                d_, s_, t_, c_ = pend.pop(0)
                cp("act" if c_ % 2 == 0 else "dve", d_, s_.ap.rearrange("p (k n) -> p k n", k=2), [s_], [t_])
                yield
        for d_, s_, t_, c_ in pend:
            cp("act" if c_ % 2 == 0 else "dve", d_, s_.ap.rearrange("p (k n) -> p k n", k=2), [s_], [t_])
            yield

    yn = [0]

    def compute(e):
        bu = e % 2
        bw = e % 3
        wuv = wu[bw].ap.rearrange("p (k n) -> p k n", k=8)
        wdv = wd[bw].ap.rearrange("p (k n) -> p k n", k=4)
        xet = XeT[bu]
        xetv = xet.ap.rearrange("p (k s) -> p k s", k=8)
        for sb_ in range(NSB):
            xe = Xe[bu][sb_]
            dma("act", xe.ap, xpad[e * CAP + sb_ * P:e * CAP + (sb_ + 1) * P, :], [xpad_b], [xe])
            pb = bk[sb_ % 2]
            pb8 = v8(pb.ap.bitcast(BF16))
            for kc in range(8):
                tr(pb8[:, kc, :], xe.ap[:, kc * P:(kc + 1) * P], identb.ap, [xe, identb], [pb])
            cp("act" if sb_ % 2 == 0 else "dve", xetv[:, :, sb_ * P:(sb_ + 1) * P], pb8, [pb], [xet])
            yield
        at = actT[bu]
        atv = at.ap.rearrange("p (k s) -> p k s", k=4)
        for cb in range(4):
            pg = bk[2 + (cb % 2)]
            pu = bk[4 + (cb % 2)]
            for kc in range(8):
                mm(pg.ap[:, 0:CAP], wuv[:, kc, cb * P:(cb + 1) * P], xetv[:, kc, :], kc == 0, kc == 7, [wu[bw], xet], [pg])
            yield
            for kc in range(8):
                mm(pu.ap[:, 0:CAP], wuv[:, kc, 512 + cb * P:512 + (cb + 1) * P], xetv[:, kc, :], kc == 0, kc == 7,
                   [wu[bw], xet], [pu])
            yield
            s_ = sgt[cb % 2]
            g_ = gt_[cb % 2]
            act(s_.ap, pg.ap[:, 0:CAP], AF.Sigmoid, [pg], [s_])
            tt("dve", g_.ap, pg.ap[:, 0:CAP], s_.ap, ALU.mult, [pg, s_], [g_])
            tt("dve", atv[:, cb, :], pu.ap[:, 0:CAP], g_.ap, ALU.mult, [pu, g_], [at])
            yield
        for sb_ in range(NSB):
            y = Yt[yn[0] % 6]
            yn[0] += 1
            for half in range(2):
                pb = bk[6 + half]
                for hc in range(4):
                    mm(pb.ap, atv[:, hc, sb_ * P:(sb_ + 1) * P], wdv[:, hc, half * 512:(half + 1) * 512],
                       hc == 0, hc == 3, [at, wd[bw]], [pb])
                cp("act" if half == 0 else "dve", y.ap[:, half * 512:(half + 1) * 512], pb.ap, [pb], [y])
                yield
            dma("act", ypad[e * CAP + sb_ * P:e * CAP + (sb_ + 1) * P, :], y.ap, [y], [Buf()])

    interleave(wload(0))
    for e in range(NE):
        interleave(compute(e), wload(e + 1) if e + 1 < NE else None, names=["cmp", "wl"])

    if dbg:
        for r in range(NE * CAP // P):
            dma("sp", dbg_ypad[r * P:(r + 1) * P, :], ypad[r * P:(r + 1) * P, :], [ypad_b], [Buf()])
            dma("sp", dbg_xpad[r * P:(r + 1) * P, :], xpad[r * P:(r + 1) * P, :], [xpad_b], [Buf()])
        dma("sp", dbg_wu, wu[1].ap, [wu[1]], [Buf()])
    S.barrier()
    A.release(m_persist)

    NW3 = 4
    Y0 = [A.alloc(D, BF16, "Y0_%d" % i) for i in range(NW3)]
    Y1 = [A.alloc(D, BF16, "Y1_%d" % i) for i in range(NW3)]
    x1t = [A.alloc(D, F32, "x1t%d" % i) for i in range(NW3)]
    mt = [A.alloc(D, F32, "mt%d" % i) for i in range(NW3)]
    ot = [A.alloc(D, F32, "ot%d" % i) for i in range(NW3)]
    st3 = [(A.alloc(12, F32, "bst6_3%d" % i), A.alloc(2, F32, "mv_3%d" % i), A.alloc(4, F32, "rs_3%d" % i))
           for i in range(NW3)]

    def P3(i):
        p2 = i % NW3
        for sl, yt in ((slot0, Y0[p2]), (slot1, Y1[p2])):
            S.dma("pool", (lambda sl_, yt_, ii: (lambda h: h.indirect_dma_start(
                out=yt_.ap, out_offset=None, in_=ypad,
                in_offset=bass.IndirectOffsetOnAxis(ap=sl_.ap[:, ii:ii + 1], axis=0))))(sl, yt, i), [sl], [yt])
        dma("sp", x1t[p2].ap, x1scr[i * P:(i + 1) * P, :], [x1_bufs[i]], [x1t[p2]])
        yield
        m = mt[p2]
        act(m.ap, Y0[p2].ap, AF.Identity, [Y0[p2], wgt0], [m], scale=wgt0.ap[:, i:i + 1])
        yield
        stt(m.ap, Y1[p2].ap, wgt1.ap[:, i:i + 1], m.ap, ALU.mult, ALU.add, [Y1[p2], wgt1, m], [m])
        yield
        tt("pool", m.ap, m.ap, g2p_b.ap, ALU.mult, [m, g2p_b], [m])
        yield
        stt(m.ap, x1t[p2].ap, ALPHA, m.ap, ALU.mult, ALU.add, [x1t[p2], m], [m])
        yield
        yield from layernorm_tm(m, D, C_EPS, ln2g_b, ln2b_b, ot[p2], st3[p2])
        dma("sp", out[i * P:(i + 1) * P, :], ot[p2].ap, [ot[p2]], [Buf()])
        yield

    for i in range(0, NT, NW3):
        interleave(*[P3(i + w) for w in range(NW3)])
    S.barrier(["sp"])
    S.run()
    for cm in reversed(bank_cms):
        cm.__exit__(None, None, None)
    arena_cm.__exit__(None, None, None)
    S.close()
    return nc


def kernel(x, c, w_ada, b_ada, w_in, lb_logits, hg_norm_w, sg_ln_g, sg_ln_b, sg_w, sg_b,
           w_out, ln1_g, ln1_b, router_group_w, router_group_b, router_expert_w,
           router_expert_b, w_up, w_down, ln2_g, ln2_b):
    f = lambda a: np.ascontiguousarray(np.asarray(a, dtype=np.float32))
    x = f(x); c = f(c)
    B, SEQ, _ = x.shape
    QT = SEQ // 4
    rw = np.concatenate([f(router_group_w)[0], f(router_expert_w)[0].transpose(1, 0, 2).reshape(D, 32)], axis=1)
    rb = np.concatenate([f(router_group_b)[0], f(router_expert_b)[0].reshape(32)])[None, :]
    shared = {
        "w_ada": f(w_ada)[0], "b_ada": f(b_ada), "w_in": f(w_in)[0],
        "lbT": f(np.asarray(lb_logits).reshape(2, 4, P).transpose(2, 0, 1).reshape(P, 8)),
        "gnw": f(np.asarray(hg_norm_w).reshape(P, 1)),
        "sg_ln_g": f(sg_ln_g), "sg_ln_b": f(sg_ln_b),
        "sg_wT": f(np.asarray(sg_w)[0].transpose(2, 0, 1)),
        "sg_b": f(np.asarray(sg_b).reshape(1, 512)),
        "w_out": f(w_out)[0], "ln1_g": f(ln1_g), "ln1_b": f(ln1_b),
        "rw": f(rw), "rb": f(rb), "w_up": f(w_up)[0], "w_down": f(w_down)[0],
        "ln2_g": f(ln2_g), "ln2_b": f(ln2_b),
    }
    in_maps = []
    for core in range(8):
        b, q = core // 4, core % 4
        xo = x[b, q * QT:(q + 1) * QT, :]
        xp = np.zeros((NPREV * P, D), np.float32)
        val = np.zeros((P, NPREV), np.float32)
        npv = q * QT
        if npv:
            xp[NPREV * P - npv:, :] = x[b, :npv, :]
            val[:, NPREV - npv // P:] = 1.0
        d = dict(shared)
        d.update({"x_own": np.ascontiguousarray(xo), "x_prev": xp, "valid": val,
                  "c_row": np.ascontiguousarray(c[b:b + 1, :])})
        in_maps.append(d)
    if _DBG:
        return in_maps
    nc = build_nc()
    res = run_bass_kernel_spmd(nc, in_maps, core_ids=list(range(8)))
    outp = np.zeros((B, SEQ, D), np.float32)
    for core in range(8):
        b, q = core // 4, core % 4
        outp[b, q * QT:(q + 1) * QT, :] = np.asarray(res.results[core]["out"], dtype=np.float32)
    return outp
```

```python
import numpy as np
import concourse.bass as bass
import concourse.mybir as mybir
from concourse.bass_utils import run_bass_kernel_spmd

F32 = mybir.dt.float32
BF16 = mybir.dt.bfloat16
I32 = mybir.dt.int32
U32 = mybir.dt.uint32
AF = mybir.ActivationFunctionType
ALU = mybir.AluOpType
AX = mybir.AxisListType

P = 128
D = 1024
NT = 16
NPREV = 48
NE = 32
CAP = 384
NSB = CAP // 128
HID = 512
ALPHA = 2.0 ** 0.25
LN_EPS = 1e-5
RMS_EPS = 1e-6
SAME_ENG_SYNC = True
_DBG = False


class Buf:
    __slots__ = ("name", "writer", "readers")

    def __init__(self, name=""):
        self.name = name
        self.writer = None
        self.readers = []


class Tl:
    __slots__ = ("ap", "b")

    def __init__(self, ap, name=""):
        self.ap = ap
        self.b = Buf(name)


def _b(x):
    return x.b if isinstance(x, Tl) else x


class Sched:
    def __init__(self, nc, n_dma_sems=24, n_sw_sems=64):
        self.nc = nc
        self.n_sw_sems = n_sw_sems
        self.sw_sems = []
        self.sw_next = 0
        self.engs = ["pe", "act", "dve", "pool", "sp"]
        self.prog = {e: [] for e in self.engs}
        self.count = {e: 0 for e in self.engs}
        self.sem = {}
        self.known = {e: {} for e in self.engs}
        self.n_dma_sems = n_dma_sems
        self.dma_sems = []
        self.dma_sem_val = []
        self.dma_rr = 0
        self._cms = []

    def open(self):
        for e in ["pe", "act", "dve", "pool"]:
            cm = self.nc.semaphore("s_" + e)
            self.sem[e] = cm.__enter__()
            self._cms.append(cm)
        for i in range(self.n_dma_sems):
            cm = self.nc.semaphore("s_dma%d" % i)
            self.dma_sems.append(cm.__enter__())
            self.dma_sem_val.append(0)
            self._cms.append(cm)
        for i in range(self.n_sw_sems):
            cm = self.nc.semaphore("s_sw%d" % i)
            self.sw_sems.append(cm.__enter__())
            self._cms.append(cm)

    def close(self):
        for cm in reversed(self._cms):
            cm.__exit__(None, None, None)

    def _semh(self, key):
        if isinstance(key, tuple):
            return self.sw_sems[key[1]] if key[0] == "sw" else self.dma_sems[key[1]]
        return self.sem[key]

    def _collect(self, eng, reads, writes):
        need = {}

        def add(ev, raw):
            if ev is None:
                return
            k, v = ev
            if k == eng:
                if eng == "pe" or not SAME_ENG_SYNC or not raw:
                    return
            if self.known[eng].get(k, 0) >= v:
                return
            if need.get(k, 0) < v:
                need[k] = v
        for b in reads:
            add(_b(b).writer, True)
        for b in writes:
            bb = _b(b)
            add(bb.writer, False)
            for r in bb.readers:
                add(r, False)
        return need

    def _emit_waits(self, eng, need):
        waits = []
        for k, v in need.items():
            self.known[eng][k] = v
            waits.append((self._semh(k), v))
        return waits

    def op(self, eng, fn, reads=(), writes=()):
        need = self._collect(eng, reads, writes)
        waits = self._emit_waits(eng, need)
        self.count[eng] += 1
        idx = self.count[eng]
        sem = self.sem[eng]

        def thunk(h, waits=waits, fn=fn, sem=sem):
            for s, v in waits:
                h.wait_ge(s, v)
            fn(h).then_inc(sem, 1)
        self.prog[eng].append(thunk)
        ev = (eng, idx)
        for b in reads:
            _b(b).readers.append(ev)
        for b in writes:
            bb = _b(b)
            bb.writer = ev
            bb.readers = []
        return ev

    def dma(self, q, fn, reads=(), writes=()):
        if q == "pool":
            return self._dma_sw(fn, reads, writes)
        need = self._collect(q, reads, writes)
        i = self.dma_rr
        self.dma_rr = (self.dma_rr + 1) % self.n_dma_sems
        key = ("dma", i)
        prev = self.dma_sem_val[i]
        if prev > 0 and self.known[q].get(key, 0) < prev:
            need[key] = max(need.get(key, 0), prev)
        waits = self._emit_waits(q, need)
        val = prev + 16
        self.dma_sem_val[i] = val
        sem = self.dma_sems[i]

        def thunk(h, waits=waits, fn=fn, sem=sem):
            for s, v in waits:
                h.wait_ge(s, v)
            fn(h).then_inc(sem, 16)
        self.prog[q].append(thunk)
        ev = (key, val)
        for b in reads:
            _b(b).readers.append(ev)
        for b in writes:
            bb = _b(b)
            bb.writer = ev
            bb.readers = []
        return ev

    def _dma_sw(self, fn, reads, writes):
        q = "pool"
        need = self._collect(q, reads, writes)
        waits = self._emit_waits(q, need)
        i = self.sw_next
        self.sw_next += 1
        assert i < self.n_sw_sems, "out of software-DMA semaphores"
        sem = self.sw_sems[i]

        def thunk(h, waits=waits, fn=fn, sem=sem):
            for s, v in waits:
                h.wait_ge(s, v)
            fn(h).then_inc(sem, 16)
        self.prog[q].append(thunk)
        ev = (("sw", i), 16)
        for b in reads:
            _b(b).readers.append(ev)
        for b in writes:
            bb = _b(b)
            bb.writer = ev
            bb.readers = []
        return ev

    def barrier(self, engs=None):
        for e in (engs or self.engs):
            need = {}
            for o in ["pe", "act", "dve", "pool"]:
                if o != e and self.count[o] > self.known[e].get(o, 0):
                    need[o] = self.count[o]
            for i in range(self.n_dma_sems):
                key = ("dma", i)
                v = self.dma_sem_val[i]
                if v > self.known[e].get(key, 0):
                    need[key] = v
            for i in range(self.sw_next):
                key = ("sw", i)
                if self.known[e].get(key, 0) < 16:
                    need[key] = 16
            waits = self._emit_waits(e, need)

            def thunk(h, waits=waits):
                for s, v in waits:
                    h.wait_ge(s, v)
            self.prog[e].append(thunk)

    def run(self):
        nc = self.nc
        with nc.Block() as block:
            @block.tensor
            def _(h):
                for t in self.prog["pe"]:
                    t(h)

            @block.scalar
            def _(h):
                for t in self.prog["act"]:
                    t(h)

            @block.vector
            def _(h):
                for t in self.prog["dve"]:
                    t(h)

            @block.gpsimd
            def _(h):
                for t in self.prog["pool"]:
                    t(h)

            @block.sync
            def _(h):
                for t in self.prog["sp"]:
                    t(h)


class Arena:
    def __init__(self, ap_f32, nbytes):
        self.ap = ap_f32
        self.nbytes = nbytes
        self.off = 0
        self.peak = 0

    def alloc(self, n, dt, name=""):
        sz = {F32: 4, BF16: 2, I32: 4, U32: 4}[dt] * n
        sz = (sz + 31) // 32 * 32
        assert self.off + sz <= self.nbytes, ("arena overflow", name, self.off, sz, self.nbytes)
        v = self.ap[:, self.off // 4:(self.off + sz) // 4]
        if dt != F32:
            v = v.bitcast(dt)
        self.off += sz
        self.peak = max(self.peak, self.off)
        return Tl(v[:, 0:n], name)

    def mark(self):
        return self.off

    def release(self, m):
        self.off = m


def build_nc(dbg=False, stop=None):
    nc = bass.Bass("TRN2", target_bir_lowering=False)

    def din(name, shape, dt=F32):
        return nc.dram_tensor(name, shape, dt, kind="ExternalInput").ap()

    x_own = din("x_own", [NT * P, D])
    x_prev = din("x_prev", [NPREV * P, D])
    valid_in = din("valid", [P, NPREV])
    c_row = din("c_row", [1, D])
    w_ada = din("w_ada", [D, 6 * D])
    b_ada = din("b_ada", [1, 6 * D])
    w_in = din("w_in", [D, 3072])
    lbT = din("lbT", [P, 8])
    gnw_in = din("gnw", [P, 1])
    sg_ln_g = din("sg_ln_g", [1, 512])
    sg_ln_b = din("sg_ln_b", [1, 512])
    sg_wT = din("sg_wT", [P, 4, P])
    sg_b = din("sg_b", [1, 512])
    w_out = din("w_out", [D, D])
    ln1_g = din("ln1_g", [1, D])
    ln1_b = din("ln1_b", [1, D])
    rw = din("rw", [D, 36])
    rb = din("rb", [1, 36])
    w_up = din("w_up", [NE, D, 2 * HID])
    w_down = din("w_down", [NE, HID, D])
    ln2_g = din("ln2_g", [1, D])
    ln2_b = din("ln2_b", [1, D])
    out = nc.dram_tensor("out", [NT * P, D], F32, kind="ExternalOutput").ap()

    sk = "ExternalOutput" if dbg else "Internal"
    xpad = nc.dram_tensor("xpad", [NE * CAP, D], BF16, kind="Internal").ap()
    ypad = nc.dram_tensor("ypad", [NE * CAP, D], BF16, kind="Internal").ap()
    if dbg:
        dbg_ypad = nc.dram_tensor("dbg_ypad", [NE * CAP, D], BF16, kind="ExternalOutput").ap()
        dbg_xpad = nc.dram_tensor("dbg_xpad", [NE * CAP, D], BF16, kind="ExternalOutput").ap()
        dbg_wu = nc.dram_tensor("dbg_wu", [P, 8 * 1024], BF16, kind="ExternalOutput").ap()
    x1scr = nc.dram_tensor("x1scr", [NT * P, D], F32, kind=sk).ap()
    if dbg:
        dbg_S = nc.dram_tensor("dbg_S", [P, 512], F32, kind="ExternalOutput").ap()
        dbg_misc = nc.dram_tensor("dbg_misc", [P, 4 * NT + NE], F32, kind="ExternalOutput").ap()
        dbg_par = nc.dram_tensor("dbg_par", [P, 5 * D], F32, kind="ExternalOutput").ap()

    S = Sched(nc)
    S.open()
    ARENA_BYTES = 211968
    arena_cm = nc.sbuf_tensor("arena", [P, ARENA_BYTES // 4], F32)
    arena_t = arena_cm.__enter__()
    A = Arena(arena_t[:], ARENA_BYTES)
    bank_cms = [nc.psum_tensor("bank%d" % i, [P, 512], F32) for i in range(8)]
    bk = [Tl(cm.__enter__()[:], "bank%d" % i) for i, cm in enumerate(bank_cms)]

    def act(out_, in_, func, reads, writes, bias=None, scale=None):
        kw = {}
        if bias is not None:
            kw["bias"] = bias
        if scale is not None:
            kw["scale"] = scale
        S.op("act", lambda h: h.activation(out=out_, in_=in_, func=func, **kw), reads, writes)

    def tt(eng, out_, in0, in1, op, reads, writes):
        S.op(eng, lambda h: h.tensor_tensor(out=out_, in0=in0, in1=in1, op=op), reads, writes)

    def ts(eng, out_, in0, s1, s2, op0, op1, reads, writes):
        if op1 is None:
            S.op(eng, lambda h: h.tensor_scalar(out=out_, in0=in0, scalar1=s1, scalar2=None, op0=op0), reads, writes)
        else:
            S.op(eng, lambda h: h.tensor_scalar(out=out_, in0=in0, scalar1=s1, scalar2=s2, op0=op0, op1=op1), reads, writes)

    def stt(out_, in0, scalar, in1, op0, op1, reads, writes):
        S.op("dve", lambda h: h.scalar_tensor_tensor(out=out_, in0=in0, scalar=scalar, in1=in1, op0=op0, op1=op1),
             reads, writes)

    def cp(eng, out_, in_, reads, writes):
        if eng == "act":
            S.op("act", lambda h: h.copy(out=out_, in_=in_), reads, writes)
        else:
            S.op(eng, lambda h: h.tensor_copy(out=out_, in_=in_), reads, writes)

    def mm(out_, lhsT, rhs, start, stop, reads, writes):
        S.op("pe", lambda h: h.matmul(out_, lhsT=lhsT, rhs=rhs, start=start, stop=stop), reads, writes)

    def tr(out_, in_, ident, reads, writes):
        S.op("pe", lambda h: h.transpose(out=out_, in_=in_, identity=ident), reads, writes)

    def dma(q, out_, in_, reads, writes):
        S.dma(q, lambda h: h.dma_start(out=out_, in_=in_), reads, writes)

    def red(out_, in_, op, reads, writes):
        S.op("dve", lambda h: h.tensor_reduce(out=out_, in_=in_, axis=AX.X, op=op), reads, writes)

    def v4(ap):
        return ap.rearrange("p (h t) -> p h t", h=4)

    def v8(ap):
        return ap.rearrange("p (k t) -> p k t", k=8)

    identf = A.alloc(P, F32, "identf")
    identb = A.alloc(P, BF16, "identb")
    mask_f = A.alloc(P, F32, "mask_f")
    ustrict = A.alloc(P, F32, "ustrict")
    ones_f = A.alloc(P, F32, "ones_f")
    ones_b = A.alloc(P, BF16, "ones_b")
    io_pj = A.alloc(P, F32, "io_pj")
    ebase = A.alloc(NE, F32, "ebase")
    eidx = A.alloc(NE, F32, "eidx")
    consts = A.alloc(8, F32, "consts")
    slot0 = A.alloc(NT, I32, "slot0")
    slot1 = A.alloc(NT, I32, "slot1")
    wgt0 = A.alloc(NT, F32, "wgt0")
    wgt1 = A.alloc(NT, F32, "wgt1")
    cntbase = A.alloc(NE, F32, "cntbase")
    ln2g_b = A.alloc(D, F32, "ln2g_b")
    ln2b_b = A.alloc(D, F32, "ln2b_b")
    g2p_b = A.alloc(D, F32, "g2p_b")

    S.op("pool", lambda h: h.iota(io_pj.ap, pattern=[[1, P]], base=0, channel_multiplier=-1,
                                  allow_small_or_imprecise_dtypes=True), [], [io_pj])
    S.op("dve", lambda h: h.tensor_single_scalar(out=identf.ap, in_=io_pj.ap, scalar=0.0, op=ALU.is_equal), [io_pj], [identf])
    S.op("dve", lambda h: h.tensor_single_scalar(out=mask_f.ap, in_=io_pj.ap, scalar=0.0, op=ALU.is_ge), [io_pj], [mask_f])
    S.op("dve", lambda h: h.tensor_single_scalar(out=ustrict.ap, in_=io_pj.ap, scalar=0.0, op=ALU.is_gt), [io_pj], [ustrict])
    cp("dve", identb.ap, identf.ap, [identf], [identb])
    S.op("pool", lambda h: h.memset(ones_f.ap, 1.0), [], [ones_f])
    S.op("pool", lambda h: h.memset(ones_b.ap, 1.0), [], [ones_b])
    S.op("pool", lambda h: h.iota(ebase.ap, pattern=[[CAP, NE]], base=0, channel_multiplier=0,
                                  allow_small_or_imprecise_dtypes=True), [], [ebase])
    S.op("pool", lambda h: h.iota(eidx.ap, pattern=[[1, NE]], base=0, channel_multiplier=0,
                                  allow_small_or_imprecise_dtypes=True), [], [eidx])
    S.op("pool", lambda h: h.memset(cntbase.ap, 0.0), [], [cntbase])
    for j, val in enumerate([1.0, 4 * LN_EPS, LN_EPS, RMS_EPS, 0.0]):
        S.op("pool", (lambda v, jj: (lambda h: h.memset(consts.ap[:, jj:jj + 1], v)))(val, j), [], [consts])
    C_ONE = consts.ap[:, 0:1]
    C_4EPS = consts.ap[:, 1:2]
    C_EPS = consts.ap[:, 2:3]
    C_REPS = consts.ap[:, 3:4]
    C_ZERO = consts.ap[:, 4:5]
    for b_ in bk:
        S.op("dve", (lambda bb: (lambda h: h.memset(bb.ap, 0.0)))(b_), [], [b_])
    dma("sp", ln2g_b.ap, ln2_g.broadcast_to([P, D]), [], [ln2g_b])
    dma("sp", ln2b_b.ap, ln2_b.broadcast_to([P, D]), [], [ln2b_b])

    m_persist = A.mark()
    w_in_b = A.alloc(8 * 3072, BF16, "w_in_b")
    w_out_b = A.alloc(8 * D, BF16, "w_out_b")
    shr1_b = A.alloc(D, F32, "shr1_b")
    g1p_b = A.alloc(D, F32, "g1p_b")
    sc2p_b = A.alloc(D, F32, "sc2p_b")
    sh2_b = A.alloc(D, F32, "sh2_b")
    ln1g_b = A.alloc(D, F32, "ln1g_b")
    ln1b_b = A.alloc(D, F32, "ln1b_b")
    lng_b = A.alloc(512, F32, "lng_b")
    lnb_b = A.alloc(512, F32, "lnb_b")
    WcT = A.alloc(512, BF16, "WcT")
    sgb_b = A.alloc(512, F32, "sgb_b")
    RW = A.alloc(8 * 36, F32, "RW")
    rb_b = A.alloc(36, F32, "rb_b")
    oml = A.alloc(4, F32, "oml")
    noml = A.alloc(4, F32, "noml")
    gnw = A.alloc(1, F32, "gnw")
    Sst = A.alloc(512, F32, "Sst")
    Sbf = A.alloc(512, BF16, "Sbf")
    sc1p = A.alloc(8, F32, "sc1p")
    validt = A.alloc(NPREV, F32, "validt")
    w_in_v = w_in_b.ap.rearrange("p (k n) -> p k n", k=8)
    w_out_v = w_out_b.ap.rearrange("p (k n) -> p k n", k=8)
    RW_v = RW.ap.rearrange("p (k n) -> p k n", k=8)

    zt = A.alloc(D, BF16, "zt")
    m_work = A.mark()
    c_b = A.alloc(D, F32, "c_b")
    sgc = A.alloc(D, F32, "sgc")
    cact = A.alloc(D, F32, "cact")
    CT = A.alloc(D, F32, "CT")
    wst = [A.alloc(8 * 512, F32, "wst%d" % i) for i in range(2)]
    bst = [A.alloc(512, F32, "bst%d" % i) for i in range(2)]
    sec = [A.alloc(D, F32, "sec%d" % i) for i in range(2)]
    lbt = A.alloc(8, F32, "lbt")
    wct_st = A.alloc(512, F32, "wct_st")
    wist = [A.alloc(1536, F32, "wist%d" % i) for i in range(2)]

    dma("sp", c_b.ap, c_row.broadcast_to([P, D]), [], [c_b])
    dma("sp", lbt.ap, lbT, [], [lbt])
    dma("sp", gnw.ap, gnw_in, [], [gnw])
    dma("sp", validt.ap, valid_in, [], [validt])
    act(sgc.ap, c_b.ap, AF.Sigmoid, [c_b], [sgc])
    tt("dve", cact.ap, c_b.ap, sgc.ap, ALU.mult, [c_b, sgc], [cact])
    tt("dve", oml.ap, lbt.ap[:, 4:8], lbt.ap[:, 0:4], ALU.subtract, [lbt], [oml])
    act(oml.ap, oml.ap, AF.Sigmoid, [oml], [oml])
    ts("dve", noml.ap, oml.ap, -1.0, None, ALU.mult, None, [oml], [noml])
    for half in range(2):
        for k4 in range(4):
            kc = half * 4 + k4
            tr(v4(bk[half].ap)[:, k4, :], cact.ap[:, kc * P:(kc + 1) * P], identf.ap, [cact, identf], [bk[half]])
        cp("act", CT.ap[:, half * 512:(half + 1) * 512], bk[half].ap, [bk[half]], [CT])
    CT_v = v8(CT.ap)
    w_ada_v = w_ada.rearrange("(k p) n -> p k n", p=P)

    def ada_section(sidx, dst, plus_one):
        for hh in range(2):
            j = sidx * 2 + hh
            w = wst[j % 2]
            bsl = bst[j % 2]
            dma("sp", w.ap.rearrange("p (k n) -> p k n", k=8), w_ada_v[:, :, j * 512:(j + 1) * 512], [], [w])
            dma("sp", bsl.ap, b_ada[:, j * 512:(j + 1) * 512].broadcast_to([P, 512]), [], [bsl])
            pb = bk[2 + (j % 2)]
            wv = w.ap.rearrange("p (k n) -> p k n", k=8)
            for kc in range(8):
                mm(pb.ap, CT_v[:, kc, :], wv[:, kc, :], kc == 0, kc == 7, [CT, w], [pb])
            tt("dve", dst.ap[:, hh * 512:(hh + 1) * 512], pb.ap, bsl.ap, ALU.add, [pb, bsl], [dst])
        if plus_one:
            ts("dve", dst.ap, dst.ap, 1.0, None, ALU.add, None, [dst], [dst])

    ada_section(0, sec[0], False)
    ada_section(1, sec[1], True)
    S.op("dve", lambda h: h.reciprocal(out=shr1_b.ap, in_=sec[1].ap), [sec[1]], [shr1_b])
    tt("dve", shr1_b.ap, shr1_b.ap, sec[0].ap, ALU.mult, [shr1_b, sec[0]], [shr1_b])
    for half in range(2):
        for k4 in range(4):
            kc = half * 4 + k4
            tr(v4(bk[half].ap)[:, k4, :], sec[1].ap[:, kc * P:(kc + 1) * P], identf.ap, [sec[1], identf], [bk[half]])
        cp("dve", sc1p.ap[:, half * 4:(half + 1) * 4], v4(bk[half].ap)[:, :, 0], [bk[half]], [sc1p])
    for kc in range(8):
        for hh in range(2):
            st_ = wist[(kc * 2 + hh) % 2]
            dma("sp", st_.ap, w_in[kc * P:(kc + 1) * P, hh * 1536:(hh + 1) * 1536], [], [st_])
            act(w_in_v[:, kc, hh * 1536:(hh + 1) * 1536], st_.ap, AF.Identity, [st_, sc1p], [w_in_b],
                scale=sc1p.ap[:, kc:kc + 1])
    for kc in range(8):
        st_ = wist[kc % 2]
        dma("sp", st_.ap[:, 0:D], w_out[kc * P:(kc + 1) * P, :], [], [st_])
        cp("act", w_out_v[:, kc, :], st_.ap[:, 0:D], [st_], [w_out_b])
    dma("sp", wct_st.ap.rearrange("p (g t) -> p g t", g=4), sg_wT, [], [wct_st])
    tt("dve", v4(WcT.ap), v4(wct_st.ap), mask_f.ap.unsqueeze(1).to_broadcast([P, 4, P]), ALU.mult,
       [wct_st, mask_f], [WcT])
    dma("sp", sgb_b.ap, sg_b.broadcast_to([P, 512]), [], [sgb_b])
    dma("sp", lng_b.ap, sg_ln_g.broadcast_to([P, 512]), [], [lng_b])
    dma("sp", lnb_b.ap, sg_ln_b.broadcast_to([P, 512]), [], [lnb_b])
    dma("sp", ln1g_b.ap, ln1_g.broadcast_to([P, D]), [], [ln1g_b])
    dma("sp", ln1b_b.ap, ln1_b.broadcast_to([P, D]), [], [ln1b_b])
    dma("sp", RW_v, rw.rearrange("(k p) n -> p k n", p=P), [], [RW])
    dma("sp", rb_b.ap, rb.broadcast_to([P, 36]), [], [rb_b])
    S.op("pool", lambda h: h.memset(Sst.ap, 0.0), [], [Sst])

    ada_section(2, g1p_b, True)
    ada_section(3, sh2_b, False)
    ada_section(4, sc2p_b, True)
    ada_section(5, g2p_b, True)

    S.op("pool", lambda h: h.memset(zt.ap, 0.0), [], [zt])
    xpad_b = Buf("xpad")

    S.barrier()
    A.release(m_work)

    xin = [A.alloc(D, F32, "xin%d" % i) for i in range(4)]
    xb = A.alloc(D, BF16, "xb")
    xT = A.alloc(D, BF16, "xT")
    tmpA = A.alloc(512, F32, "tmpA")
    tmpB = A.alloc(512, F32, "tmpB")
    tmpC = A.alloc(512, F32, "tmpC")
    sg_ = [A.alloc(512, F32, "sg%d" % i) for i in range(2)]
    vtm_ = [A.alloc(512, BF16, "v_tm%d" % i) for i in range(3)]
    qs_ = [A.alloc(512, BF16, "qs%d" % i) for i in range(2)]
    gs_ = [A.alloc(512, BF16, "gs%d" % i) for i in range(3)]
    tu_ = [A.alloc(512, BF16, "tu%d" % i) for i in range(2)]
    gv_ = [A.alloc(512, F32, "gv%d" % i) for i in range(2)]
    yaT_ = [A.alloc(512, BF16, "yaT%d" % i) for i in range(2)]
    ybT_ = [A.alloc(512, BF16, "ybT%d" % i) for i in range(3)]
    logf = A.alloc(512, F32, "logf")
    cum = A.alloc(512, F32, "cum")
    ncol = A.alloc(16, F32, "ncol")
    ekl = A.alloc(512, F32, "ekl")
    khT = A.alloc(512, BF16, "khT")
    kh_ = [A.alloc(512, BF16, "kh%d" % i) for i in range(2)]
    dec_ = [A.alloc(4, F32, "dec%d" % i) for i in range(2)]
    vn = A.alloc(512, BF16, "vn")
    stM = (A.alloc(12, F32, "bst6M"), A.alloc(2, F32, "mvM"), A.alloc(4, F32, "rsM"))
    stT = (A.alloc(12, F32, "bst6T"), A.alloc(2, F32, "mvT"), A.alloc(4, F32, "rsT"))
    eq = [A.alloc(32, F32, "eq%d" % i) for i in range(4)]
    ek = [A.alloc(P, F32, "ek%d" % i) for i in range(4)]
    qt = A.alloc(512, BF16, "qt")
    kt = [A.alloc(P, BF16, "kt%d" % i) for i in range(4)]
    scT_ = [A.alloc(512, BF16, "scT%d" % i) for i in range(2)]
    qh_ = [A.alloc(512, BF16, "qh%d" % i) for i in range(2)]
    osq = A.alloc(512, BF16, "osq")
    rstd = A.alloc(512, F32, "rstd")
    rr = A.alloc(D, F32, "rr")
    x1 = A.alloc(D, F32, "x1")
    h2 = rr
    h2b = [A.alloc(D, BF16, "h2b%d" % i) for i in range(2)]
    h2T = A.alloc(D, F32, "h2T")
    lg_ = [A.alloc(36, F32, "lg%d" % i) for i in range(2)]
    sm = A.alloc(64, F32, "sm")
    ml = A.alloc(NE, F32, "ml")
    mx8 = A.alloc(8, F32, "mx8")
    ix8 = A.alloc(8, U32, "ix8")
    A0 = A.alloc(NE, F32, "A0")
    A1 = A.alloc(NE, F32, "A1")
    AA = A.alloc(NE, F32, "AA")
    Cc = A.alloc(NE, F32, "Cc")
    x1_bufs = [Buf("x1scr%d" % i) for i in range(NT)]
    print("arena after phase1 alloc:", A.off, "of", A.nbytes)

    bT, bA, bB, bC, bD, bE, bF, bG = bk
    bT_b8 = v8(bT.ap.bitcast(BF16))
    bD_b4 = v4(bD.ap.bitcast(BF16)[:, 0:512])
    bC4 = v4(bC.ap)
    bD4 = v4(bD.ap)
    bE4 = v4(bE.ap)
    S4 = v4(Sst.ap)
    Sb4 = v4(Sbf.ap)
    xT_v = v8(xT.ap)
    logf4, cum4, ekl4, khT4 = (v4(t.ap) for t in (logf, cum, ekl, khT))
    mask3 = mask_f.ap.unsqueeze(1).to_broadcast([P, 4, P])

    _steps = {}

    def interleave(*gens, names=None):
        items = [[g, n, 0, 0.0] for g, n in zip(gens, names or [None] * len(gens)) if g is not None]
        lens = [_steps.get(it[1]) for it in items]
        prop = False
        mx = max(lens) if prop else 1
        while items:
            for it in list(items):
                if prop:
                    it[3] += _steps[it[1]] / mx
                    k = int(it[3])
                    it[3] -= k
                else:
                    k = 1
                for _ in range(k):
                    try:
                        next(it[0])
                        it[2] += 1
                    except StopIteration:
                        if it[1] is not None:
                            _steps[it[1]] = max(it[2], 1)
                        items.remove(it)
                        break

    def load_xT(src_rows, xi):
        dma("sp", xi.ap, src_rows, [], [xi])
        tt("pool", xb.ap, xi.ap, shr1_b.ap, ALU.add, [xi, shr1_b], [xb])
        yield
        for kc in range(8):
            tr(bT_b8[:, kc, :], xb.ap[:, kc * P:(kc + 1) * P], identb.ap, [xb, identb], [bT])
        cp("act", xT.ap, bT.ap.bitcast(BF16), [bT], [xT])
        yield

    def proj_fm(bank, col0):
        b4 = v4(bank.ap)
        for hb in range(4):
            for kc in range(8):
                mm(b4[:, hb, :], w_in_v[:, kc, col0 + hb * P:col0 + (hb + 1) * P], xT_v[:, kc, :],
                   kc == 0, kc == 7, [w_in_b, xT], [bank])
            yield

    def proj_tm(bank, col0):
        for kc in range(8):
            mm(bank.ap, xT_v[:, kc, :], w_in_v[:, kc, col0:col0 + 512], kc == 0, kc == 7, [xT, w_in_b], [bank])
            if kc % 2 == 1:
                yield

    def gate_state(sg, valid_col, kh, dec):
        sg4 = v4(sg.ap)
        tt("dve", logf4, sg4, oml.ap.unsqueeze(2).to_broadcast([P, 4, P]), ALU.mult, [sg, oml], [logf])
        act(logf.ap, logf.ap, AF.Ln, [logf], [logf], bias=C_ONE, scale=-1.0)
        yield
        for hd in range(4):
            S.op("dve", (lambda hd_: (lambda h: h.tensor_tensor_scan(
                out=cum4[:, hd_, :], data0=ones_f.ap, data1=logf4[:, hd_, :], initial=0.0,
                op0=ALU.mult, op1=ALU.add)))(hd), [logf, ones_f], [cum])
        yield
        tt("dve", ekl4, cum4[:, :, P - 1:P].to_broadcast([P, 4, P]), cum4, ALU.subtract, [cum], [ekl])
        act(ekl.ap, ekl.ap, AF.Exp, [ekl], [ekl])
        act(dec.ap, cum4[:, :, P - 1], AF.Exp, [cum], [dec])
        yield
        if valid_col is None:
            tt("dve", khT.ap, ekl.ap, sg.ap, ALU.mult, [ekl, sg], [khT])
        else:
            stt(khT.ap, ekl.ap, valid_col, sg.ap, ALU.mult, ALU.mult, [ekl, sg, validt], [khT])
        yield
        for hd in range(4):
            tr(bD_b4[:, hd, :], khT4[:, hd, :], identb.ap, [khT, identb], [bD])
        cp("act", kh.ap, bD.ap.bitcast(BF16)[:, 0:512], [bD], [kh])
        yield

    def state_update(v_tm, kh, dec):
        kh4 = v4(kh.ap)
        for hd in range(4):
            mm(bC4[:, hd, :], kh4[:, hd, :], v_tm.ap[:, hd * P:(hd + 1) * P], True, True, [kh, v_tm], [bC])
        yield
        for hd in range(4):
            stt(S4[:, hd, :], S4[:, hd, :], dec.ap[:, hd:hd + 1], bC4[:, hd, :], ALU.mult, ALU.add,
                [Sst, dec, bC], [Sst])
        yield

    def layernorm_tm(src, width, eps_ap, g_b, b_b, dst, stats):
        st6_, mv_, r_ = stats
        nch = width // 512
        for c in range(nch):
            S.op("dve", (lambda c_: (lambda h: h.bn_stats(out=st6_.ap[:, c_ * 6:(c_ + 1) * 6],
                                                          in_=src.ap[:, c_ * 512:(c_ + 1) * 512])))(c), [src], [st6_])
        S.op("dve", lambda h: h.bn_aggr(out=mv_.ap, in_=st6_.ap[:, 0:6 * nch]), [st6_], [mv_])
        yield
        act(r_.ap[:, 0:1], mv_.ap[:, 1:2], AF.Ln, [mv_], [r_], bias=eps_ap)
        act(r_.ap[:, 0:1], r_.ap[:, 0:1], AF.Exp, [r_], [r_], scale=-0.5)
        yield
        stt(r_.ap[:, 1:2], mv_.ap[:, 0:1], -1.0, r_.ap[:, 0:1], ALU.mult, ALU.mult, [mv_, r_], [r_])
        act(src.ap, src.ap, AF.Identity, [src, r_], [src], bias=r_.ap[:, 1:2], scale=r_.ap[:, 0:1])
        yield
        tt("pool", src.ap, src.ap, g_b.ap, ALU.mult, [src, g_b], [src])
        yield
        tt("dve", dst.ap, src.ap, b_b.ap, ALU.add, [src, b_b], [dst])
        yield

    def PF(j):
        s = j % 2
        yield from load_xT(x_prev[j * P:(j + 1) * P, :], xin[j % 4])
        yield from proj_fm(bA, 512)
        act(tmpA.ap, bA.ap, AF.Exp, [bA], [tmpA])
        act(tmpA.ap, tmpA.ap, AF.Ln, [tmpA], [tmpA], bias=C_ONE)
        act(sg_[s].ap, tmpA.ap, AF.Exp, [tmpA], [sg_[s]], scale=-1.0)
        yield
        yield from proj_tm(bB, 1024)
        cp("act", vtm_[s].ap, bB.ap, [bB], [vtm_[s]])
        yield

    def PM(j):
        s = j % 2
        yield from gate_state(sg_[s], validt.ap[:, j:j + 1], kh_[s], dec_[s])
        yield from state_update(vtm_[s], kh_[s], dec_[s])

    def zero_gen():
        for r in range(NE * CAP // P):
            dma("sp", xpad[r * P:(r + 1) * P, :], zt.ap, [zt], [Buf()])
            yield

    zg = zero_gen()
    for j in range(NPREV + 1):
        interleave(PF(j) if j < NPREV else None, PM(j - 1) if j >= 1 else None, names=["PF", "PM"])
        for _ in range(2):
            next(zg, None)
    for _ in zg:
        pass
    cp("act", Sbf.ap, Sst.ap, [Sst], [Sbf])
    if dbg:
        dma("sp", dbg_S, Sst.ap, [Sst], [Buf()])
        for jj, t_ in enumerate([shr1_b, g1p_b, sc2p_b, sh2_b, g2p_b]):
            dma("sp", dbg_par[:, jj * D:(jj + 1) * D], t_.ap, [t_], [Buf()])
    S.barrier()

    def FE(i):
        s = i % 2
        yield from load_xT(x_own[i * P:(i + 1) * P, :], xin[i % 4])
        yield from proj_fm(bA, 2048)
        act(tu_[s].ap, bA.ap, AF.Gelu, [bA], [tu_[s]])
        yield from proj_tm(bB, 2560)
        act(gv_[s].ap, bB.ap, AF.Gelu, [bB], [gv_[s]])
        yield

    def sigmoid_exp(dst, bank, sign):
        act(dst.ap, bank.ap, AF.Exp, [bank], [dst], scale=-sign)
        act(dst.ap, dst.ap, AF.Ln, [dst], [dst], bias=C_ONE)
        act(dst.ap, dst.ap, AF.Exp, [dst], [dst], scale=-1.0)

    def FX(i):
        s = i % 2
        yield from proj_fm(bA, 0)
        sigmoid_exp(tmpA, bA, 1.0)
        yield
        tt("dve", qs_[s].ap, bA.ap, tmpA.ap, ALU.mult, [bA, tmpA], [qs_[s]])
        yield from proj_fm(bB, 1536)
        sigmoid_exp(tmpB, bB, 1.0)
        yield
        tt("dve", gs_[i % 3].ap, bB.ap, tmpB.ap, ALU.mult, [bB, tmpB], [gs_[i % 3]])
        yield from proj_fm(bA, 512)
        sigmoid_exp(sg_[s], bA, -1.0)
        yield
        yield from proj_tm(bB, 1024)
        cp("act", vtm_[i % 3].ap, bB.ap, [bB], [vtm_[i % 3]])
        yield

    def M1(i):
        s = i % 2
        tu, gv, ybT = tu_[s], gv_[s], ybT_[i % 3]
        bT4 = v4(bT.ap)
        yield from layernorm_tm(gv, 512, C_EPS, lng_b, lnb_b, vn, stM)
        for g in range(4):
            mm(bT4[:, g, :], vn.ap[:, g * P:(g + 1) * P], v4(WcT.ap)[:, g, :], True, True, [vn, WcT], [bT])
        tt("dve", tmpC.ap, bT.ap, sgb_b.ap, ALU.add, [bT, sgb_b], [tmpC])
        yield
        tt("dve", ybT.ap, tu.ap, tmpC.ap, ALU.mult, [tu, tmpC], [ybT])
        yield

    def M2a(i):
        s = i % 2
        sg, qs, kh, dec, scT, qh = sg_[s], qs_[s], kh_[s], dec_[s], scT_[s], qh_[s]
        sg4, qs4, qt4, scT4, qh4 = (v4(t.ap) for t in (sg, qs, qt, scT, qh))
        yield from gate_state(sg, None, kh, dec)
        ts("dve", ncol.ap.rearrange("p (h i) -> p h i", h=4)[:, :, 1:4],
           cum.ap.rearrange("p (h b j) -> p h b j", h=4, b=4)[:, :, 0:3, 31], -1.0, None, ALU.mult, None, [cum], [ncol])
        yield
        n = 0
        for hd in range(4):
            for I in range(4):
                e_q = eq[n % 4]
                e_k = ek[n % 4]
                k_t = kt[n % 4]
                n += 1
                M_ = 32 * (I + 1)
                bq = C_ZERO if I == 0 else ncol.ap[:, hd * 4 + I:hd * 4 + I + 1]
                bkk = C_ZERO if I == 0 else cum4[:, hd, 32 * I - 1:32 * I]
                act(e_q.ap, cum4[:, hd, 32 * I:32 * I + 32], AF.Exp, [cum, ncol], [e_q], bias=bq)
                act(e_k.ap[:, 0:M_], cum4[:, hd, 0:M_], AF.Exp, [cum], [e_k], bias=bkk, scale=-1.0)
                stt(qt4[:, hd, 32 * I:32 * I + 32], e_q.ap, oml.ap[:, hd:hd + 1], qs4[:, hd, 32 * I:32 * I + 32],
                    ALU.mult, ALU.mult, [e_q, oml, qs], [qt])
                tt("pool", k_t.ap[:, 0:M_], e_k.ap[:, 0:M_], sg4[:, hd, 0:M_], ALU.mult, [e_k, sg], [k_t])
                mm(bD4[0:M_, hd, 32 * I:32 * I + 32], k_t.ap[:, 0:M_], qt4[:, hd, 32 * I:32 * I + 32], True, True,
                   [k_t, qt], [bD])
                yield
        tt("dve", scT4, bD4, mask3, ALU.mult, [bD, mask_f], [scT])
        act(ekl.ap, cum.ap, AF.Exp, [cum], [ekl])
        yield
        for hd in range(4):
            stt(qh4[:, hd, :], ekl4[:, hd, :], oml.ap[:, hd:hd + 1], qs4[:, hd, :], ALU.mult, ALU.mult,
                [ekl, oml, qs], [qh])
        yield

    def M2b(i):
        s = i % 2
        v_tm, gs, kh, dec, scT, qh, yaT = vtm_[i % 3], gs_[i % 3], kh_[s], dec_[s], scT_[s], qh_[s], yaT_[s]
        scT4, qh4 = v4(scT.ap), v4(qh.ap)
        for hd in range(4):
            mm(bE4[:, hd, :], v_tm.ap[:, hd * P:(hd + 1) * P], scT4[:, hd, :], True, False, [v_tm, scT], [bE])
            mm(bE4[:, hd, :], Sb4[:, hd, :], qh4[:, hd, :], False, True, [Sbf, qh], [bE])
        yield
        yield from state_update(v_tm, kh, dec)
        cp("act", Sbf.ap, Sst.ap, [Sst], [Sbf])
        act(osq.ap, bE.ap, AF.Square, [bE], [osq])
        yield
        mm(bC.ap, ones_b.ap, osq.ap, True, True, [ones_b, osq], [bC])
        yield
        act(rstd.ap, bC.ap, AF.Ln, [bC], [rstd], bias=C_REPS, scale=1.0 / 128.0)
        yield
        act(rstd.ap, rstd.ap, AF.Exp, [rstd], [rstd], scale=-0.5)
        yield
        stt(rstd.ap, bE.ap, gnw.ap[:, 0:1], rstd.ap, ALU.mult, ALU.mult, [bE, gnw, rstd], [rstd])
        yield
        tt("pool", yaT.ap, rstd.ap, gs.ap, ALU.mult, [rstd, gs], [yaT])
        yield

    def T1(i):
        lg = lg_[i % 2]
        s = i % 2
        xi = xin[i % 4]
        yaT4, ybT4 = v4(yaT_[s].ap), v4(ybT_[i % 3].ap)
        for half, pb in ((0, bF), (1, bG)):
            for k in range(8):
                lhs = yaT4[:, k, :] if k < 4 else ybT4[:, k - 4, :]
                mm(pb.ap, lhs, w_out_v[:, k, half * 512:(half + 1) * 512], k == 0, k == 7,
                   [yaT_[s], ybT_[i % 3], w_out_b], [pb])
                if k % 4 == 3:
                    yield
        for half, pb in ((0, bF), (1, bG)):
            tt("dve", rr.ap[:, half * 512:(half + 1) * 512], pb.ap, g1p_b.ap[:, half * 512:(half + 1) * 512],
               ALU.mult, [pb, g1p_b], [rr])
            yield
        stt(rr.ap, xi.ap, ALPHA, rr.ap, ALU.mult, ALU.add, [xi, rr], [rr])
        yield
        yield from layernorm_tm(rr, D, C_EPS, ln1g_b, ln1b_b, x1, stT)
        dma("sp", x1scr[i * P:(i + 1) * P, :], x1.ap, [x1], [x1_bufs[i]])
        hb_ = h2b[i % 2]
        tt("pool", h2.ap, x1.ap, sc2p_b.ap, ALU.mult, [x1, sc2p_b], [h2])
        yield
        tt("dve", h2.ap, h2.ap, sh2_b.ap, ALU.add, [h2, sh2_b], [h2])
        cp("act", hb_.ap, h2.ap, [h2], [hb_])
        yield
        for half, pb in ((0, bF), (1, bG)):
            for k4 in range(4):
                kc = half * 4 + k4
                tr(v4(pb.ap)[:, k4, :], h2.ap[:, kc * P:(kc + 1) * P], identf.ap, [h2, identf], [pb])
            cp("act", h2T.ap[:, half * 512:(half + 1) * 512], pb.ap, [pb], [h2T])
            yield
        h2T_v = v8(h2T.ap)
        for kc in range(8):
            mm(bF.ap[:, 0:36], h2T_v[:, kc, :], RW_v[:, kc, :], kc == 0, kc == 7, [h2T, RW], [bF])
        yield
        tt("dve", lg.ap, bF.ap[:, 0:36], rb_b.ap, ALU.add, [bF, rb_b], [lg])
        yield

    def T2(i):
        lg = lg_[i % 2]
        hb_ = h2b[i % 2]
        red(sm.ap[:, 0:1], lg.ap[:, 0:4], ALU.max, [lg], [sm])
        ts("dve", sm.ap[:, 1:2], sm.ap[:, 0:1], -1.0, None, ALU.mult, None, [sm], [sm])
        yield
        ts("dve", sm.ap[:, 4:8], lg.ap[:, 0:4], sm.ap[:, 0:1], None, ALU.is_equal, None, [lg, sm], [sm])
        act(sm.ap[:, 12:16], lg.ap[:, 0:4], AF.Exp, [lg, sm], [sm], bias=sm.ap[:, 1:2])
        yield
        red(sm.ap[:, 2:3], sm.ap[:, 12:16], ALU.add, [sm], [sm])
        S.op("dve", lambda h: h.reciprocal(out=sm.ap[:, 3:4], in_=sm.ap[:, 2:3]), [sm], [sm])
        yield
        ts("dve", sm.ap[:, 8:12], sm.ap[:, 4:8], 1.0, 1e30, ALU.subtract, ALU.mult, [sm], [sm])
        for g in range(4):
            ts("dve", ml.ap[:, g * 8:(g + 1) * 8], lg.ap[:, 4 + g * 8:4 + (g + 1) * 8], sm.ap[:, 8 + g:9 + g], None,
               ALU.add, None, [lg, sm], [ml])
        yield
        S.op("dve", lambda h: h.max(out=mx8.ap, in_=ml.ap), [ml], [mx8])
        S.op("dve", lambda h: h.max_index(out=ix8.ap, in_max=mx8.ap, in_values=ml.ap), [ml, mx8], [ix8])
        yield
        tt("dve", sm.ap[:, 16:17], mx8.ap[:, 1:2], mx8.ap[:, 0:1], ALU.subtract, [mx8], [sm])
        act(sm.ap[:, 17:18], sm.ap[:, 16:17], AF.Exp, [sm], [sm])
        yield
        ts("dve", sm.ap[:, 18:19], sm.ap[:, 17:18], 1.0, None, ALU.add, None, [sm], [sm])
        S.op("dve", lambda h: h.reciprocal(out=sm.ap[:, 19:20], in_=sm.ap[:, 18:19]), [sm], [sm])
        yield
        tt("dve", wgt0.ap[:, i:i + 1], sm.ap[:, 3:4], sm.ap[:, 19:20], ALU.mult, [sm], [wgt0])
        tt("dve", wgt1.ap[:, i:i + 1], wgt0.ap[:, i:i + 1], sm.ap[:, 17:18], ALU.mult, [sm, wgt0], [wgt1])
        yield
        cp("dve", sm.ap[:, 20:22], ix8.ap[:, 0:2], [ix8], [sm])
        ts("dve", A0.ap, eidx.ap, sm.ap[:, 20:21], None, ALU.is_equal, None, [eidx, sm], [A0])
        ts("dve", A1.ap, eidx.ap, sm.ap[:, 21:22], None, ALU.is_equal, None, [eidx, sm], [A1])
        tt("dve", AA.ap, A0.ap, A1.ap, ALU.add, [A0, A1], [AA])
        yield
        mm(bT.ap[:, 0:32], ustrict.ap, AA.ap, True, True, [ustrict, AA], [bT])
        mm(bT.ap[:, 64:96], ones_f.ap, AA.ap, True, True, [ones_f, AA], [bT])
        tt("dve", Cc.ap, bT.ap[:, 0:32], cntbase.ap, ALU.add, [bT, cntbase], [Cc])
        tt("dve", cntbase.ap, cntbase.ap, bT.ap[:, 64:96], ALU.add, [cntbase, bT], [cntbase])
        yield
        ts("dve", Cc.ap, Cc.ap, float(CAP - 1), None, ALU.min, None, [Cc], [Cc])
        tt("dve", Cc.ap, Cc.ap, ebase.ap, ALU.add, [Cc, ebase], [Cc])
        yield
        tt("dve", A0.ap, A0.ap, Cc.ap, ALU.mult, [A0, Cc], [A0])
        tt("dve", A1.ap, A1.ap, Cc.ap, ALU.mult, [A1, Cc], [A1])
        red(sm.ap[:, 22:23], A0.ap, ALU.add, [A0], [sm])
        red(sm.ap[:, 23:24], A1.ap, ALU.add, [A1], [sm])
        yield
        ts("dve", sm.ap[:, 22:24], sm.ap[:, 22:24], 0.0, float(NE * CAP - 1), ALU.max, ALU.min, [sm], [sm])
        cp("dve", slot0.ap[:, i:i + 1], sm.ap[:, 22:23], [sm], [slot0])
        cp("dve", slot1.ap[:, i:i + 1], sm.ap[:, 23:24], [sm], [slot1])
        yield
        for sl in (slot0, slot1):
            S.dma("pool", (lambda sl_, hb__, ii: (lambda h: h.indirect_dma_start(
                out=xpad, out_offset=bass.IndirectOffsetOnAxis(ap=sl_.ap[:, ii:ii + 1], axis=0),
                in_=hb__.ap, in_offset=None)))(sl, hb_, i), [hb_, sl], [Buf()])
        yield

    def stage(g, k):
        return g(k) if 0 <= k < NT else None

    for t in range(NT + 5):
        if t < NT:
            interleave(FE(t))
        interleave(stage(M2a, t - 1), stage(FX, t), stage(T1, t - 3), stage(M2b, t - 2), stage(M1, t - 1),
                   stage(T2, t - 4), names=["M2a", "FX", "T1", "M2b", "M1", "T2"])

    if dbg:
        dbgm = A.alloc(4 * NT + NE, F32, "dbgm")
        cp("dve", dbgm.ap[:, 0:NT], slot0.ap, [slot0], [dbgm])
        cp("dve", dbgm.ap[:, NT:2 * NT], slot1.ap, [slot1], [dbgm])
        cp("dve", dbgm.ap[:, 2 * NT:3 * NT], wgt0.ap, [wgt0], [dbgm])
        cp("dve", dbgm.ap[:, 3 * NT:4 * NT], wgt1.ap, [wgt1], [dbgm])
        cp("dve", dbgm.ap[:, 4 * NT:4 * NT + NE], cntbase.ap, [cntbase], [dbgm])
        dma("sp", dbg_misc, dbgm.ap, [dbgm], [Buf()])
    S.barrier()
    A.release(m_persist)
    if stop == "p1":
        S.run()
        for cm in reversed(bank_cms):
            cm.__exit__(None, None, None)
        arena_cm.__exit__(None, None, None)
        S.close()
        return nc

    wu = [A.alloc(8 * 1024, BF16, "wu%d" % i) for i in range(3)]
    wd = [A.alloc(4 * 1024, BF16, "wd%d" % i) for i in range(3)]
    Xe = [[A.alloc(D, BF16, "Xe%d_%d" % (i, s)) for s in range(NSB)] for i in range(2)]
    XeT = [A.alloc(8 * CAP, BF16, "XeT%d" % i) for i in range(2)]
    sgt = [A.alloc(CAP, F32, "sgt%d" % i) for i in range(2)]
    gt_ = [A.alloc(CAP, F32, "gt%d" % i) for i in range(2)]
    actT = [A.alloc(4 * CAP, BF16, "actT%d" % i) for i in range(2)]
    Yt = [A.alloc(D, BF16, "Yt%d" % i) for i in range(6)]
    ypad_b = Buf("ypad")
    print("arena after phase2 alloc:", A.off, "of", A.nbytes)
    w_up_v = w_up.rearrange("e (k p) n -> e p k n", p=P)
    w_down_v = w_down.rearrange("e (k p) n -> e p k n", p=P)
    stg = [A.alloc(2 * 1024, F32, "stg%d" % i) for i in range(4)]
    print("arena after phase2 alloc (+staging):", A.off, "of", A.nbytes)
    stg_n = [0]

    def wload(e):
        bw = e % 3
        wuv = wu[bw].ap.rearrange("p (k n) -> p k n", k=8)
        wdv = wd[bw].ap.rearrange("p (k n) -> p k n", k=4)
        chunks = [(wuv[:, 2 * k:2 * k + 2, :], w_up_v[e, :, 2 * k:2 * k + 2, :], wu[bw]) for k in range(4)] + \
                 [(wdv[:, 2 * k:2 * k + 2, :], w_down_v[e, :, 2 * k:2 * k + 2, :], wd[bw]) for k in range(2)]
        pend = []
        for ci, (dst, src, dtl) in enumerate(chunks):
            st = stg[stg_n[0] % 4]
            stg_n[0] += 1
            dma("sp", st.ap.rearrange("p (k n) -> p k n", k=2), src, [], [st])
            pend.append((dst, st, dtl, ci))
            yield
            if len(pend) > 2:
                d_, s_, t_, c_ = pend.pop(0)
                cp("act" if c_ % 2 == 0 else "dve", d_, s_.ap.rearrange("p (k n) -> p k n", k=2), [s_], [t_])
                yield
        for d_, s_, t_, c_ in pend:
            cp("act" if c_ % 2 == 0 else "dve", d_, s_.ap.rearrange("p (k n) -> p k n", k=2), [s_], [t_])
            yield

    yn = [0]

    def compute(e):
        bu = e % 2
        bw = e % 3
        wuv = wu[bw].ap.rearrange("p (k n) -> p k n", k=8)
        wdv = wd[bw].ap.rearrange("p (k n) -> p k n", k=4)
        xet = XeT[bu]
        xetv = xet.ap.rearrange("p (k s) -> p k s", k=8)
        for sb_ in range(NSB):
            xe = Xe[bu][sb_]
            dma("sp", xe.ap, xpad[e * CAP + sb_ * P:e * CAP + (sb_ + 1) * P, :], [xpad_b], [xe])
            pb = bk[sb_ % 2]
            pb8 = v8(pb.ap.bitcast(BF16))
            for kc in range(8):
                tr(pb8[:, kc, :], xe.ap[:, kc * P:(kc + 1) * P], identb.ap, [xe, identb], [pb])
            cp("act" if sb_ % 2 == 0 else "dve", xetv[:, :, sb_ * P:(sb_ + 1) * P], pb8, [pb], [xet])
            yield
        at = actT[bu]
        atv = at.ap.rearrange("p (k s) -> p k s", k=4)
        for cb in range(4):
            pg = bk[2 + (cb % 2)]
            pu = bk[4 + (cb % 2)]
            for kc in range(8):
                mm(pg.ap[:, 0:CAP], wuv[:, kc, cb * P:(cb + 1) * P], xetv[:, kc, :], kc == 0, kc == 7, [wu[bw], xet], [pg])
            yield
            for kc in range(8):
                mm(pu.ap[:, 0:CAP], wuv[:, kc, 512 + cb * P:512 + (cb + 1) * P], xetv[:, kc, :], kc == 0, kc == 7,
                   [wu[bw], xet], [pu])
            yield
            s_ = sgt[cb % 2]
            g_ = gt_[cb % 2]
            act(s_.ap, pg.ap[:, 0:CAP], AF.Sigmoid, [pg], [s_])
            tt("dve", g_.ap, pg.ap[:, 0:CAP], s_.ap, ALU.mult, [pg, s_], [g_])
            tt("dve", atv[:, cb, :], pu.ap[:, 0:CAP], g_.ap, ALU.mult, [pu, g_], [at])
            yield
        for sb_ in range(NSB):
            y = Yt[yn[0] % 6]
            yn[0] += 1
            for half in range(2):
                pb = bk[6 + half]
                for hc in range(4):
                    mm(pb.ap, atv[:, hc, sb_ * P:(sb_ + 1) * P], wdv[:, hc, half * 512:(half + 1) * 512],
                       hc == 0, hc == 3, [at, wd[bw]], [pb])
                cp("act" if half == 0 else "dve", y.ap[:, half * 512:(half + 1) * 512], pb.ap, [pb], [y])
                yield
            dma("sp", ypad[e * CAP + sb_ * P:e * CAP + (sb_ + 1) * P, :], y.ap, [y], [Buf()])

    interleave(wload(0))
    for e in range(NE):
        interleave(compute(e), wload(e + 1) if e + 1 < NE else None, names=["cmp", "wl"])

    if dbg:
        for r in range(NE * CAP // P):
            dma("sp", dbg_ypad[r * P:(r + 1) * P, :], ypad[r * P:(r + 1) * P, :], [ypad_b], [Buf()])
            dma("sp", dbg_xpad[r * P:(r + 1) * P, :], xpad[r * P:(r + 1) * P, :], [xpad_b], [Buf()])
        dma("sp", dbg_wu, wu[1].ap, [wu[1]], [Buf()])
    S.barrier()
    A.release(m_persist)

    NW3 = 4
    Y0 = [A.alloc(D, BF16, "Y0_%d" % i) for i in range(NW3)]
    Y1 = [A.alloc(D, BF16, "Y1_%d" % i) for i in range(NW3)]
    x1t = [A.alloc(D, F32, "x1t%d" % i) for i in range(NW3)]
    mt = [A.alloc(D, F32, "mt%d" % i) for i in range(NW3)]
    ot = [A.alloc(D, F32, "ot%d" % i) for i in range(NW3)]
    st3 = [(A.alloc(12, F32, "bst6_3%d" % i), A.alloc(2, F32, "mv_3%d" % i), A.alloc(4, F32, "rs_3%d" % i))
           for i in range(NW3)]

    def P3(i):
        p2 = i % NW3
        for sl, yt in ((slot0, Y0[p2]), (slot1, Y1[p2])):
            S.dma("pool", (lambda sl_, yt_, ii: (lambda h: h.indirect_dma_start(
                out=yt_.ap, out_offset=None, in_=ypad,
                in_offset=bass.IndirectOffsetOnAxis(ap=sl_.ap[:, ii:ii + 1], axis=0))))(sl, yt, i), [sl], [yt])
        dma("sp", x1t[p2].ap, x1scr[i * P:(i + 1) * P, :], [x1_bufs[i]], [x1t[p2]])
        yield
        m = mt[p2]
        act(m.ap, Y0[p2].ap, AF.Identity, [Y0[p2], wgt0], [m], scale=wgt0.ap[:, i:i + 1])
        yield
        stt(m.ap, Y1[p2].ap, wgt1.ap[:, i:i + 1], m.ap, ALU.mult, ALU.add, [Y1[p2], wgt1, m], [m])
        yield
        tt("pool", m.ap, m.ap, g2p_b.ap, ALU.mult, [m, g2p_b], [m])
        yield
        stt(m.ap, x1t[p2].ap, ALPHA, m.ap, ALU.mult, ALU.add, [x1t[p2], m], [m])
        yield
        yield from layernorm_tm(m, D, C_EPS, ln2g_b, ln2b_b, ot[p2], st3[p2])
        dma("sp", out[i * P:(i + 1) * P, :], ot[p2].ap, [ot[p2]], [Buf()])
        yield

    for i in range(0, NT, NW3):
        interleave(*[P3(i + w) for w in range(NW3)])
    S.barrier(["sp"])
    S.run()
    for cm in reversed(bank_cms):
        cm.__exit__(None, None, None)
    arena_cm.__exit__(None, None, None)
    S.close()
    return nc


def kernel(x, c, w_ada, b_ada, w_in, lb_logits, hg_norm_w, sg_ln_g, sg_ln_b, sg_w, sg_b,
           w_out, ln1_g, ln1_b, router_group_w, router_group_b, router_expert_w,
           router_expert_b, w_up, w_down, ln2_g, ln2_b):
    f = lambda a: np.ascontiguousarray(np.asarray(a, dtype=np.float32))
    x = f(x); c = f(c)
    B, SEQ, _ = x.shape
    QT = SEQ // 4
    rw = np.concatenate([f(router_group_w)[0], f(router_expert_w)[0].transpose(1, 0, 2).reshape(D, 32)], axis=1)
    rb = np.concatenate([f(router_group_b)[0], f(router_expert_b)[0].reshape(32)])[None, :]
    shared = {
        "w_ada": f(w_ada)[0], "b_ada": f(b_ada), "w_in": f(w_in)[0],
        "lbT": f(np.asarray(lb_logits).reshape(2, 4, P).transpose(2, 0, 1).reshape(P, 8)),
        "gnw": f(np.asarray(hg_norm_w).reshape(P, 1)),
        "sg_ln_g": f(sg_ln_g), "sg_ln_b": f(sg_ln_b),
        "sg_wT": f(np.asarray(sg_w)[0].transpose(2, 0, 1)),
        "sg_b": f(np.asarray(sg_b).reshape(1, 512)),
        "w_out": f(w_out)[0], "ln1_g": f(ln1_g), "ln1_b": f(ln1_b),
        "rw": f(rw), "rb": f(rb), "w_up": f(w_up)[0], "w_down": f(w_down)[0],
        "ln2_g": f(ln2_g), "ln2_b": f(ln2_b),
    }
    in_maps = []
    for core in range(8):
        b, q = core // 4, core % 4
        xo = x[b, q * QT:(q + 1) * QT, :]
        xp = np.zeros((NPREV * P, D), np.float32)
        val = np.zeros((P, NPREV), np.float32)
        npv = q * QT
        if npv:
            xp[NPREV * P - npv:, :] = x[b, :npv, :]
            val[:, NPREV - npv // P:] = 1.0
        d = dict(shared)
        d.update({"x_own": np.ascontiguousarray(xo), "x_prev": xp, "valid": val,
                  "c_row": np.ascontiguousarray(c[b:b + 1, :])})
        in_maps.append(d)
    if _DBG:
        return in_maps
    nc = build_nc()
    res = run_bass_kernel_spmd(nc, in_maps, core_ids=list(range(8)))
    outp = np.zeros((B, SEQ, D), np.float32)
    for core in range(8):
        b, q = core // 4, core % 4
        outp[b, q * QT:(q + 1) * QT, :] = np.asarray(res.results[core]["out"], dtype=np.float32)
    return outp
```

```python
import numpy as np
import concourse.bass as bass
import concourse.mybir as mybir
from concourse.bass_utils import run_bass_kernel_spmd

F32 = mybir.dt.float32
BF16 = mybir.dt.bfloat16
I32 = mybir.dt.int32
U32 = mybir.dt.uint32
AF = mybir.ActivationFunctionType
ALU = mybir.AluOpType
AX = mybir.AxisListType

P = 128
D = 1024
NT = 16
NPREV = 48
NE = 32
CAP = 384
NSB = CAP // 128
HID = 512
ALPHA = 2.0 ** 0.25
LN_EPS = 1e-5
RMS_EPS = 1e-6
SAME_ENG_SYNC = True
_DBG = False


class Buf:
    __slots__ = ("name", "writer", "readers")

    def __init__(self, name=""):
        self.name = name
        self.writer = None
        self.readers = []


class Tl:
    __slots__ = ("ap", "b")

    def __init__(self, ap, name=""):
        self.ap = ap
        self.b = Buf(name)


def _b(x):
    return x.b if isinstance(x, Tl) else x


class Sched:
    def __init__(self, nc, n_dma_sems=24, n_sw_sems=64):
        self.nc = nc
        self.n_sw_sems = n_sw_sems
        self.sw_sems = []
        self.sw_next = 0
        self.engs = ["pe", "act", "dve", "pool", "sp"]
        self.prog = {e: [] for e in self.engs}
        self.count = {e: 0 for e in self.engs}
        self.sem = {}
        self.known = {e: {} for e in self.engs}
        self.n_dma_sems = n_dma_sems
        self.dma_sems = []
        self.dma_sem_val = []
        self.dma_rr = 0
        self._cms = []

    def open(self):
        for e in ["pe", "act", "dve", "pool"]:
            cm = self.nc.semaphore("s_" + e)
            self.sem[e] = cm.__enter__()
            self._cms.append(cm)
        for i in range(self.n_dma_sems):
            cm = self.nc.semaphore("s_dma%d" % i)
            self.dma_sems.append(cm.__enter__())
            self.dma_sem_val.append(0)
            self._cms.append(cm)
        for i in range(self.n_sw_sems):
            cm = self.nc.semaphore("s_sw%d" % i)
            self.sw_sems.append(cm.__enter__())
            self._cms.append(cm)

    def close(self):
        for cm in reversed(self._cms):
            cm.__exit__(None, None, None)

    def _semh(self, key):
        if isinstance(key, tuple):
            return self.sw_sems[key[1]] if key[0] == "sw" else self.dma_sems[key[1]]
        return self.sem[key]

    def _collect(self, eng, reads, writes):
        need = {}

        def add(ev, raw):
            if ev is None:
                return
            k, v = ev
            if k == eng:
                if eng == "pe" or not SAME_ENG_SYNC or not raw:
                    return
            if self.known[eng].get(k, 0) >= v:
                return
            if need.get(k, 0) < v:
                need[k] = v
        for b in reads:
            add(_b(b).writer, True)
        for b in writes:
            bb = _b(b)
            add(bb.writer, False)
            for r in bb.readers:
                add(r, False)
        return need

    def _emit_waits(self, eng, need):
        waits = []
        for k, v in need.items():
            self.known[eng][k] = v
            waits.append((self._semh(k), v))
        return waits

    def op(self, eng, fn, reads=(), writes=()):
        need = self._collect(eng, reads, writes)
        waits = self._emit_waits(eng, need)
        self.count[eng] += 1
        idx = self.count[eng]
        sem = self.sem[eng]

        def thunk(h, waits=waits, fn=fn, sem=sem):
            for s, v in waits:
                h.wait_ge(s, v)
            fn(h).then_inc(sem, 1)
        self.prog[eng].append(thunk)
        ev = (eng, idx)
        for b in reads:
            _b(b).readers.append(ev)
        for b in writes:
            bb = _b(b)
            bb.writer = ev
            bb.readers = []
        return ev

    def dma(self, q, fn, reads=(), writes=()):
        if q == "pool":
            return self._dma_sw(fn, reads, writes)
        need = self._collect(q, reads, writes)
        i = self.dma_rr
        self.dma_rr = (self.dma_rr + 1) % self.n_dma_sems
        key = ("dma", i)
        prev = self.dma_sem_val[i]
        if prev > 0 and self.known[q].get(key, 0) < prev:
            need[key] = max(need.get(key, 0), prev)
        waits = self._emit_waits(q, need)
        val = prev + 16
        self.dma_sem_val[i] = val
        sem = self.dma_sems[i]

        def thunk(h, waits=waits, fn=fn, sem=sem):
            for s, v in waits:
                h.wait_ge(s, v)
            fn(h).then_inc(sem, 16)
        self.prog[q].append(thunk)
        ev = (key, val)
        for b in reads:
            _b(b).readers.append(ev)
        for b in writes:
            bb = _b(b)
            bb.writer = ev
            bb.readers = []
        return ev

    def _dma_sw(self, fn, reads, writes):
        q = "pool"
        need = self._collect(q, reads, writes)
        waits = self._emit_waits(q, need)
        i = self.sw_next
        self.sw_next += 1
        assert i < self.n_sw_sems, "out of software-DMA semaphores"
        sem = self.sw_sems[i]

        def thunk(h, waits=waits, fn=fn, sem=sem):
            for s, v in waits:
                h.wait_ge(s, v)
            fn(h).then_inc(sem, 16)
        self.prog[q].append(thunk)
        ev = (("sw", i), 16)
        for b in reads:
            _b(b).readers.append(ev)
        for b in writes:
            bb = _b(b)
            bb.writer = ev
            bb.readers = []
        return ev

    def barrier(self, engs=None):
        for e in (engs or self.engs):
            need = {}
            for o in ["pe", "act", "dve", "pool"]:
                if o != e and self.count[o] > self.known[e].get(o, 0):
                    need[o] = self.count[o]
            for i in range(self.n_dma_sems):
                key = ("dma", i)
                v = self.dma_sem_val[i]
                if v > self.known[e].get(key, 0):
                    need[key] = v
            for i in range(self.sw_next):
                key = ("sw", i)
                if self.known[e].get(key, 0) < 16:
                    need[key] = 16
            waits = self._emit_waits(e, need)

            def thunk(h, waits=waits):
                for s, v in waits:
                    h.wait_ge(s, v)
            self.prog[e].append(thunk)

    def run(self):
        nc = self.nc
        with nc.Block() as block:
            @block.tensor
            def _(h):
                for t in self.prog["pe"]:
                    t(h)

            @block.scalar
            def _(h):
                for t in self.prog["act"]:
                    t(h)

            @block.vector
            def _(h):
                for t in self.prog["dve"]:
                    t(h)

            @block.gpsimd
            def _(h):
                for t in self.prog["pool"]:
                    t(h)

            @block.sync
            def _(h):
                for t in self.prog["sp"]:
                    t(h)


class Arena:
    def __init__(self, ap_f32, nbytes):
        self.ap = ap_f32
        self.nbytes = nbytes
        self.off = 0
        self.peak = 0

    def alloc(self, n, dt, name=""):
        sz = {F32: 4, BF16: 2, I32: 4, U32: 4}[dt] * n
        sz = (sz + 31) // 32 * 32
        assert self.off + sz <= self.nbytes, ("arena overflow", name, self.off, sz, self.nbytes)
        v = self.ap[:, self.off // 4:(self.off + sz) // 4]
        if dt != F32:
            v = v.bitcast(dt)
        self.off += sz
        self.peak = max(self.peak, self.off)
        return Tl(v[:, 0:n], name)

    def mark(self):
        return self.off

    def release(self, m):
        self.off = m


def build_nc(dbg=False, stop=None):
    nc = bass.Bass("TRN2", target_bir_lowering=False)

    def din(name, shape, dt=F32):
        return nc.dram_tensor(name, shape, dt, kind="ExternalInput").ap()

    x_own = din("x_own", [NT * P, D])
    x_prev = din("x_prev", [NPREV * P, D])
    valid_in = din("valid", [P, NPREV])
    c_row = din("c_row", [1, D])
    w_ada = din("w_ada", [D, 6 * D])
    b_ada = din("b_ada", [1, 6 * D])
    w_in = din("w_in", [D, 3072])
    lbT = din("lbT", [P, 8])
    gnw_in = din("gnw", [P, 1])
    sg_ln_g = din("sg_ln_g", [1, 512])
    sg_ln_b = din("sg_ln_b", [1, 512])
    sg_wT = din("sg_wT", [P, 4, P])
    sg_b = din("sg_b", [1, 512])
    w_out = din("w_out", [D, D])
    ln1_g = din("ln1_g", [1, D])
    ln1_b = din("ln1_b", [1, D])
    rw = din("rw", [D, 36])
    rb = din("rb", [1, 36])
    w_up = din("w_up", [NE, P, 8 * 1024])
    w_down = din("w_down", [NE, P, 4 * 1024])
    ln2_g = din("ln2_g", [1, D])
    ln2_b = din("ln2_b", [1, D])
    out = nc.dram_tensor("out", [NT * P, D], F32, kind="ExternalOutput").ap()

    sk = "ExternalOutput" if dbg else "Internal"
    xpad = nc.dram_tensor("xpad", [NE * CAP, D], BF16, kind="Internal").ap()
    ypad = nc.dram_tensor("ypad", [NE * CAP, D], BF16, kind="Internal").ap()
    if dbg:
        dbg_ypad = nc.dram_tensor("dbg_ypad", [NE * CAP, D], BF16, kind="ExternalOutput").ap()
        dbg_xpad = nc.dram_tensor("dbg_xpad", [NE * CAP, D], BF16, kind="ExternalOutput").ap()
        dbg_wu = nc.dram_tensor("dbg_wu", [P, 8 * 1024], BF16, kind="ExternalOutput").ap()
    x1scr = nc.dram_tensor("x1scr", [NT * P, D], F32, kind=sk).ap()
    if dbg:
        dbg_S = nc.dram_tensor("dbg_S", [P, 512], F32, kind="ExternalOutput").ap()
        dbg_misc = nc.dram_tensor("dbg_misc", [P, 4 * NT + NE], F32, kind="ExternalOutput").ap()
        dbg_par = nc.dram_tensor("dbg_par", [P, 5 * D], F32, kind="ExternalOutput").ap()

    S = Sched(nc)
    S.open()
    ARENA_BYTES = 211968
    arena_cm = nc.sbuf_tensor("arena", [P, ARENA_BYTES // 4], F32)
    arena_t = arena_cm.__enter__()
    A = Arena(arena_t[:], ARENA_BYTES)
    bank_cms = [nc.psum_tensor("bank%d" % i, [P, 512], F32) for i in range(8)]
    bk = [Tl(cm.__enter__()[:], "bank%d" % i) for i, cm in enumerate(bank_cms)]

    def act(out_, in_, func, reads, writes, bias=None, scale=None):
        kw = {}
        if bias is not None:
            kw["bias"] = bias
        if scale is not None:
            kw["scale"] = scale
        S.op("act", lambda h: h.activation(out=out_, in_=in_, func=func, **kw), reads, writes)

    def tt(eng, out_, in0, in1, op, reads, writes):
        S.op(eng, lambda h: h.tensor_tensor(out=out_, in0=in0, in1=in1, op=op), reads, writes)

    def ts(eng, out_, in0, s1, s2, op0, op1, reads, writes):
        if op1 is None:
            S.op(eng, lambda h: h.tensor_scalar(out=out_, in0=in0, scalar1=s1, scalar2=None, op0=op0), reads, writes)
        else:
            S.op(eng, lambda h: h.tensor_scalar(out=out_, in0=in0, scalar1=s1, scalar2=s2, op0=op0, op1=op1), reads, writes)

    def stt(out_, in0, scalar, in1, op0, op1, reads, writes):
        S.op("dve", lambda h: h.scalar_tensor_tensor(out=out_, in0=in0, scalar=scalar, in1=in1, op0=op0, op1=op1),
             reads, writes)

    def cp(eng, out_, in_, reads, writes):
        if eng == "act":
            S.op("act", lambda h: h.copy(out=out_, in_=in_), reads, writes)
        else:
            S.op(eng, lambda h: h.tensor_copy(out=out_, in_=in_), reads, writes)

    def mm(out_, lhsT, rhs, start, stop, reads, writes):
        S.op("pe", lambda h: h.matmul(out_, lhsT=lhsT, rhs=rhs, start=start, stop=stop), reads, writes)

    def tr(out_, in_, ident, reads, writes):
        S.op("pe", lambda h: h.transpose(out=out_, in_=in_, identity=ident), reads, writes)

    def dma(q, out_, in_, reads, writes):
        S.dma(q, lambda h: h.dma_start(out=out_, in_=in_), reads, writes)

    def red(out_, in_, op, reads, writes):
        S.op("dve", lambda h: h.tensor_reduce(out=out_, in_=in_, axis=AX.X, op=op), reads, writes)

    def v4(ap):
        return ap.rearrange("p (h t) -> p h t", h=4)

    def v8(ap):
        return ap.rearrange("p (k t) -> p k t", k=8)

    identf = A.alloc(P, F32, "identf")
    identb = A.alloc(P, BF16, "identb")
    mask_f = A.alloc(P, F32, "mask_f")
    ustrict = A.alloc(P, F32, "ustrict")
    ones_f = A.alloc(P, F32, "ones_f")
    ones_b = A.alloc(P, BF16, "ones_b")
    io_pj = A.alloc(P, F32, "io_pj")
    ebase = A.alloc(NE, F32, "ebase")
    eidx = A.alloc(NE, F32, "eidx")
    consts = A.alloc(8, F32, "consts")
    slot0 = A.alloc(NT, I32, "slot0")
    slot1 = A.alloc(NT, I32, "slot1")
    wgt0 = A.alloc(NT, F32, "wgt0")
    wgt1 = A.alloc(NT, F32, "wgt1")
    cntbase = A.alloc(NE, F32, "cntbase")
    ln2g_b = A.alloc(D, F32, "ln2g_b")
    ln2b_b = A.alloc(D, F32, "ln2b_b")
    g2p_b = A.alloc(D, F32, "g2p_b")

    S.op("pool", lambda h: h.iota(io_pj.ap, pattern=[[1, P]], base=0, channel_multiplier=-1,
                                  allow_small_or_imprecise_dtypes=True), [], [io_pj])
    S.op("dve", lambda h: h.tensor_single_scalar(out=identf.ap, in_=io_pj.ap, scalar=0.0, op=ALU.is_equal), [io_pj], [identf])
    S.op("dve", lambda h: h.tensor_single_scalar(out=mask_f.ap, in_=io_pj.ap, scalar=0.0, op=ALU.is_ge), [io_pj], [mask_f])
    S.op("dve", lambda h: h.tensor_single_scalar(out=ustrict.ap, in_=io_pj.ap, scalar=0.0, op=ALU.is_gt), [io_pj], [ustrict])
    cp("dve", identb.ap, identf.ap, [identf], [identb])
    S.op("pool", lambda h: h.memset(ones_f.ap, 1.0), [], [ones_f])
    S.op("pool", lambda h: h.memset(ones_b.ap, 1.0), [], [ones_b])
    S.op("pool", lambda h: h.iota(ebase.ap, pattern=[[CAP, NE]], base=0, channel_multiplier=0,
                                  allow_small_or_imprecise_dtypes=True), [], [ebase])
    S.op("pool", lambda h: h.iota(eidx.ap, pattern=[[1, NE]], base=0, channel_multiplier=0,
                                  allow_small_or_imprecise_dtypes=True), [], [eidx])
    S.op("pool", lambda h: h.memset(cntbase.ap, 0.0), [], [cntbase])
    for j, val in enumerate([1.0, 4 * LN_EPS, LN_EPS, RMS_EPS, 0.0]):
        S.op("pool", (lambda v, jj: (lambda h: h.memset(consts.ap[:, jj:jj + 1], v)))(val, j), [], [consts])
    C_ONE = consts.ap[:, 0:1]
    C_4EPS = consts.ap[:, 1:2]
    C_EPS = consts.ap[:, 2:3]
    C_REPS = consts.ap[:, 3:4]
    C_ZERO = consts.ap[:, 4:5]
    for b_ in bk:
        S.op("dve", (lambda bb: (lambda h: h.memset(bb.ap, 0.0)))(b_), [], [b_])
    dma("sp", ln2g_b.ap, ln2_g.broadcast_to([P, D]), [], [ln2g_b])
    dma("sp", ln2b_b.ap, ln2_b.broadcast_to([P, D]), [], [ln2b_b])

    m_persist = A.mark()
    w_in_b = A.alloc(8 * 3072, BF16, "w_in_b")
    w_out_b = A.alloc(8 * D, BF16, "w_out_b")
    shr1_b = A.alloc(D, F32, "shr1_b")
    g1p_b = A.alloc(D, F32, "g1p_b")
    sc2p_b = A.alloc(D, F32, "sc2p_b")
    sh2_b = A.alloc(D, F32, "sh2_b")
    ln1g_b = A.alloc(D, F32, "ln1g_b")
    ln1b_b = A.alloc(D, F32, "ln1b_b")
    lng_b = A.alloc(512, F32, "lng_b")
    lnb_b = A.alloc(512, F32, "lnb_b")
    WcT = A.alloc(512, BF16, "WcT")
    sgb_b = A.alloc(512, F32, "sgb_b")
    RW = A.alloc(8 * 36, F32, "RW")
    rb_b = A.alloc(36, F32, "rb_b")
    oml = A.alloc(4, F32, "oml")
    noml = A.alloc(4, F32, "noml")
    gnw = A.alloc(1, F32, "gnw")
    Sst = A.alloc(512, F32, "Sst")
    Sbf = A.alloc(512, BF16, "Sbf")
    sc1p = A.alloc(8, F32, "sc1p")
    validt = A.alloc(NPREV, F32, "validt")
    w_in_v = w_in_b.ap.rearrange("p (k n) -> p k n", k=8)
    w_out_v = w_out_b.ap.rearrange("p (k n) -> p k n", k=8)
    RW_v = RW.ap.rearrange("p (k n) -> p k n", k=8)

    zt = A.alloc(D, BF16, "zt")
    m_work = A.mark()
    c_b = A.alloc(D, F32, "c_b")
    sgc = A.alloc(D, F32, "sgc")
    cact = A.alloc(D, F32, "cact")
    CT = A.alloc(D, F32, "CT")
    wst = [A.alloc(8 * 512, F32, "wst%d" % i) for i in range(2)]
    bst = [A.alloc(512, F32, "bst%d" % i) for i in range(2)]
    sec = [A.alloc(D, F32, "sec%d" % i) for i in range(2)]
    lbt = A.alloc(8, F32, "lbt")
    wct_st = A.alloc(512, F32, "wct_st")
    wist = [A.alloc(1536, F32, "wist%d" % i) for i in range(2)]

    dma("sp", c_b.ap, c_row.broadcast_to([P, D]), [], [c_b])
    dma("sp", lbt.ap, lbT, [], [lbt])
    dma("sp", gnw.ap, gnw_in, [], [gnw])
    dma("sp", validt.ap, valid_in, [], [validt])
    act(sgc.ap, c_b.ap, AF.Sigmoid, [c_b], [sgc])
    tt("dve", cact.ap, c_b.ap, sgc.ap, ALU.mult, [c_b, sgc], [cact])
    tt("dve", oml.ap, lbt.ap[:, 4:8], lbt.ap[:, 0:4], ALU.subtract, [lbt], [oml])
    act(oml.ap, oml.ap, AF.Sigmoid, [oml], [oml])
    ts("dve", noml.ap, oml.ap, -1.0, None, ALU.mult, None, [oml], [noml])
    for half in range(2):
        for k4 in range(4):
            kc = half * 4 + k4
            tr(v4(bk[half].ap)[:, k4, :], cact.ap[:, kc * P:(kc + 1) * P], identf.ap, [cact, identf], [bk[half]])
        cp("act", CT.ap[:, half * 512:(half + 1) * 512], bk[half].ap, [bk[half]], [CT])
    CT_v = v8(CT.ap)
    w_ada_v = w_ada.rearrange("(k p) n -> p k n", p=P)

    def ada_section(sidx, dst, plus_one):
        for hh in range(2):
            j = sidx * 2 + hh
            w = wst[j % 2]
            bsl = bst[j % 2]
            dma("sp", w.ap.rearrange("p (k n) -> p k n", k=8), w_ada_v[:, :, j * 512:(j + 1) * 512], [], [w])
            dma("sp", bsl.ap, b_ada[:, j * 512:(j + 1) * 512].broadcast_to([P, 512]), [], [bsl])
            pb = bk[2 + (j % 2)]
            wv = w.ap.rearrange("p (k n) -> p k n", k=8)
            for kc in range(8):
                mm(pb.ap, CT_v[:, kc, :], wv[:, kc, :], kc == 0, kc == 7, [CT, w], [pb])
            tt("dve", dst.ap[:, hh * 512:(hh + 1) * 512], pb.ap, bsl.ap, ALU.add, [pb, bsl], [dst])
        if plus_one:
            ts("dve", dst.ap, dst.ap, 1.0, None, ALU.add, None, [dst], [dst])

    ada_section(0, sec[0], False)
    ada_section(1, sec[1], True)
    S.op("dve", lambda h: h.reciprocal(out=shr1_b.ap, in_=sec[1].ap), [sec[1]], [shr1_b])
    tt("dve", shr1_b.ap, shr1_b.ap, sec[0].ap, ALU.mult, [shr1_b, sec[0]], [shr1_b])
    for half in range(2):
        for k4 in range(4):
            kc = half * 4 + k4
            tr(v4(bk[half].ap)[:, k4, :], sec[1].ap[:, kc * P:(kc + 1) * P], identf.ap, [sec[1], identf], [bk[half]])
        cp("dve", sc1p.ap[:, half * 4:(half + 1) * 4], v4(bk[half].ap)[:, :, 0], [bk[half]], [sc1p])
    for kc in range(8):
        for hh in range(2):
            st_ = wist[(kc * 2 + hh) % 2]
            dma("sp", st_.ap, w_in[kc * P:(kc + 1) * P, hh * 1536:(hh + 1) * 1536], [], [st_])
            act(w_in_v[:, kc, hh * 1536:(hh + 1) * 1536], st_.ap, AF.Identity, [st_, sc1p], [w_in_b],
                scale=sc1p.ap[:, kc:kc + 1])
    for kc in range(8):
        st_ = wist[kc % 2]
        dma("sp", st_.ap[:, 0:D], w_out[kc * P:(kc + 1) * P, :], [], [st_])
        cp("act", w_out_v[:, kc, :], st_.ap[:, 0:D], [st_], [w_out_b])
    dma("sp", wct_st.ap.rearrange("p (g t) -> p g t", g=4), sg_wT, [], [wct_st])
    tt("dve", v4(WcT.ap), v4(wct_st.ap), mask_f.ap.unsqueeze(1).to_broadcast([P, 4, P]), ALU.mult,
       [wct_st, mask_f], [WcT])
    dma("sp", sgb_b.ap, sg_b.broadcast_to([P, 512]), [], [sgb_b])
    dma("sp", lng_b.ap, sg_ln_g.broadcast_to([P, 512]), [], [lng_b])
    dma("sp", lnb_b.ap, sg_ln_b.broadcast_to([P, 512]), [], [lnb_b])
    dma("sp", ln1g_b.ap, ln1_g.broadcast_to([P, D]), [], [ln1g_b])
    dma("sp", ln1b_b.ap, ln1_b.broadcast_to([P, D]), [], [ln1b_b])
    dma("sp", RW_v, rw.rearrange("(k p) n -> p k n", p=P), [], [RW])
    dma("sp", rb_b.ap, rb.broadcast_to([P, 36]), [], [rb_b])
    S.op("pool", lambda h: h.memset(Sst.ap, 0.0), [], [Sst])

    ada_section(2, g1p_b, True)
    ada_section(3, sh2_b, False)
    ada_section(4, sc2p_b, True)
    ada_section(5, g2p_b, True)

    S.op("pool", lambda h: h.memset(zt.ap, 0.0), [], [zt])
    xpad_b = Buf("xpad")

    S.barrier()
    A.release(m_work)

    xin = [A.alloc(D, F32, "xin%d" % i) for i in range(4)]
    xb = A.alloc(D, BF16, "xb")
    xT = A.alloc(D, BF16, "xT")
    tmpA = A.alloc(512, F32, "tmpA")
    tmpB = A.alloc(512, F32, "tmpB")
    tmpC = A.alloc(512, F32, "tmpC")
    sg_ = [A.alloc(512, F32, "sg%d" % i) for i in range(2)]
    vtm_ = [A.alloc(512, BF16, "v_tm%d" % i) for i in range(3)]
    qs_ = [A.alloc(512, BF16, "qs%d" % i) for i in range(2)]
    gs_ = [A.alloc(512, BF16, "gs%d" % i) for i in range(3)]
    tu_ = [A.alloc(512, BF16, "tu%d" % i) for i in range(2)]
    gv_ = [A.alloc(512, F32, "gv%d" % i) for i in range(2)]
    yaT_ = [A.alloc(512, BF16, "yaT%d" % i) for i in range(2)]
    ybT_ = [A.alloc(512, BF16, "ybT%d" % i) for i in range(3)]
    logf = A.alloc(512, F32, "logf")
    cum = A.alloc(512, F32, "cum")
    ncol = A.alloc(16, F32, "ncol")
    ekl = A.alloc(512, F32, "ekl")
    khT = A.alloc(512, BF16, "khT")
    kh_ = [A.alloc(512, BF16, "kh%d" % i) for i in range(2)]
    dec_ = [A.alloc(4, F32, "dec%d" % i) for i in range(2)]
    vn = A.alloc(512, BF16, "vn")
    stM = (A.alloc(12, F32, "bst6M"), A.alloc(2, F32, "mvM"), A.alloc(4, F32, "rsM"))
    stT = (A.alloc(12, F32, "bst6T"), A.alloc(2, F32, "mvT"), A.alloc(4, F32, "rsT"))
    eq = [A.alloc(32, F32, "eq%d" % i) for i in range(4)]
    ek = [A.alloc(P, F32, "ek%d" % i) for i in range(4)]
    qt = A.alloc(512, BF16, "qt")
    kt = [A.alloc(P, BF16, "kt%d" % i) for i in range(4)]
    scT_ = [A.alloc(512, BF16, "scT%d" % i) for i in range(2)]
    qh_ = [A.alloc(512, BF16, "qh%d" % i) for i in range(2)]
    osq = A.alloc(512, BF16, "osq")
    rstd = A.alloc(512, F32, "rstd")
    rr = A.alloc(D, F32, "rr")
    x1 = A.alloc(D, F32, "x1")
    h2 = rr
    h2b = [A.alloc(D, BF16, "h2b%d" % i) for i in range(2)]
    h2T = A.alloc(D, F32, "h2T")
    lg_ = [A.alloc(36, F32, "lg%d" % i) for i in range(2)]
    sm = A.alloc(64, F32, "sm")
    ml = A.alloc(NE, F32, "ml")
    mx8 = A.alloc(8, F32, "mx8")
    ix8 = A.alloc(8, U32, "ix8")
    A0 = A.alloc(NE, F32, "A0")
    A1 = A.alloc(NE, F32, "A1")
    AA = A.alloc(NE, F32, "AA")
    Cc = A.alloc(NE, F32, "Cc")
    x1_bufs = [Buf("x1scr%d" % i) for i in range(NT)]
    print("arena after phase1 alloc:", A.off, "of", A.nbytes)

    bT, bA, bB, bC, bD, bE, bF, bG = bk
    bT_b8 = v8(bT.ap.bitcast(BF16))
    bD_b4 = v4(bD.ap.bitcast(BF16)[:, 0:512])
    bC4 = v4(bC.ap)
    bD4 = v4(bD.ap)
    bE4 = v4(bE.ap)
    S4 = v4(Sst.ap)
    Sb4 = v4(Sbf.ap)
    xT_v = v8(xT.ap)
    logf4, cum4, ekl4, khT4 = (v4(t.ap) for t in (logf, cum, ekl, khT))
    mask3 = mask_f.ap.unsqueeze(1).to_broadcast([P, 4, P])

    def interleave(*gens):
        gens = [g for g in gens if g is not None]
        while gens:
            for g in list(gens):
                try:
                    next(g)
                except StopIteration:
                    gens.remove(g)

    def load_xT(src_rows, xi):
        dma("sp", xi.ap, src_rows, [], [xi])
        tt("pool", xb.ap, xi.ap, shr1_b.ap, ALU.add, [xi, shr1_b], [xb])
        yield
        for kc in range(8):
            tr(bT_b8[:, kc, :], xb.ap[:, kc * P:(kc + 1) * P], identb.ap, [xb, identb], [bT])
        cp("act", xT.ap, bT.ap.bitcast(BF16), [bT], [xT])
        yield

    def proj_fm(bank, col0):
        b4 = v4(bank.ap)
        for hb in range(4):
            for kc in range(8):
                mm(b4[:, hb, :], w_in_v[:, kc, col0 + hb * P:col0 + (hb + 1) * P], xT_v[:, kc, :],
                   kc == 0, kc == 7, [w_in_b, xT], [bank])
            yield

    def proj_tm(bank, col0):
        for kc in range(8):
            mm(bank.ap, xT_v[:, kc, :], w_in_v[:, kc, col0:col0 + 512], kc == 0, kc == 7, [xT, w_in_b], [bank])
            if kc % 2 == 1:
                yield

    def gate_state(sg, valid_col, kh, dec):
        sg4 = v4(sg.ap)
        tt("dve", logf4, sg4, oml.ap.unsqueeze(2).to_broadcast([P, 4, P]), ALU.mult, [sg, oml], [logf])
        act(logf.ap, logf.ap, AF.Ln, [logf], [logf], bias=C_ONE, scale=-1.0)
        yield
        for hd in range(4):
            S.op("dve", (lambda hd_: (lambda h: h.tensor_tensor_scan(
                out=cum4[:, hd_, :], data0=ones_f.ap, data1=logf4[:, hd_, :], initial=0.0,
                op0=ALU.mult, op1=ALU.add)))(hd), [logf, ones_f], [cum])
        yield
        tt("dve", ekl4, cum4[:, :, P - 1:P].to_broadcast([P, 4, P]), cum4, ALU.subtract, [cum], [ekl])
        act(ekl.ap, ekl.ap, AF.Exp, [ekl], [ekl])
        act(dec.ap, cum4[:, :, P - 1], AF.Exp, [cum], [dec])
        yield
        if valid_col is None:
            tt("dve", khT.ap, ekl.ap, sg.ap, ALU.mult, [ekl, sg], [khT])
        else:
            stt(khT.ap, ekl.ap, valid_col, sg.ap, ALU.mult, ALU.mult, [ekl, sg, validt], [khT])
        yield
        for hd in range(4):
            tr(bD_b4[:, hd, :], khT4[:, hd, :], identb.ap, [khT, identb], [bD])
        cp("act", kh.ap, bD.ap.bitcast(BF16)[:, 0:512], [bD], [kh])
        yield

    def state_update(v_tm, kh, dec):
        kh4 = v4(kh.ap)
        for hd in range(4):
            mm(bC4[:, hd, :], kh4[:, hd, :], v_tm.ap[:, hd * P:(hd + 1) * P], True, True, [kh, v_tm], [bC])
        yield
        for hd in range(4):
            stt(S4[:, hd, :], S4[:, hd, :], dec.ap[:, hd:hd + 1], bC4[:, hd, :], ALU.mult, ALU.add,
                [Sst, dec, bC], [Sst])
        yield

    def layernorm_tm(src, width, eps_ap, g_b, b_b, dst, stats):
        st6_, mv_, r_ = stats
        nch = width // 512
        for c in range(nch):
            S.op("dve", (lambda c_: (lambda h: h.bn_stats(out=st6_.ap[:, c_ * 6:(c_ + 1) * 6],
                                                          in_=src.ap[:, c_ * 512:(c_ + 1) * 512])))(c), [src], [st6_])
        S.op("dve", lambda h: h.bn_aggr(out=mv_.ap, in_=st6_.ap[:, 0:6 * nch]), [st6_], [mv_])
        yield
        act(r_.ap[:, 0:1], mv_.ap[:, 1:2], AF.Ln, [mv_], [r_], bias=eps_ap)
        act(r_.ap[:, 0:1], r_.ap[:, 0:1], AF.Exp, [r_], [r_], scale=-0.5)
        yield
        stt(r_.ap[:, 1:2], mv_.ap[:, 0:1], -1.0, r_.ap[:, 0:1], ALU.mult, ALU.mult, [mv_, r_], [r_])
        act(src.ap, src.ap, AF.Identity, [src, r_], [src], bias=r_.ap[:, 1:2], scale=r_.ap[:, 0:1])
        yield
        tt("pool", src.ap, src.ap, g_b.ap, ALU.mult, [src, g_b], [src])
        yield
        tt("dve", dst.ap, src.ap, b_b.ap, ALU.add, [src, b_b], [dst])
        yield

    def PF(j):
        s = j % 2
        yield from load_xT(x_prev[j * P:(j + 1) * P, :], xin[j % 4])
        yield from proj_fm(bA, 512)
        act(tmpA.ap, bA.ap, AF.Exp, [bA], [tmpA])
        act(tmpA.ap, tmpA.ap, AF.Ln, [tmpA], [tmpA], bias=C_ONE)
        act(sg_[s].ap, tmpA.ap, AF.Exp, [tmpA], [sg_[s]], scale=-1.0)
        yield
        yield from proj_tm(bB, 1024)
        cp("act", vtm_[s].ap, bB.ap, [bB], [vtm_[s]])
        yield

    def PM(j):
        s = j % 2
        yield from gate_state(sg_[s], validt.ap[:, j:j + 1], kh_[s], dec_[s])
        yield from state_update(vtm_[s], kh_[s], dec_[s])

    def zero_gen():
        for r in range(NE * CAP // P):
            dma("sp", xpad[r * P:(r + 1) * P, :], zt.ap, [zt], [Buf()])
            yield

    zg = zero_gen()
    for j in range(NPREV + 1):
        interleave(PF(j) if j < NPREV else None, PM(j - 1) if j >= 1 else None)
        for _ in range(2):
            next(zg, None)
    for _ in zg:
        pass
    cp("act", Sbf.ap, Sst.ap, [Sst], [Sbf])
    if dbg:
        dma("sp", dbg_S, Sst.ap, [Sst], [Buf()])
        for jj, t_ in enumerate([shr1_b, g1p_b, sc2p_b, sh2_b, g2p_b]):
            dma("sp", dbg_par[:, jj * D:(jj + 1) * D], t_.ap, [t_], [Buf()])
    S.barrier()

    def FE(i):
        s = i % 2
        yield from load_xT(x_own[i * P:(i + 1) * P, :], xin[i % 4])
        yield from proj_fm(bA, 2048)
        act(tu_[s].ap, bA.ap, AF.Gelu, [bA], [tu_[s]])
        yield from proj_tm(bB, 2560)
        act(gv_[s].ap, bB.ap, AF.Gelu, [bB], [gv_[s]])
        yield

    def sigmoid_exp(dst, bank, sign):
        act(dst.ap, bank.ap, AF.Exp, [bank], [dst], scale=-sign)
        act(dst.ap, dst.ap, AF.Ln, [dst], [dst], bias=C_ONE)
        act(dst.ap, dst.ap, AF.Exp, [dst], [dst], scale=-1.0)

    def FX(i):
        s = i % 2
        yield from proj_fm(bA, 0)
        sigmoid_exp(tmpA, bA, 1.0)
        yield
        tt("dve", qs_[s].ap, bA.ap, tmpA.ap, ALU.mult, [bA, tmpA], [qs_[s]])
        yield from proj_fm(bB, 1536)
        sigmoid_exp(tmpB, bB, 1.0)
        yield
        tt("dve", gs_[i % 3].ap, bB.ap, tmpB.ap, ALU.mult, [bB, tmpB], [gs_[i % 3]])
        yield from proj_fm(bA, 512)
        sigmoid_exp(sg_[s], bA, -1.0)
        yield
        yield from proj_tm(bB, 1024)
        cp("act", vtm_[i % 3].ap, bB.ap, [bB], [vtm_[i % 3]])
        yield

    def M1(i):
        s = i % 2
        tu, gv, ybT = tu_[s], gv_[s], ybT_[i % 3]
        bT4 = v4(bT.ap)
        yield from layernorm_tm(gv, 512, C_EPS, lng_b, lnb_b, vn, stM)
        for g in range(4):
            mm(bT4[:, g, :], vn.ap[:, g * P:(g + 1) * P], v4(WcT.ap)[:, g, :], True, True, [vn, WcT], [bT])
        yield
        tt("dve", tmpC.ap, bT.ap, sgb_b.ap, ALU.add, [bT, sgb_b], [tmpC])
        yield
        tt("dve", ybT.ap, tu.ap, tmpC.ap, ALU.mult, [tu, tmpC], [ybT])
        yield

    def M2a(i):
        s = i % 2
        sg, qs, kh, dec, scT, qh = sg_[s], qs_[s], kh_[s], dec_[s], scT_[s], qh_[s]
        sg4, qs4, qt4, scT4, qh4 = (v4(t.ap) for t in (sg, qs, qt, scT, qh))
        yield from gate_state(sg, None, kh, dec)
        ts("dve", ncol.ap.rearrange("p (h i) -> p h i", h=4)[:, :, 1:4],
           cum.ap.rearrange("p (h b j) -> p h b j", h=4, b=4)[:, :, 0:3, 31], -1.0, None, ALU.mult, None, [cum], [ncol])
        yield
        n = 0
        for hd in range(4):
            for I in range(4):
                e_q = eq[n % 4]
                e_k = ek[n % 4]
                k_t = kt[n % 4]
                n += 1
                M_ = 32 * (I + 1)
                bq = C_ZERO if I == 0 else ncol.ap[:, hd * 4 + I:hd * 4 + I + 1]
                bkk = C_ZERO if I == 0 else cum4[:, hd, 32 * I - 1:32 * I]
                act(e_q.ap, cum4[:, hd, 32 * I:32 * I + 32], AF.Exp, [cum, ncol], [e_q], bias=bq)
                act(e_k.ap[:, 0:M_], cum4[:, hd, 0:M_], AF.Exp, [cum], [e_k], bias=bkk, scale=-1.0)
                stt(qt4[:, hd, 32 * I:32 * I + 32], e_q.ap, oml.ap[:, hd:hd + 1], qs4[:, hd, 32 * I:32 * I + 32],
                    ALU.mult, ALU.mult, [e_q, oml, qs], [qt])
                tt("pool", k_t.ap[:, 0:M_], e_k.ap[:, 0:M_], sg4[:, hd, 0:M_], ALU.mult, [e_k, sg], [k_t])
                mm(bD4[0:M_, hd, 32 * I:32 * I + 32], k_t.ap[:, 0:M_], qt4[:, hd, 32 * I:32 * I + 32], True, True,
                   [k_t, qt], [bD])
                yield
        tt("dve", scT4, bD4, mask3, ALU.mult, [bD, mask_f], [scT])
        act(ekl.ap, cum.ap, AF.Exp, [cum], [ekl])
        yield
        for hd in range(4):
            stt(qh4[:, hd, :], ekl4[:, hd, :], oml.ap[:, hd:hd + 1], qs4[:, hd, :], ALU.mult, ALU.mult,
                [ekl, oml, qs], [qh])
        yield

    def M2b(i):
        s = i % 2
        v_tm, gs, kh, dec, scT, qh, yaT = vtm_[i % 3], gs_[i % 3], kh_[s], dec_[s], scT_[s], qh_[s], yaT_[s]
        scT4, qh4 = v4(scT.ap), v4(qh.ap)
        for hd in range(4):
            mm(bE4[:, hd, :], v_tm.ap[:, hd * P:(hd + 1) * P], scT4[:, hd, :], True, False, [v_tm, scT], [bE])
            mm(bE4[:, hd, :], Sb4[:, hd, :], qh4[:, hd, :], False, True, [Sbf, qh], [bE])
        yield
        yield from state_update(v_tm, kh, dec)
        cp("act", Sbf.ap, Sst.ap, [Sst], [Sbf])
        act(osq.ap, bE.ap, AF.Square, [bE], [osq])
        yield
        mm(bC.ap, ones_b.ap, osq.ap, True, True, [ones_b, osq], [bC])
        yield
        act(rstd.ap, bC.ap, AF.Ln, [bC], [rstd], bias=C_REPS, scale=1.0 / 128.0)
        yield
        act(rstd.ap, rstd.ap, AF.Exp, [rstd], [rstd], scale=-0.5)
        yield
        stt(rstd.ap, bE.ap, gnw.ap[:, 0:1], rstd.ap, ALU.mult, ALU.mult, [bE, gnw, rstd], [rstd])
        yield
        tt("pool", yaT.ap, rstd.ap, gs.ap, ALU.mult, [rstd, gs], [yaT])
        yield

    def T1(i):
        lg = lg_[i % 2]
        s = i % 2
        xi = xin[i % 4]
        yaT4, ybT4 = v4(yaT_[s].ap), v4(ybT_[i % 3].ap)
        for half, pb in ((0, bF), (1, bG)):
            for k in range(8):
                lhs = yaT4[:, k, :] if k < 4 else ybT4[:, k - 4, :]
                mm(pb.ap, lhs, w_out_v[:, k, half * 512:(half + 1) * 512], k == 0, k == 7,
                   [yaT_[s], ybT_[i % 3], w_out_b], [pb])
                if k % 4 == 3:
                    yield
        for half, pb in ((0, bF), (1, bG)):
            tt("dve", rr.ap[:, half * 512:(half + 1) * 512], pb.ap, g1p_b.ap[:, half * 512:(half + 1) * 512],
               ALU.mult, [pb, g1p_b], [rr])
            yield
        stt(rr.ap, xi.ap, ALPHA, rr.ap, ALU.mult, ALU.add, [xi, rr], [rr])
        yield
        yield from layernorm_tm(rr, D, C_EPS, ln1g_b, ln1b_b, x1, stT)
        dma("sp", x1scr[i * P:(i + 1) * P, :], x1.ap, [x1], [x1_bufs[i]])
        hb_ = h2b[i % 2]
        tt("pool", h2.ap, x1.ap, sc2p_b.ap, ALU.mult, [x1, sc2p_b], [h2])
        yield
        tt("dve", h2.ap, h2.ap, sh2_b.ap, ALU.add, [h2, sh2_b], [h2])
        cp("act", hb_.ap, h2.ap, [h2], [hb_])
        yield
        for half, pb in ((0, bF), (1, bG)):
            for k4 in range(4):
                kc = half * 4 + k4
                tr(v4(pb.ap)[:, k4, :], h2.ap[:, kc * P:(kc + 1) * P], identf.ap, [h2, identf], [pb])
            cp("act", h2T.ap[:, half * 512:(half + 1) * 512], pb.ap, [pb], [h2T])
            yield
        h2T_v = v8(h2T.ap)
        for kc in range(8):
            mm(bF.ap[:, 0:36], h2T_v[:, kc, :], RW_v[:, kc, :], kc == 0, kc == 7, [h2T, RW], [bF])
        yield
        tt("dve", lg.ap, bF.ap[:, 0:36], rb_b.ap, ALU.add, [bF, rb_b], [lg])
        yield

    def T2(i):
        lg = lg_[i % 2]
        hb_ = h2b[i % 2]
        red(sm.ap[:, 0:1], lg.ap[:, 0:4], ALU.max, [lg], [sm])
        ts("dve", sm.ap[:, 1:2], sm.ap[:, 0:1], -1.0, None, ALU.mult, None, [sm], [sm])
        yield
        ts("dve", sm.ap[:, 4:8], lg.ap[:, 0:4], sm.ap[:, 0:1], None, ALU.is_equal, None, [lg, sm], [sm])
        act(sm.ap[:, 12:16], lg.ap[:, 0:4], AF.Exp, [lg, sm], [sm], bias=sm.ap[:, 1:2])
        yield
        red(sm.ap[:, 2:3], sm.ap[:, 12:16], ALU.add, [sm], [sm])
        S.op("dve", lambda h: h.reciprocal(out=sm.ap[:, 3:4], in_=sm.ap[:, 2:3]), [sm], [sm])
        yield
        ts("dve", sm.ap[:, 8:12], sm.ap[:, 4:8], 1.0, 1e30, ALU.subtract, ALU.mult, [sm], [sm])
        for g in range(4):
            ts("dve", ml.ap[:, g * 8:(g + 1) * 8], lg.ap[:, 4 + g * 8:4 + (g + 1) * 8], sm.ap[:, 8 + g:9 + g], None,
               ALU.add, None, [lg, sm], [ml])
        yield
        S.op("dve", lambda h: h.max(out=mx8.ap, in_=ml.ap), [ml], [mx8])
        S.op("dve", lambda h: h.max_index(out=ix8.ap, in_max=mx8.ap, in_values=ml.ap), [ml, mx8], [ix8])
        yield
        tt("dve", sm.ap[:, 16:17], mx8.ap[:, 1:2], mx8.ap[:, 0:1], ALU.subtract, [mx8], [sm])
        act(sm.ap[:, 17:18], sm.ap[:, 16:17], AF.Exp, [sm], [sm])
        yield
        ts("dve", sm.ap[:, 18:19], sm.ap[:, 17:18], 1.0, None, ALU.add, None, [sm], [sm])
        S.op("dve", lambda h: h.reciprocal(out=sm.ap[:, 19:20], in_=sm.ap[:, 18:19]), [sm], [sm])
        yield
        tt("dve", wgt0.ap[:, i:i + 1], sm.ap[:, 3:4], sm.ap[:, 19:20], ALU.mult, [sm], [wgt0])
        tt("dve", wgt1.ap[:, i:i + 1], wgt0.ap[:, i:i + 1], sm.ap[:, 17:18], ALU.mult, [sm, wgt0], [wgt1])
        yield
        cp("dve", sm.ap[:, 20:22], ix8.ap[:, 0:2], [ix8], [sm])
        ts("dve", A0.ap, eidx.ap, sm.ap[:, 20:21], None, ALU.is_equal, None, [eidx, sm], [A0])
        ts("dve", A1.ap, eidx.ap, sm.ap[:, 21:22], None, ALU.is_equal, None, [eidx, sm], [A1])
        tt("dve", AA.ap, A0.ap, A1.ap, ALU.add, [A0, A1], [AA])
        yield
        mm(bT.ap[:, 0:32], ustrict.ap, AA.ap, True, True, [ustrict, AA], [bT])
        mm(bT.ap[:, 64:96], ones_f.ap, AA.ap, True, True, [ones_f, AA], [bT])
        yield
        tt("dve", Cc.ap, bT.ap[:, 0:32], cntbase.ap, ALU.add, [bT, cntbase], [Cc])
        ts("dve", Cc.ap, Cc.ap, float(CAP - 1), None, ALU.min, None, [Cc], [Cc])
        tt("dve", Cc.ap, Cc.ap, ebase.ap, ALU.add, [Cc, ebase], [Cc])
        tt("dve", cntbase.ap, cntbase.ap, bT.ap[:, 64:96], ALU.add, [cntbase, bT], [cntbase])
        yield
        tt("dve", A0.ap, A0.ap, Cc.ap, ALU.mult, [A0, Cc], [A0])
        tt("dve", A1.ap, A1.ap, Cc.ap, ALU.mult, [A1, Cc], [A1])
        red(sm.ap[:, 22:23], A0.ap, ALU.add, [A0], [sm])
        red(sm.ap[:, 23:24], A1.ap, ALU.add, [A1], [sm])
        yield
        ts("dve", sm.ap[:, 22:24], sm.ap[:, 22:24], 0.0, float(NE * CAP - 1), ALU.max, ALU.min, [sm], [sm])
        cp("dve", slot0.ap[:, i:i + 1], sm.ap[:, 22:23], [sm], [slot0])
        cp("dve", slot1.ap[:, i:i + 1], sm.ap[:, 23:24], [sm], [slot1])
        yield
        for sl in (slot0, slot1):
            S.dma("pool", (lambda sl_, hb__, ii: (lambda h: h.indirect_dma_start(
                out=xpad, out_offset=bass.IndirectOffsetOnAxis(ap=sl_.ap[:, ii:ii + 1], axis=0),
                in_=hb__.ap, in_offset=None)))(sl, hb_, i), [hb_, sl], [Buf()])
        yield

    def stage(g, k):
        return g(k) if 0 <= k < NT else None

    for t in range(NT + 5):
        if t < NT:
            interleave(FE(t))
        interleave(stage(M2a, t - 1), stage(FX, t), stage(T1, t - 3), stage(M2b, t - 2), stage(M1, t - 1),
                   stage(T2, t - 4))

    if dbg:
        dbgm = A.alloc(4 * NT + NE, F32, "dbgm")
        cp("dve", dbgm.ap[:, 0:NT], slot0.ap, [slot0], [dbgm])
        cp("dve", dbgm.ap[:, NT:2 * NT], slot1.ap, [slot1], [dbgm])
        cp("dve", dbgm.ap[:, 2 * NT:3 * NT], wgt0.ap, [wgt0], [dbgm])
        cp("dve", dbgm.ap[:, 3 * NT:4 * NT], wgt1.ap, [wgt1], [dbgm])
        cp("dve", dbgm.ap[:, 4 * NT:4 * NT + NE], cntbase.ap, [cntbase], [dbgm])
        dma("sp", dbg_misc, dbgm.ap, [dbgm], [Buf()])
    S.barrier()
    A.release(m_persist)
    if stop == "p1":
        S.run()
        for cm in reversed(bank_cms):
            cm.__exit__(None, None, None)
        arena_cm.__exit__(None, None, None)
        S.close()
        return nc

    wu = [A.alloc(8 * 1024, BF16, "wu%d" % i) for i in range(3)]
    wd = [A.alloc(4 * 1024, BF16, "wd%d" % i) for i in range(3)]
    Xe = [[A.alloc(D, BF16, "Xe%d_%d" % (i, s)) for s in range(NSB)] for i in range(2)]
    XeT = [A.alloc(8 * CAP, BF16, "XeT%d" % i) for i in range(2)]
    sgt = [A.alloc(CAP, F32, "sgt%d" % i) for i in range(2)]
    gt_ = [A.alloc(CAP, F32, "gt%d" % i) for i in range(2)]
    actT = [A.alloc(4 * CAP, BF16, "actT%d" % i) for i in range(2)]
    Yt = [A.alloc(D, BF16, "Yt%d" % i) for i in range(6)]
    ypad_b = Buf("ypad")
    print("arena after phase2 alloc:", A.off, "of", A.nbytes)
    WCH = 4096
    stg = [A.alloc(WCH, F32, "stg%d" % i) for i in range(4)]
    print("arena after phase2 alloc (+staging):", A.off, "of", A.nbytes)
    stg_n = [0]

    def wload(e):
        bw = e % 3
        chunks = [(wu[bw].ap[:, 0:WCH], w_up[e, :, 0:WCH], wu[bw]),
                  (wu[bw].ap[:, WCH:2 * WCH], w_up[e, :, WCH:2 * WCH], wu[bw]),
                  (wd[bw].ap[:, 0:WCH], w_down[e, :, :], wd[bw])]
        pend = []

        def cast(d_, s_, t_):
            h_ = WCH // 2
            cp("act", d_[:, 0:h_], s_.ap[:, 0:h_], [s_], [t_])
            yield
            cp("dve", d_[:, h_:WCH], s_.ap[:, h_:WCH], [s_], [t_])
            yield

        def cast_wd(d_, s_, t_):
            tt("pool", d_.rearrange("p (k n) -> p k n", k=4), s_.ap.rearrange("p (k n) -> p k n", k=4),
               g2p_b.ap.unsqueeze(1).to_broadcast([P, 4, D]), ALU.mult, [s_, g2p_b], [t_])
            yield

        for ci, (dst, src, dtl) in enumerate(chunks):
            st = stg[stg_n[0] % 4]
            stg_n[0] += 1
            dma("sp", st.ap, src, [], [st])
            pend.append((dst, st, dtl, ci))
            yield
            if len(pend) > 2:
                d_, s_, t_, c_ = pend.pop(0)
                yield from (cast_wd(d_, s_, t_) if c_ == 2 else cast(d_, s_, t_))
        for d_, s_, t_, c_ in pend:
            yield from (cast_wd(d_, s_, t_) if c_ == 2 else cast(d_, s_, t_))

    yn = [0]

    def compute(e):
        bu = e % 2
        bw = e % 3
        wuv = wu[bw].ap.rearrange("p (k n) -> p k n", k=8)
        wdv = wd[bw].ap.rearrange("p (k n) -> p k n", k=4)
        xet = XeT[bu]
        xetv = xet.ap.rearrange("p (k s) -> p k s", k=8)
        for sb_ in range(NSB):
            xe = Xe[bu][sb_]
            dma("act", xe.ap, xpad[e * CAP + sb_ * P:e * CAP + (sb_ + 1) * P, :], [xpad_b], [xe])
            pb = bk[sb_ % 2]
            pb8 = v8(pb.ap.bitcast(BF16))
            for kc in range(8):
                tr(pb8[:, kc, :], xe.ap[:, kc * P:(kc + 1) * P], identb.ap, [xe, identb], [pb])
            cp("act" if sb_ % 2 == 0 else "dve", xetv[:, :, sb_ * P:(sb_ + 1) * P], pb8, [pb], [xet])
            yield
        at = actT[bu]
        atv = at.ap.rearrange("p (k s) -> p k s", k=4)
        for cb in range(4):
            pg = bk[2 + (cb % 2)]
            pu = bk[4 + (cb % 2)]
            for kc in range(8):
                mm(pg.ap[:, 0:CAP], wuv[:, kc, cb * P:(cb + 1) * P], xetv[:, kc, :], kc == 0, kc == 7, [wu[bw], xet], [pg])
            yield
            for kc in range(8):
                mm(pu.ap[:, 0:CAP], wuv[:, kc, 512 + cb * P:512 + (cb + 1) * P], xetv[:, kc, :], kc == 0, kc == 7,
                   [wu[bw], xet], [pu])
            yield
            s_ = sgt[cb % 2]
            g_ = gt_[cb % 2]
            act(s_.ap, pg.ap[:, 0:CAP], AF.Sigmoid, [pg], [s_])
            tt("dve", g_.ap, pg.ap[:, 0:CAP], s_.ap, ALU.mult, [pg, s_], [g_])
            tt("dve", atv[:, cb, :], pu.ap[:, 0:CAP], g_.ap, ALU.mult, [pu, g_], [at])
            yield
        for sb_ in range(NSB):
            y = Yt[yn[0] % 6]
            yn[0] += 1
            for half in range(2):
                pb = bk[6 + half]
                for hc in range(4):
                    mm(pb.ap, atv[:, hc, sb_ * P:(sb_ + 1) * P], wdv[:, hc, half * 512:(half + 1) * 512],
                       hc == 0, hc == 3, [at, wd[bw]], [pb])
                cp("act" if half == 0 else "dve", y.ap[:, half * 512:(half + 1) * 512], pb.ap, [pb], [y])
                yield
            dma("act", ypad[e * CAP + sb_ * P:e * CAP + (sb_ + 1) * P, :], y.ap, [y], [Buf()])

    interleave(wload(0))
    for e in range(NE):
        interleave(compute(e), wload(e + 1) if e + 1 < NE else None)

    if dbg:
        for r in range(NE * CAP // P):
            dma("sp", dbg_ypad[r * P:(r + 1) * P, :], ypad[r * P:(r + 1) * P, :], [ypad_b], [Buf()])
            dma("sp", dbg_xpad[r * P:(r + 1) * P, :], xpad[r * P:(r + 1) * P, :], [xpad_b], [Buf()])
        dma("sp", dbg_wu, wu[1].ap, [wu[1]], [Buf()])
    S.barrier()
    A.release(m_persist)

    NW3 = 4
    Y0 = [A.alloc(D, BF16, "Y0_%d" % i) for i in range(NW3)]
    Y1 = [A.alloc(D, BF16, "Y1_%d" % i) for i in range(NW3)]
    x1t = [A.alloc(D, F32, "x1t%d" % i) for i in range(NW3)]
    mt = [A.alloc(D, F32, "mt%d" % i) for i in range(NW3)]
    ot = [A.alloc(D, F32, "ot%d" % i) for i in range(NW3)]
    st3 = [(A.alloc(12, F32, "bst6_3%d" % i), A.alloc(2, F32, "mv_3%d" % i), A.alloc(4, F32, "rs_3%d" % i))
           for i in range(NW3)]

    def P3(i):
        p2 = i % NW3
        for sl, yt in ((slot0, Y0[p2]), (slot1, Y1[p2])):
            S.dma("pool", (lambda sl_, yt_, ii: (lambda h: h.indirect_dma_start(
                out=yt_.ap, out_offset=None, in_=ypad,
                in_offset=bass.IndirectOffsetOnAxis(ap=sl_.ap[:, ii:ii + 1], axis=0))))(sl, yt, i), [sl], [yt])
        dma("sp", x1t[p2].ap, x1scr[i * P:(i + 1) * P, :], [x1_bufs[i]], [x1t[p2]])
        yield
        m = mt[p2]
        act(m.ap, Y0[p2].ap, AF.Identity, [Y0[p2], wgt0], [m], scale=wgt0.ap[:, i:i + 1])
        yield
        stt(m.ap, Y1[p2].ap, wgt1.ap[:, i:i + 1], m.ap, ALU.mult, ALU.add, [Y1[p2], wgt1, m], [m])
        yield
        stt(m.ap, x1t[p2].ap, ALPHA, m.ap, ALU.mult, ALU.add, [x1t[p2], m], [m])
        yield
        yield from layernorm_tm(m, D, C_EPS, ln2g_b, ln2b_b, ot[p2], st3[p2])
        dma("sp", out[i * P:(i + 1) * P, :], ot[p2].ap, [ot[p2]], [Buf()])
        yield

    for i in range(0, NT, NW3):
        interleave(*[P3(i + w) for w in range(NW3)])
    S.barrier(["sp"])
    S.run()
    for cm in reversed(bank_cms):
        cm.__exit__(None, None, None)
    arena_cm.__exit__(None, None, None)
    S.close()
    return nc


def kernel(x, c, w_ada, b_ada, w_in, lb_logits, hg_norm_w, sg_ln_g, sg_ln_b, sg_w, sg_b,
           w_out, ln1_g, ln1_b, router_group_w, router_group_b, router_expert_w,
           router_expert_b, w_up, w_down, ln2_g, ln2_b):
    f = lambda a: np.ascontiguousarray(np.asarray(a, dtype=np.float32))
    x = f(x); c = f(c)
    B, SEQ, _ = x.shape
    QT = SEQ // 4
    rw = np.concatenate([f(router_group_w)[0], f(router_expert_w)[0].transpose(1, 0, 2).reshape(D, 32)], axis=1)
    rb = np.concatenate([f(router_group_b)[0], f(router_expert_b)[0].reshape(32)])[None, :]
    shared = {
        "w_ada": f(w_ada)[0], "b_ada": f(b_ada), "w_in": f(w_in)[0],
        "lbT": f(np.asarray(lb_logits).reshape(2, 4, P).transpose(2, 0, 1).reshape(P, 8)),
        "gnw": f(np.asarray(hg_norm_w).reshape(P, 1)),
        "sg_ln_g": f(sg_ln_g), "sg_ln_b": f(sg_ln_b),
        "sg_wT": f(np.asarray(sg_w)[0].transpose(2, 0, 1)),
        "sg_b": f(np.asarray(sg_b).reshape(1, 512)),
        "w_out": f(w_out)[0], "ln1_g": f(ln1_g), "ln1_b": f(ln1_b),
        "rw": f(rw), "rb": f(rb), "w_up": np.ascontiguousarray(f(w_up)[0].reshape(NE, 8, P, 2 * HID).transpose(0, 2, 1, 3)).reshape(NE, P, 8 * 2 * HID),
        "w_down": np.ascontiguousarray(f(w_down)[0].reshape(NE, 4, P, D).transpose(0, 2, 1, 3)).reshape(NE, P, 4 * D),
        "ln2_g": f(ln2_g), "ln2_b": f(ln2_b),
    }
    in_maps = []
    for core in range(8):
        b, q = core // 4, core % 4
        xo = x[b, q * QT:(q + 1) * QT, :]
        xp = np.zeros((NPREV * P, D), np.float32)
        val = np.zeros((P, NPREV), np.float32)
        npv = q * QT
        if npv:
            xp[NPREV * P - npv:, :] = x[b, :npv, :]
            val[:, NPREV - npv // P:] = 1.0
        d = dict(shared)
        d.update({"x_own": np.ascontiguousarray(xo), "x_prev": xp, "valid": val,
                  "c_row": np.ascontiguousarray(c[b:b + 1, :])})
        in_maps.append(d)
    if _DBG:
        return in_maps
    nc = build_nc()
    res = run_bass_kernel_spmd(nc, in_maps, core_ids=list(range(8)))
    outp = np.zeros((B, SEQ, D), np.float32)
    for core in range(8):
        b, q = core // 4, core % 4
        outp[b, q * QT:(q + 1) * QT, :] = np.asarray(res.results[core]["out"], dtype=np.float32)
    return outp
```
